# Optimizing a Trainium2 kernel written in Bass

```python
import math
import jax, jax.numpy as jnp
from jax import lax
import numpy as np

D_MODEL = 1024
BATCH = 16
SEQ = 2048
DEPTH = 1

PLE_DIM = 256
WIDTH_A = 1024
CONV_A = 3
WIDTH_B = 1024
CONV_B = 31
N_GROUPS = 4
EXPERTS_PER_GROUP = 8
N_EXPERTS = N_GROUPS * EXPERTS_PER_GROUP
TOP_K_IN_GROUP = 2
D_EXPERT = 512
ROW_BLOCK = 128
EPS = 1e-6
IN_COLS = 3 * WIDTH_A + 2 * WIDTH_B + 2 * D_MODEL

kernel_name = "hybrid_conv_conformer_hiermoe_ple"


def rmsnorm(x, g):
    xf = x.astype(jnp.float32)
    y = xf * lax.rsqrt(jnp.mean(xf * xf, axis=-1, keepdims=True) + EPS)
    return (y * g.astype(jnp.float32)).astype(x.dtype)


def layernorm(x, g, b):
    xf = x.astype(jnp.float32)
    mu = jnp.mean(xf, axis=-1, keepdims=True)
    var = jnp.mean(jnp.square(xf - mu), axis=-1, keepdims=True)
    y = (xf - mu) * lax.rsqrt(var + EPS)
    return (y * g.astype(jnp.float32) + b.astype(jnp.float32)).astype(x.dtype)


def causal_dwconv(u, w):
    k, c = w.shape
    return lax.conv_general_dilated(
        u, w.astype(u.dtype)[:, None, :], window_strides=(1,), padding=[(k - 1, 0)],
        dimension_numbers=("NWC", "WIO", "NWC"), feature_group_count=c)


def token_mixer(h, w_in, conv_a_w, w_out_a, conv_b_w, conv_b_b, ln_b_g, ln_b_b, w_out_b, b_gate, w_o):
    proj = h @ w_in
    cuts = [WIDTH_A, 2 * WIDTH_A, 3 * WIDTH_A, 3 * WIDTH_A + WIDTH_B, 3 * WIDTH_A + 2 * WIDTH_B]
    a_b, a_c, a_x, c_val, c_gate, gates = jnp.split(proj, cuts, axis=-1)
    y_a = (a_b * causal_dwconv(a_c * a_x, conv_a_w)) @ w_out_a
    u = c_val * jax.nn.sigmoid(c_gate)
    u = causal_dwconv(u, conv_b_w) + conv_b_b
    u = jax.nn.silu(layernorm(u, ln_b_g, ln_b_b))
    y_b = u @ w_out_b
    g = jax.nn.sigmoid(gates + b_gate)
    g_a, g_b = jnp.split(g, 2, axis=-1)
    return (g_a * y_a + g_b * y_b) @ w_o


def hierarchical_moe(h, w_rg, b_rg, w_re, b_re, w_gate, w_up, w_down):
    bsz, seq, d = h.shape
    t = bsz * seq
    hf = h.reshape(t, d)
    g_logits = (hf @ w_rg + b_rg).astype(jnp.float32)
    g_prob = jax.nn.softmax(g_logits, axis=-1)
    grp = jnp.argmax(g_logits, axis=-1).astype(jnp.int32)
    p_grp = jnp.take_along_axis(g_prob, grp[:, None], axis=-1)
    e_logits = (hf @ w_re + b_re).astype(jnp.float32).reshape(t, N_GROUPS, EXPERTS_PER_GROUP)
    e_logits = jnp.take_along_axis(e_logits, grp[:, None, None], axis=1)[:, 0]
    top_v, top_i = lax.top_k(e_logits, TOP_K_IN_GROUP)
    gate_w = jax.nn.softmax(top_v, axis=-1) * p_grp
    expert_id = grp[:, None] * EXPERTS_PER_GROUP + top_i.astype(jnp.int32)

    n_assign = t * TOP_K_IN_GROUP
    flat_e = expert_id.reshape(n_assign)
    flat_w = gate_w.reshape(n_assign)
    order = jnp.argsort(flat_e)
    sorted_e = flat_e[order]
    tok = order // TOP_K_IN_GROUP
    counts = jnp.bincount(flat_e, length=N_EXPERTS)
    padded = (counts + ROW_BLOCK - 1) // ROW_BLOCK * ROW_BLOCK
    start = jnp.cumsum(counts) - counts
    pad_end = jnp.cumsum(padded)
    pad_start = pad_end - padded
    dest = pad_start[sorted_e] + (jnp.arange(n_assign, dtype=jnp.int32) - start[sorted_e])
    n_blocks = -(-(n_assign + N_EXPERTS * (ROW_BLOCK - 1)) // ROW_BLOCK)
    buf = jnp.zeros((n_blocks * ROW_BLOCK, d), hf.dtype).at[dest].set(hf[tok])
    block_start = jnp.arange(n_blocks, dtype=jnp.int32) * ROW_BLOCK
    block_expert = jnp.minimum(jnp.searchsorted(pad_end, block_start, side="right"), N_EXPERTS - 1)

    def expert_block(args):
        xb, e = args
        a = xb @ w_gate[e]
        b = xb @ w_up[e]
        return (jax.nn.silu(a) * b) @ w_down[e]

    y_buf = lax.map(expert_block, (buf.reshape(n_blocks, ROW_BLOCK, d), block_expert)).reshape(-1, d)
    y = y_buf[dest] * flat_w[order][:, None].astype(hf.dtype)
    out = jax.ops.segment_sum(y, tok, num_segments=t)
    return out.reshape(bsz, seq, d)


def setup_inputs(seed: int = 0) -> dict:
    key = jax.random.key(seed)
    ks = jax.random.split(key, 32)
    n = lambda k, shape, s: jax.random.normal(k, shape, jnp.float32) * s
    L, D = DEPTH, D_MODEL
    return {
        "x": n(ks[0], (BATCH, SEQ, D), 1.0),
        "p": n(ks[1], (DEPTH, BATCH, SEQ, PLE_DIM), 1.0),
        "mix_norm_g": 1.0 + n(ks[2], (L, D), 0.02),
        "w_in": n(ks[3], (L, D, IN_COLS), D ** -0.5),
        "conv_a_w": n(ks[4], (L, CONV_A, WIDTH_A), CONV_A ** -0.5),
        "w_out_a": n(ks[5], (L, WIDTH_A, D), WIDTH_A ** -0.5),
        "conv_b_w": n(ks[6], (L, CONV_B, WIDTH_B), CONV_B ** -0.5),
        "conv_b_b": n(ks[7], (L, WIDTH_B), 0.02),
        "ln_b_g": 1.0 + n(ks[8], (L, WIDTH_B), 0.02),
        "ln_b_b": n(ks[9], (L, WIDTH_B), 0.02),
        "w_out_b": n(ks[10], (L, WIDTH_B, D), WIDTH_B ** -0.5),
        "b_gate": n(ks[11], (L, 2 * D), 0.02),
        "w_o": n(ks[12], (L, D, D), D ** -0.5),
        "ffn_norm_g": 1.0 + n(ks[13], (L, D), 0.02),
        "w_router_group": n(ks[14], (L, D, N_GROUPS), D ** -0.5),
        "b_router_group": n(ks[15], (L, N_GROUPS), 0.01),
        "w_router_expert": n(ks[16], (L, D, N_EXPERTS), D ** -0.5),
        "b_router_expert": n(ks[17], (L, N_EXPERTS), 0.01),
        "w_exp_gate": n(ks[18], (L, N_EXPERTS, D, D_EXPERT), D ** -0.5),
        "w_exp_up": n(ks[19], (L, N_EXPERTS, D, D_EXPERT), D ** -0.5),
        "w_exp_down": n(ks[20], (L, N_EXPERTS, D_EXPERT, D), D_EXPERT ** -0.5),
        "ple_norm_g": 1.0 + n(ks[21], (L, D), 0.02),
        "w_ple_gate": n(ks[22], (L, D, D), D ** -0.5),
        "w_ple_proj": n(ks[23], (L, PLE_DIM, D), PLE_DIM ** -0.5),
        "final_norm_g": 1.0 + n(ks[24], (D,), 0.02),
    }


def reference(x, p, mix_norm_g, w_in, conv_a_w, w_out_a, conv_b_w, conv_b_b, ln_b_g, ln_b_b,
              w_out_b, b_gate, w_o, ffn_norm_g, w_router_group, b_router_group, w_router_expert,
              b_router_expert, w_exp_gate, w_exp_up, w_exp_down, ple_norm_g, w_ple_gate, w_ple_proj,
              final_norm_g):
    for i in range(DEPTH):
        h = rmsnorm(x, mix_norm_g[i])
        x = x + token_mixer(h, w_in[i], conv_a_w[i], w_out_a[i], conv_b_w[i], conv_b_b[i],
                            ln_b_g[i], ln_b_b[i], w_out_b[i], b_gate[i], w_o[i])
        h = rmsnorm(x, ffn_norm_g[i])
        x = x + hierarchical_moe(h, w_router_group[i], b_router_group[i], w_router_expert[i],
                                 b_router_expert[i], w_exp_gate[i], w_exp_up[i], w_exp_down[i])
        hp = rmsnorm(x, ple_norm_g[i])
        x = x + jax.nn.sigmoid(hp @ w_ple_gate[i]) * (p[i] @ w_ple_proj[i])
    return rmsnorm(x, final_norm_g)
```

```python
import numpy as np
import concourse.bass as bass
import concourse.mybir as mybir
from concourse.bass_utils import run_bass_kernel_spmd
from contextlib import ExitStack

F32 = mybir.dt.float32
BF16 = mybir.dt.bfloat16
ALU = mybir.AluOpType
AF = mybir.ActivationFunctionType
AX = mybir.AxisListType

NCORES = 8
D = 1024
SEQ = 2048
TOK_CORE = 4096
PT = 1024
NPASS = TOK_CORE // PT
NJ = PT // 128
NTT = PT // 512
NE = 32
DE = 512
PLE = 256
EPS = 1e-6
NV = 42
NT5 = 8
CAP = 512
CG = 512
NBLK = CAP // 128
NG = TOK_CORE // 128
ENG_KEYS = ("pe", "act", "dve", "pool", "sp")
STRICT_SYNC = [True]


class Region:
    __slots__ = ("w", "rs", "name")

    def __init__(self, name=""):
        self.w = None
        self.rs = []
        self.name = name


class Sched:
    def __init__(self, nc, n_dma_sems=24):
        self.nc = nc
        self.ops = {k: [] for k in ENG_KEYS}
        self.count = {k: 0 for k in ENG_KEYS}
        self.seen = {k: {} for k in ENG_KEYS}
        self.n_dma_sems = n_dma_sems
        self.ring = n_dma_sems // 2
        self.dma_k = {"sw": 0, "hw": 0}
        self.needed = set()

    def _deps(self, reads, writes):
        deps = set()
        for r in reads:
            if r.w is not None:
                deps.add(r.w)
        for w in writes:
            if w.w is not None:
                deps.add(w.w)
            for t in w.rs:
                deps.add(t)
        return deps

    def _filter(self, eng, deps, raw, is_dma=False):
        waits = []
        seen = self.seen[eng]
        for t in sorted(deps):
            kind, key, val = t
            if kind == "e" and key == eng and not is_dma and not (STRICT_SYNC[0] and eng != "pe"):
                if eng == "pe" or t not in raw:
                    continue
            sk = (kind, key)
            if seen.get(sk, 0) >= val:
                continue
            seen[sk] = val
            waits.append(t)
            self.needed.add(t)
        return waits

    def _finish(self, tok, reads, writes):
        for r in reads:
            r.rs.append(tok)
            if len(r.rs) > 64:
                best = {}
                for t in r.rs:
                    k = (t[0], t[1])
                    if k not in best or best[k][2] < t[2]:
                        best[k] = t
                r.rs = list(best.values())
        for w in writes:
            w.w = tok
            w.rs = []

    def op(self, eng, fn, reads=(), writes=()):
        deps = self._deps(reads, writes)
        raw = set(r.w for r in reads if r.w is not None)
        waits = self._filter(eng, deps, raw)
        self.count[eng] += 1
        tok = ("e", eng, self.count[eng])
        self.ops[eng].append((waits, fn, tok))
        self._finish(tok, reads, writes)
        return tok

    def dma(self, eng, fn, reads=(), writes=()):
        deps = self._deps(reads, writes)
        rk = "sw" if eng == "pool" else "hw"
        k = self.dma_k[rk]
        self.dma_k[rk] += 1
        s = k % self.ring + (0 if rk == "sw" else self.ring)
        val = 16 * (k // self.ring + 1)
        if val > 16:
            deps.add(("d", s, val - 16))
        raw = set(r.w for r in reads if r.w is not None)
        waits = self._filter(eng, deps, raw, is_dma=True)
        tok = ("d", s, val)
        self.ops[eng].append((waits, fn, tok))
        self._finish(tok, reads, writes)
        return tok

    def final_wait(self, eng, toks):
        waits = self._filter(eng, set(toks), set())
        self.ops[eng].append((waits, None, None))

    def emit(self):
        nc = self.nc
        rank = {}
        for k in ENG_KEYS:
            idxs = sorted(v for (kind, key, v) in self.needed if kind == "e" and key == k)
            for i, v in enumerate(idxs):
                rank[(k, v)] = i + 1
        with ExitStack() as es:
            esem = {k: es.enter_context(nc.semaphore("s_" + k)) for k in ENG_KEYS}
            dsem = [es.enter_context(nc.semaphore("d_%d" % i)) for i in range(self.n_dma_sems)]
            block = es.enter_context(nc.Block())

            def run(eng_key):
                def body(e):
                    for waits, fn, tok in self.ops[eng_key]:
                        for (kind, key, val) in waits:
                            if kind == "e":
                                e.wait_ge(esem[key], rank[(key, val)])
                            else:
                                e.wait_ge(dsem[key], val)
                        if fn is None:
                            continue
                        ins = fn(e)
                        if tok[0] == "d":
                            ins.then_inc(dsem[tok[1]], 16)
                        elif tok in self.needed:
                            ins.then_inc(esem[eng_key], 1)
                return body

            block.tensor(run("pe"))
            block.scalar(run("act"))
            block.vector(run("dve"))
            block.gpsimd(run("pool"))
            block.sync(run("sp"))


def build_nc(n_pass=NPASS, n_exp=NE):
    nc = bass.Bass("TRN2", target_bir_lowering=False)

    def din(name, shape):
        return nc.dram_tensor(name, list(shape), F32, kind="ExternalInput").ap()

    x_d = din("x", [TOK_CORE, D])
    p_d = din("p", [TOK_CORE, PLE])
    ident_d = din("ident", [128, 128])
    ustr_d = din("ustrict", [128, 128])
    ebase_d = din("ebase", [128, NE])
    mix_g = din("mix_norm_g", [D])
    w_in = din("w_in", [D, 7168])
    conv_a_w = din("conv_a_w", [3, D])
    w_out_a = din("w_out_a", [D, D])
    conv_b_w = din("conv_b_w", [31, D])
    conv_b_b = din("conv_b_b", [D])
    ln_b_g = din("ln_b_g", [D])
    ln_b_b = din("ln_b_b", [D])
    w_out_b = din("w_out_b", [D, D])
    b_gate = din("b_gate", [2 * D])
    w_o = din("w_o", [D, D])
    ffn_g = din("ffn_norm_g", [D])
    w_rg = din("w_router_group", [D, 4])
    b_rg = din("b_router_group", [4])
    w_re = din("w_router_expert", [D, NE])
    b_re = din("b_router_expert", [NE])
    w_eg = din("w_exp_gate", [NE, D, DE])
    w_eu = din("w_exp_up", [NE, D, DE])
    w_ed = din("w_exp_down", [NE, DE, D])
    ple_g = din("ple_norm_g", [D])
    w_pg = din("w_ple_gate", [D, D])
    w_pp = din("w_ple_proj", [PLE, D])
    fin_g = din("final_norm_g", [D])
    out_d = nc.dram_tensor("out", [TOK_CORE, D], F32, kind="ExternalOutput").ap()
    xs_d = nc.dram_tensor("xs_scratch", [TOK_CORE, D], F32).ap()
    buf_d = nc.dram_tensor("buf_scratch", [NE * CAP, D], BF16).ap()
    ybuf_d = nc.dram_tensor("ybuf_scratch", [NE * CAP, D], BF16).ap()

    es = ExitStack()
    with es:
        def sb(name, shape, dt):
            return es.enter_context(nc.sbuf_tensor(name, list(shape), dt))

        S = Sched(nc)

        x_tok = sb("x_tok", [128, NJ, D], F32)
        hT = sb("hT", [128, 8, PT], BF16)
        uA = sb("uA", [128, 8, PT], BF16)
        uB = sb("uB", [128, 8, PT], BF16)
        mT = sb("mT", [128, 8, PT], BF16)
        slots = [sb("wslot%d" % i, [128, 12288], BF16) for i in range(2)]
        haloA = sb("haloA", [128, 8, 2], F32)
        haloB = sb("haloB", [128, 8, 30], BF16)
        identf = sb("identf", [128, 128], F32)
        identb = sb("identb", [128, 128], BF16)
        onesm = sb("onesm", [128, 128], BF16)
        ones_row = sb("ones_row", [1, 128], BF16)
        epsc = sb("epsc", [128, 1], F32)
        cv = sb("cv", [128, 8, NV], F32)
        gbc = sb("gbc", [128, D], F32)
        wr = sb("wr", [128, 8, 36], BF16)
        rb = sb("rb", [1, 36], BF16)
        ustr_b = sb("ustr_b", [128, 128], BF16)
        ones128 = sb("ones128", [128, 128], BF16)
        ebase_t = sb("ebase_t", [128, NE], F32)
        cnt_bc = sb("cnt_bc", [128, NE], F32)
        slot_i = sb("slot_i", [128, 2 * NG], mybir.dt.int32)
        slot_g = sb("slot_g", [128, 2 * NG], mybir.dt.int32)
        w12 = sb("w12", [128, 2 * NG], F32)
        maskb = sb("maskb", [128, NJ, NE], BF16)
        hn = [sb("hn%d" % i, [128, D], BF16) for i in range(2)]
        stat = sb("stat", [128, 64], F32)
        uGb = [sb("uGb%d" % i, [128, 30 + PT], BF16) for i in range(2)]
        vA = sb("vA", [128, 2 + PT], F32)
        abuf = sb("abuf", [128, PT], F32)
        vrows = abuf
        accs = [sb("acc%d" % i, [128, PT], F32) for i in range(3)]
        t512 = [sb("t512_%d" % i, [128, 512], F32) for i in range(NT5)]
        sqb = [sb("sqb%d" % i, [128, 512], BF16) for i in range(2)]
        ptile = [sb("ptile%d" % i, [128, PLE], F32) for i in range(3)]
        ptb = [sb("ptb%d" % i, [128, PLE], BF16) for i in range(2)]
        rt = sb("rt", [128, 192], F32)

        banks = [es.enter_context(nc.psum_tensor("bank%d" % i, [128, 512], F32)) for i in range(8)]

        R_x = [[Region() for _ in range(2)] for _ in range(NJ)]
        R_hT = [Region() for _ in range(NJ)]
        R_uA = [[Region() for _ in range(NTT)] for _ in range(8)]
        R_uB = [[Region() for _ in range(NTT)] for _ in range(8)]
        R_m = [[Region() for _ in range(NTT)] for _ in range(8)]
        R_slot = [Region(), Region(), Region()]
        R_haloA = [Region() for _ in range(8)]
        R_haloB = [Region() for _ in range(8)]
        R_const = Region()
        R_cv = Region()
        R_cnt = Region()
        R_slotw = [Region() for _ in range(NG)]
        R_gbc = Region()
        R_hn = [Region(), Region()]
        R_stat = [Region() for _ in range(64)]
        R_uG = [Region(), Region()]
        R_vA = Region()
        R_ab = Region()
        R_vrows = R_ab
        R_acc = [Region() for _ in range(3)]
        R_t = [Region() for _ in range(NT5)]
        R_sq = [Region(), Region()]
        R_pt = [Region(), Region(), Region()]
        R_ptb = [Region(), Region()]
        R_rt = Region()
        R_bank = [Region() for _ in range(8)]
        out_toks = []
        state = {"bank": 0, "slot": 0, "t": 0, "stat": 0, "hn": 0}

        resv = set()

        def next_bank():
            while True:
                b = state["bank"]
                state["bank"] = (b + 1) % 8
                if b not in resv:
                    return b

        t_resv = set()

        def next_t():
            while True:
                t = state["t"]
                state["t"] = (t + 1) % NT5
                if t not in t_resv:
                    return t

        def next_stat():
            t = state["stat"]
            state["stat"] = (t + 1) % 64
            return t

        def mm(out, lhsT, rhs, start, stop, reads, writes):
            S.op("pe", lambda e: e.matmul(out=out, lhsT=lhsT, rhs=rhs, start=start, stop=stop), reads, writes)

        def act(out, in_, func, reads, writes, bias=None, scale=None, accum_out=None):
            kw = {}
            if bias is not None:
                kw["bias"] = bias
            if scale is not None:
                kw["scale"] = scale
            if accum_out is not None:
                kw["accum_out"] = accum_out
            S.op("act", lambda e: e.activation(out=out, in_=in_, func=func, **kw), reads, writes)

        def tt(eng, out, in0, in1, op, reads, writes):
            S.op(eng, lambda e: e.tensor_tensor(out=out, in0=in0, in1=in1, op=op), reads, writes)

        def ts(eng, out, in0, s1, s2, op0, op1, reads, writes):
            if s2 is None:
                S.op(eng, lambda e: e.tensor_scalar(out=out, in0=in0, scalar1=s1, scalar2=None, op0=op0), reads, writes)
            else:
                S.op(eng, lambda e: e.tensor_scalar(out=out, in0=in0, scalar1=s1, scalar2=s2, op0=op0, op1=op1), reads, writes)

        def stt(out, in0, scalar, in1, op0, op1, reads, writes):
            S.op("dve", lambda e: e.scalar_tensor_tensor(out=out, in0=in0, scalar=scalar, in1=in1, op0=op0, op1=op1), reads, writes)

        def copy(eng, out, in_, reads, writes):
            S.op(eng, lambda e: e.tensor_copy(out=out, in_=in_), reads, writes)

        def memset(eng, ap, val, writes):
            S.op(eng, lambda e: e.memset(ap, val), (), writes)

        def dma(eng, out, in_, reads, writes, slow=False):
            if slow:
                return S.dma(eng, lambda e: e.dma_start(out=out, in_=in_, allow_slow_non_contiguous=True), reads, writes)
            return S.dma(eng, lambda e: e.dma_start(out=out, in_=in_), reads, writes)

        for j in range(NJ):
            dma("sp", x_tok[:, j, :], x_d[j * 128:(j + 1) * 128, :], (), [R_x[j][0], R_x[j][1]])

        dma("sp", identf[:], ident_d, (), [R_const])
        copy("dve", identb[:], identf[:], [R_const], [R_const])
        memset("pool", onesm[:], 1.0 / 1024.0, [R_const])
        memset("pool", ones_row[:], 1.0, [R_const])
        memset("pool", epsc[:], EPS, [R_const])
        dma("sp", vrows[0:31, :], conv_b_w, (), [R_vrows])
        dma("sp", vrows[31:34, :], conv_a_w, (), [R_vrows])
        for r, v in ((34, conv_b_b), (35, ln_b_g), (36, ln_b_b), (39, mix_g), (40, ffn_g), (41, ple_g)):
            dma("sp", vrows[r:r + 1, :], v.rearrange("(o d) -> o d", o=1), (), [R_vrows])
        dma("sp", vrows[37:39, :], b_gate.rearrange("(o d) -> o d", o=2), (), [R_vrows])
        dma("sp", gbc[:], ffn_g.partition_broadcast(128), (), [R_gbc])
        dma("sp", accs[0][:, 0:128], ustr_d, (), [R_acc[0]])
        copy("dve", ustr_b[:], accs[0][:, 0:128], [R_acc[0]], [R_const])
        dma("sp", ebase_t[:], ebase_d, (), [R_const])
        memset("pool", ones128[:], 1.0, [R_const])
        memset("pool", cnt_bc[:], 0.0, [R_cnt])
        dma("pool", wr[:, :, 0:4], w_rg.rearrange("(k p) n -> p k n", p=128), (), [R_const])
        dma("pool", wr[:, :, 4:36], w_re.rearrange("(k p) n -> p k n", p=128), (), [R_const])
        dma("pool", rb[0:1, 0:4], b_rg.rearrange("(o d) -> o d", o=1), (), [R_const])
        dma("pool", rb[0:1, 4:36], b_re.rearrange("(o d) -> o d", o=1), (), [R_const])
        for c in range(8):
            b = next_bank()
            mm(banks[b][:, 0:NV], vrows[0:NV, c * 128:(c + 1) * 128], identf[0:NV, 0:NV], True, True,
               [R_vrows, R_const], [R_bank[b]])
            copy("dve", cv[:, c, :], banks[b][:, 0:NV], [R_bank[b]], [R_cv])
        CB0, CA0, CBB, LNG, LNB, BGA, BGB, GMIX, GFFN, GPLE = 0, 31, 34, 35, 36, 37, 38, 39, 40, 41

        def load_unit(parts, s=None):
            if s is None:
                s = state["slot"]
                state["slot"] = 1 - s
            for (off, k, n, src) in parts:
                dst = slots[s][:, off:off + k * n].rearrange("p (k n) -> p k n", k=k)
                dma("pool", dst, src.rearrange("(k p) n -> p k n", p=128), (), [R_slot[s]])
            return s

        def rstd_of(xap, xregs, junk_ap=None, junk_regs=None):
            si = next_stat()
            hb = state["hn"]
            if junk_ap is None:
                junk_ap, junk_regs = hn[hb][:], [R_hn[hb]]
            act(junk_ap, xap, AF.Square, xregs + [R_stat[si]], junk_regs + [R_stat[si]], accum_out=stat[:, si:si + 1])
            act(stat[:, si:si + 1], stat[:, si:si + 1], AF.Sqrt, [R_stat[si], R_const], [R_stat[si]], bias=epsc[:, 0:1], scale=1.0 / D)
            S.op("dve", lambda e: e.reciprocal(out=stat[:, si:si + 1], in_=stat[:, si:si + 1]), [R_stat[si]], [R_stat[si]])
            return si

        def norm_batch(tiles, gcol=None, gain_bc=False):
            sis = []
            for (xap, xregs, dst, dregs, hn_ap, hn_regs) in tiles:
                si = next_stat()
                sis.append(si)
                act(hn[0][:], xap, AF.Square, xregs + [R_stat[si]], [R_hn[0], R_stat[si]], accum_out=stat[:, si:si + 1])
            for si in sis:
                act(stat[:, si:si + 1], stat[:, si:si + 1], AF.Sqrt, [R_stat[si], R_const], [R_stat[si]], bias=epsc[:, 0:1], scale=1.0 / D)
            for si in sis:
                S.op("dve", lambda e, si=si: e.reciprocal(out=stat[:, si:si + 1], in_=stat[:, si:si + 1]), [R_stat[si]], [R_stat[si]])
            for si, (xap, xregs, dst, dregs, hn_ap, hn_regs) in zip(sis, tiles):
                if gain_bc:
                    stt(hn_ap, xap, stat[:, si:si + 1], gbc[:], ALU.mult, ALU.mult, xregs + [R_stat[si], R_gbc], hn_regs)
                else:
                    ts("dve", hn_ap, xap, stat[:, si:si + 1], None, ALU.mult, None, xregs + [R_stat[si]], hn_regs)
            pts = []

            def evac(i_):
                (b, pT) = pts[i_]
                (xap, xregs, dst, dregs, hn_ap, hn_regs) = tiles[i_]
                if gcol is not None:
                    tt("dve", dst, pT, cv[:, :, gcol:gcol + 1].to_broadcast([128, 8, 128]), ALU.mult, [R_bank[b], R_cv], dregs)
                elif i_ % 2 == 0:
                    act(dst, pT, AF.Copy, [R_bank[b]], dregs)
                else:
                    copy("dve", dst, pT, [R_bank[b]], dregs)

            for i_, (xap, xregs, dst, dregs, hn_ap, hn_regs) in enumerate(tiles):
                b = next_bank()
                pT = banks[b][:].bitcast(BF16).rearrange("p (c t) -> p c t", c=8)
                pts.append((b, pT))
                for c in range(8):
                    S.op("pe", lambda e, c=c, pT=pT, hn_ap=hn_ap: e.transpose(out=pT[:, c, :], in_=hn_ap[:, c * 128:(c + 1) * 128], identity=identb[:]),
                         hn_regs + [R_const], [R_bank[b]])
                if i_ >= 2:
                    evac(i_ - 2)
            for i_ in range(max(0, len(tiles) - 2), len(tiles)):
                evac(i_)

        def norm_T(xap, xregs, dst, dregs, gcol=None, gain_bc=False, hn_ap=None, hn_regs=None):
            si = rstd_of(xap, xregs)
            hb = state["hn"]
            state["hn"] = 1 - hb
            if hn_ap is None:
                hn_ap, hn_regs = hn[hb][:], [R_hn[hb]]
            if gain_bc:
                stt(hn_ap, xap, stat[:, si:si + 1], gbc[:], ALU.mult, ALU.mult, xregs + [R_stat[si], R_gbc], hn_regs)
            else:
                ts("dve", hn_ap, xap, stat[:, si:si + 1], None, ALU.mult, None, xregs + [R_stat[si]], hn_regs)
            b = next_bank()
            pT = banks[b][:].bitcast(BF16).rearrange("p (c t) -> p c t", c=8)
            for c in range(8):
                S.op("pe", lambda e, c=c: e.transpose(out=pT[:, c, :], in_=hn_ap[:, c * 128:(c + 1) * 128], identity=identb[:]),
                     hn_regs + [R_const], [R_bank[b]])
            if gcol is not None:
                tt("dve", dst, pT, cv[:, :, gcol:gcol + 1].to_broadcast([128, 8, 128]), ALU.mult, [R_bank[b], R_cv], dregs)
            else:
                act(dst, pT, AF.Copy, [R_bank[b]], dregs)
            return hb

        def barrier():
            toks = [("e", k, S.count[k]) for k in ("pe", "act", "dve", "pool") if S.count[k] > 0]
            for rk, base in (("sw", 0), ("hw", S.ring)):
                nk = S.dma_k[rk]
                for s_ in range(min(nk, S.ring)):
                    k_last = ((nk - 1 - s_) // S.ring) * S.ring + s_
                    toks.append(("d", base + s_, 16 * (k_last // S.ring + 1)))
            for eng in ENG_KEYS:
                S.final_wait(eng, toks)

        def hT_reads(t):
            return [R_hT[4 * t + i] for i in range(4)]

        zero_toks = []
        _breg = {}

        def bcheck(e):
            if "r" not in _breg:
                _breg["r"] = e.to_reg(NE * CAP - 1)
            return _breg["r"]

        scat_toks, xs_toks = [], []
        nxt = None
        for ps in range(n_pass):
            tok0 = ps * PT
            first_half = (ps % 2 == 0)
            for j in range(NJ):
                if ps > 0:
                    dma("sp", x_tok[:, j, :], x_d[tok0 + j * 128: tok0 + (j + 1) * 128, :], (), [R_x[j][0], R_x[j][1]])

            def s1_parts(g_):
                return [(j * 2048, 8, 256, w_in[:, j * 1024 + g_ * 256: j * 1024 + (g_ + 1) * 256]) for j in range(5)]

            def ypartsA(g_):
                return [(0, 8, 512, w_out_a[:, g_ * 512:(g_ + 1) * 512]),
                        (4096, 8, 512, w_in[:, 5120 + g_ * 512: 5120 + (g_ + 1) * 512])]

            def ypartsB(g_):
                return [(0, 8, 512, w_out_b[:, g_ * 512:(g_ + 1) * 512]),
                        (4096, 8, 512, w_in[:, 6144 + g_ * 512: 6144 + (g_ + 1) * 512])]

            if nxt is None:
                nxt = load_unit(s1_parts(0))
            norm_batch([(x_tok[:, j, :], [R_x[j][0], R_x[j][1]], hT[:, :, j * 128:(j + 1) * 128], [R_hT[j]],
                         mT[:, j, :], [R_m[j][0], R_m[j][1]]) for j in range(NJ)], gcol=GMIX)

            if ps == 0:
                for c in range(8):
                    memset("pool", mT[:, c, :], 0.0, [R_m[c][0], R_m[c][1]])
                buf_v = buf_d.rearrange("(n p) d -> p n d", p=128)
                zero_pending = list(range(NE * CAP // 128 // 8))

                def zero_some(n_):
                    for _ in range(n_):
                        if zero_pending:
                            i = zero_pending.pop(0)
                            zero_toks.append(dma("act", buf_v[:, 8 * i:8 * i + 8, :], mT[:],
                                                 [R_m[c_][t_] for c_ in range(8) for t_ in range(NTT)], [Region()]))


            dgA = accs[0][:].bitcast(BF16).rearrange("p (k m) -> p k m", m=128)
            dgB = accs[1][:].bitcast(BF16).rearrange("p (k m) -> p k m", m=128)[:, 0:15, :]

            KPE = 23

            def dg_build(c):
                tt("dve", dgA, identb[:].unsqueeze(1).to_broadcast([128, 16, 128]),
                   cv[:, c, CB0:CB0 + 16].unsqueeze(2).to_broadcast([128, 16, 128]), ALU.mult, [R_const, R_cv], [R_acc[0]])
                tt("dve", dgB[:, 0:KPE - 16, :], identb[:].unsqueeze(1).to_broadcast([128, KPE - 16, 128]),
                   cv[:, c, CB0 + 16:CB0 + KPE].unsqueeze(2).to_broadcast([128, KPE - 16, 128]), ALU.mult, [R_const, R_cv], [R_acc[1]])

            def conv_pe(c, ub):
                bks = []
                for t in range(NTT):
                    b = next_bank()
                    bks.append(b)
                    for k in range(KPE):
                        lhsT = dgA[:, k, :] if k < 16 else dgB[:, k - 16, :]
                        mm(banks[b][:], lhsT, uGb[ub][:, k + t * 512: k + (t + 1) * 512], k == 0, k == KPE - 1,
                           [R_acc[0], R_acc[1], R_uG[ub]], [R_bank[b]])
                tmps = [next_t() for _ in range(NTT)]
                for idx, k in enumerate(range(KPE, 31)):
                    for t in range(NTT):
                        if idx == 0:
                            in1, in1_regs = banks[bks[t]][:], [R_bank[bks[t]]]
                        else:
                            in1, in1_regs = t512[tmps[t]][:], [R_t[tmps[t]]]
                        stt(t512[tmps[t]][:], uGb[ub][:, k + t * 512: k + (t + 1) * 512], cv[:, c, CB0 + k:CB0 + k + 1], in1,
                            ALU.mult, ALU.add, [R_uG[ub], R_cv] + in1_regs, [R_t[tmps[t]]])
                for t in range(NTT):
                    act(uB[:, c, t * 512:(t + 1) * 512], t512[tmps[t]][:], AF.Identity, [R_t[tmps[t]], R_cv], [R_uB[c][t]],
                        bias=cv[:, c, CBB:CBB + 1])

            for c in range(8):
                ub = c % 2
                uG = uGb[ub]
                if c % 2 == 0:
                    s = nxt
                    nxt = load_unit(s1_parts(c // 2 + 1) if c < 6 else ypartsA(0))
                if ps == 0:
                    zero_some(3 if c < 7 else 99)
                if c >= 1:
                    dg_build(c - 1)
                if first_half:
                    memset("pool", uG[:, 0:30], 0.0, [R_uG[ub]])
                    memset("pool", vA[:, 0:2], 0.0, [R_vA])
                else:
                    copy("pool", uG[:, 0:30], haloB[:, c, :], [R_haloB[c]], [R_uG[ub]])
                    copy("pool", vA[:, 0:2], haloA[:, c, :], [R_haloA[c]], [R_vA])
                for t in range(NTT):
                    bk = []
                    for j in range(5):
                        b = next_bank()
                        bk.append(b)
                        for k in range(8):
                            mm(banks[b][:], slots[s][:, j * 2048 + k * 256 + ub * 128: j * 2048 + k * 256 + (ub + 1) * 128],
                               hT[:, k, t * 512:(t + 1) * 512], k == 0, k == 7,
                               [R_slot[s]] + hT_reads(t), [R_bank[b]])
                    sl = slice(t * 512, (t + 1) * 512)
                    act(abuf[:, sl], banks[bk[0]][:], AF.Copy, [R_bank[bk[0]]], [R_ab])
                    tx = next_t()
                    act(t512[tx][:], banks[bk[2]][:], AF.Copy, [R_bank[bk[2]]], [R_t[tx]])
                    tt("dve", vA[:, 2 + t * 512: 2 + (t + 1) * 512], banks[bk[1]][:], t512[tx][:], ALU.mult,
                       [R_bank[bk[1]], R_t[tx]], [R_vA])
                    tg = next_t()
                    act(t512[tg][:], banks[bk[4]][:], AF.Sigmoid, [R_bank[bk[4]]], [R_t[tg]])
                    tt("dve", uG[:, 30 + t * 512: 30 + (t + 1) * 512], banks[bk[3]][:], t512[tg][:], ALU.mult,
                       [R_bank[bk[3]], R_t[tg]], [R_uG[ub]])
                if c >= 1:
                    conv_pe(c - 1, 1 - ub)
                a2 = accs[2]
                ts("dve", a2[:], vA[:, 0:PT], cv[:, c, CA0:CA0 + 1], None, ALU.mult, None, [R_vA, R_cv], [R_acc[2]])
                for k in range(1, 3):
                    stt(a2[:], vA[:, k:k + PT], cv[:, c, CA0 + k:CA0 + k + 1], a2[:], ALU.mult, ALU.add,
                        [R_vA, R_cv, R_acc[2]], [R_acc[2]])
                tt("dve", uA[:, c, :], a2[:], abuf[:], ALU.mult, [R_acc[2], R_ab], [R_uA[c][0], R_uA[c][1]])
                if first_half:
                    copy("pool", haloB[:, c, :], uG[:, PT:PT + 30], [R_uG[ub]], [R_haloB[c]])
                    copy("pool", haloA[:, c, :], vA[:, PT:PT + 2], [R_vA], [R_haloA[c]])
            dg_build(7)
            conv_pe(7, 1)

            ln_tm, ln_tr = [None] * NTT, [None] * NTT

            def ln_stats():
                for t in range(NTT):
                    sl = slice(t * 512, (t + 1) * 512)
                    bm = next_bank()
                    bq = next_bank()
                    for c in range(8):
                        q = c % 2
                        act(sqb[q][:], uB[:, c, sl], AF.Square, [R_uB[c][t]], [R_sq[q]])
                        mm(banks[bm][:], onesm[:], uB[:, c, sl], c == 0, c == 7, [R_const, R_uB[c][t]], [R_bank[bm]])
                        mm(banks[bq][:], onesm[:], sqb[q][:], c == 0, c == 7, [R_const, R_sq[q]], [R_bank[bq]])
                    tm, tq, tr = next_t(), next_t(), next_t()
                    t_resv.add(tm)
                    t_resv.add(tr)
                    act(t512[tm][:], banks[bm][:], AF.Copy, [R_bank[bm]], [R_t[tm]])
                    act(t512[tq][:], banks[bm][:], AF.Square, [R_bank[bm]], [R_t[tq]])
                    stt(t512[tr][:], banks[bq][:], EPS, t512[tq][:], ALU.add, ALU.subtract, [R_bank[bq], R_t[tq]], [R_t[tr]])
                    act(t512[tr][:], t512[tr][:], AF.Sqrt, [R_t[tr]], [R_t[tr]])
                    S.op("dve", lambda e, tr=tr: e.reciprocal(out=t512[tr][:], in_=t512[tr][:]), [R_t[tr]], [R_t[tr]])
                    ln_tm[t], ln_tr[t] = tm, tr

            def ln_norm(c):
                for t in range(NTT):
                    sl = slice(t * 512, (t + 1) * 512)
                    tm, tr = ln_tm[t], ln_tr[t]
                    ta = next_t()
                    tt("dve", t512[ta][:], uB[:, c, sl], t512[tm][:], ALU.subtract, [R_uB[c][t], R_t[tm]], [R_t[ta]])
                    tt("dve", t512[ta][:], t512[ta][:], t512[tr][:], ALU.mult, [R_t[ta], R_t[tr]], [R_t[ta]])
                    act(uB[:, c, sl], t512[ta][:], AF.Silu, [R_t[ta], R_cv], [R_uB[c][t]],
                        bias=cv[:, c, LNB:LNB + 1], scale=cv[:, c, LNG:LNG + 1])

            def sweep(c, s, src_, rr_, bcol, first):
                cc = c % 4
                for t in range(NTT):
                    sl = slice(t * 512, (t + 1) * 512)
                    by = next_bank()
                    bg = next_bank()
                    for k in range(8):
                        mm(banks[by][:], slots[s][:, k * 512 + cc * 128: k * 512 + (cc + 1) * 128], src_[:, k, sl], k == 0, k == 7,
                           [R_slot[s], rr_[k][t]], [R_bank[by]])
                    for k in range(8):
                        mm(banks[bg][:], slots[s][:, 4096 + k * 512 + cc * 128: 4096 + k * 512 + (cc + 1) * 128], hT[:, k, sl],
                           k == 0, k == 7, [R_slot[s]] + hT_reads(t), [R_bank[bg]])
                    sg = next_t()
                    act(t512[sg][:], banks[bg][:], AF.Sigmoid, [R_bank[bg], R_cv], [R_t[sg]], bias=cv[:, c, bcol:bcol + 1])
                    if first:
                        tt("dve", mT[:, c, sl], banks[by][:], t512[sg][:], ALU.mult, [R_bank[by], R_t[sg]], [R_m[c][t]])
                    else:
                        t2 = next_t()
                        tt("dve", t512[t2][:], banks[by][:], t512[sg][:], ALU.mult, [R_bank[by], R_t[sg]], [R_t[t2]])
                        tt("pool", mT[:, c, sl], mT[:, c, sl], t512[t2][:], ALU.add, [R_m[c][t], R_t[t2]], [R_m[c][t]])

            for c in range(8):
                if c % 4 == 0:
                    s = nxt
                    nxt = load_unit(ypartsA(1) if c == 0 else ypartsB(0))
                sweep(c, s, uA, R_uA, BGA, True)
                if c == 0:
                    ln_stats()
                else:
                    ln_norm(c - 1)
            ln_norm(7)
            t_resv.clear()
            for c in range(8):
                if c % 4 == 0:
                    s = nxt
                    nxt = load_unit(ypartsB(1) if c == 0 else [(0, 8, 1024, w_o)])
                sweep(c, s, uB, R_uB, BGB, False)

            s = nxt

            def exp_parts(e):
                return [(0, 8, 512, w_eg[e]), (4096, 8, 512, w_eu[e]), (8192, 4, 1024, w_ed[e])]

            ple_parts = [(0, 8, 1024, w_pg), (8192, 2, 1024, w_pp)]
            if ps + 1 < n_pass:
                nxt = load_unit(s1_parts(0))
            else:
                nxt = load_unit(exp_parts(0) if n_exp > 0 else ple_parts)
            for j in range(NJ):
                for h in range(2):
                    b = next_bank()
                    for k in range(8):
                        mm(banks[b][:], mT[:, k, j * 128:(j + 1) * 128], slots[s][:, k * 1024 + h * 512: k * 1024 + (h + 1) * 512],
                           k == 0, k == 7, [R_slot[s], R_m[k][j // 4]], [R_bank[b]])
                    tt("dve", x_tok[:, j, h * 512:(h + 1) * 512], x_tok[:, j, h * 512:(h + 1) * 512], banks[b][:], ALU.add,
                       [R_x[j][h], R_bank[b]], [R_x[j][h]])
            if ps + 1 == n_pass and n_exp == NE:
                load_unit(exp_parts(1), s)

            bL = next_bank()
            resv.add(bL)
            Lps = banks[bL][:].rearrange("p (j c) -> p j c", j=NJ)
            for j in range(NJ):
                xs_toks.append(dma("sp", xs_d[tok0 + j * 128: tok0 + (j + 1) * 128, :], x_tok[:, j, :], [R_x[j][0], R_x[j][1]], [Region()]))
            norm_batch([(x_tok[:, j, :], [R_x[j][0], R_x[j][1]], hT[:, :, j * 128:(j + 1) * 128], [R_hT[j]],
                         uA[:, j, :], [R_uA[j][0], R_uA[j][1]]) for j in range(NJ)], gain_bc=True)
            for j in range(NJ):
                for k in range(8):
                    mm(Lps[:, j, 0:36], hT[:, k, j * 128:(j + 1) * 128], wr[:, k, :], k == 0, False,
                       [R_hT[j], R_const], [R_bank[bL]])
                mm(Lps[:, j, 0:36], ones_row[0:1, :], rb[0:1, :], False, True, [R_const], [R_bank[bL]])
            A0, A1 = accs[0], accs[1]
            rr = [R_acc[0], R_acc[1]]
            rw = rr
            v3 = lambda ap, n: ap.rearrange("p (j c) -> p j c", j=NJ)
            L = v3(A0[:, 0:288], 36)
            sel = v3(A0[:, 288:544], 32)
            mask1 = v3(A0[:, 544:800], 32)
            sm = lambda i: A0[:, 800 + 8 * i: 808 + 8 * i]
            gmax, gsum, pgrp, m1, m2, dd, e2, den, w1, w2, s1f, s2f, ov1, ov2 = (sm(i) for i in range(14))
            gmask = v3(A0[:, 912:944], 4)
            pen = v3(A0[:, 944:976], 4)
            gex = v3(A0[:, 976:1008], 4)
            mask2 = v3(A1[:, 0:256], 32)
            rank = v3(A1[:, 256:512], 32)
            over = v3(A1[:, 512:768], 32)
            sel2 = v3(A1[:, 768:1024], 32)
            tmp = sel2
            bc = lambda ap, n: ap.unsqueeze(2).to_broadcast([128, NJ, n])
            red = lambda out, in_, op: S.op("dve", lambda e: e.tensor_reduce(out=out, in_=in_, axis=AX.X, op=op), rr, rw)
            act(L, Lps[:, :, 0:36], AF.Copy, [R_bank[bL]], rw)
            resv.discard(bL)
            red(gmax, L[:, :, 0:4], ALU.max)
            tt("dve", gmask, L[:, :, 0:4], bc(gmax, 4), ALU.is_ge, rr, rw)
            tt("dve", gex, L[:, :, 0:4], bc(gmax, 4), ALU.subtract, rr, rw)
            act(gex, gex, AF.Exp, rr, rw)
            red(gsum, gex, ALU.add)
            S.op("dve", lambda e: e.reciprocal(out=pgrp, in_=gsum), rr, rw)
            ts("dve", pen, gmask, 1.0, 1e30, ALU.subtract, ALU.mult, rr, rw)
            tt("dve", sel.rearrange("p j (g e) -> p j g e", g=4), L[:, :, 4:36].rearrange("p j (g e) -> p j g e", g=4),
               pen.unsqueeze(3).to_broadcast([128, NJ, 4, 8]), ALU.add, rr, rw)
            red(m1, sel, ALU.max)
            tt("dve", mask1, sel, bc(m1, 32), ALU.is_ge, rr, rw)
            stt(sel2, mask1, -1e30, sel, ALU.mult, ALU.add, rr, rw)
            red(m2, sel2, ALU.max)
            tt("dve", mask2, sel2, bc(m2, 32), ALU.is_ge, rr, rw)
            tt("dve", dd, m2, m1, ALU.subtract, rr, rw)
            act(e2, dd, AF.Exp, rr, rw)
            ts("dve", den, e2, 1.0, None, ALU.add, None, rr, rw)
            S.op("dve", lambda e: e.reciprocal(out=den, in_=den), rr, rw)
            tt("dve", w1, den, pgrp, ALU.mult, rr, rw)
            tt("dve", w2, w1, e2, ALU.mult, rr, rw)
            R_mb = Region()
            tt("dve", maskb[:], mask1, mask2, ALU.add, rr, [R_mb])
            b2 = next_bank()
            Rps = banks[b2][:].rearrange("p (j c) -> p j c", j=NJ)
            for j in range(NJ):
                mm(Rps[:, j, 0:NE], ustr_b[:], maskb[:, j, :], True, j == 0, [R_const, R_mb], [R_bank[b2]])
                for j2 in range(j):
                    mm(Rps[:, j, 0:NE], ones128[:], maskb[:, j2, :], False, j2 == j - 1, [R_const, R_mb], [R_bank[b2]])
            for j2 in range(NJ):
                mm(Rps[:, NJ - 1, NE:2 * NE], ones128[:], maskb[:, j2, :], j2 == 0, j2 == NJ - 1, [R_const, R_mb], [R_bank[b2]])
            tt("dve", rank, Rps[:, :, 0:NE], cnt_bc[:].unsqueeze(1).to_broadcast([128, NJ, NE]), ALU.add, rr + [R_bank[b2], R_cnt], rw)
            tt("dve", cnt_bc[:], cnt_bc[:], Rps[:, NJ - 1, NE:2 * NE], ALU.add, [R_cnt, R_bank[b2]], [R_cnt])
            ts("dve", over, rank, float(CAP), None, ALU.is_ge, None, rr, rw)
            tt("dve", rank, rank, ebase_t[:].unsqueeze(1).to_broadcast([128, NJ, NE]), ALU.add, rr + [R_const], rw)
            stt(rank, over, 1.0e6, rank, ALU.mult, ALU.add, rr, rw)
            for (mk, src_, dst_) in ((mask1, rank, s1f), (mask2, rank, s2f), (mask1, over, ov1), (mask2, over, ov2)):
                tt("dve", tmp, mk, src_, ALU.mult, rr, rw)
                red(dst_, tmp, ALU.add)
            ts("dve", ov1, ov1, -1.0, 1.0, ALU.mult, ALU.add, rr, rw)
            ts("dve", ov2, ov2, -1.0, 1.0, ALU.mult, ALU.add, rr, rw)
            g0 = ps * NJ
            R_sw = [R_slotw[g0 + j] for j in range(NJ)]
            w12v = w12[:, 2 * g0:2 * g0 + 2 * NJ].rearrange("p (j k) -> p j k", k=2)
            siv = slot_i[:, 2 * g0:2 * g0 + 2 * NJ].rearrange("p (j k) -> p j k", k=2)
            tt("dve", w12v[:, :, 0], w1, ov1, ALU.mult, rr, R_sw)
            tt("dve", w12v[:, :, 1], w2, ov2, ALU.mult, rr, R_sw)
            copy("dve", siv[:, :, 0], s1f, rr, R_sw)
            copy("dve", siv[:, :, 1], s2f, rr, R_sw)
            sgv = slot_g[:, 2 * g0:2 * g0 + 2 * NJ].rearrange("p (j k) -> p j k", k=2)
            ts("dve", s1f, s1f, float(NE * CAP - 1), None, ALU.min, None, rr, rw)
            ts("dve", s2f, s2f, float(NE * CAP - 1), None, ALU.min, None, rr, rw)
            copy("dve", sgv[:, :, 0], s1f, rr, R_sw)
            copy("dve", sgv[:, :, 1], s2f, rr, R_sw)
            if ps == 0:
                S.final_wait("pool", zero_toks)
            for j in range(NJ):
                g = g0 + j
                for kk in range(2):
                    idx = slot_i[:, 2 * g + kk:2 * g + kk + 1]
                    scat_toks.append(S.dma("pool", lambda e, idx=idx, j=j: e.indirect_dma_start(
                        out=buf_d, out_offset=bass.IndirectOffsetOnAxis(ap=idx, axis=0), in_=uA[:, j, :], in_offset=None,
                        bounds_check=bcheck(e), oob_is_err=False), [R_uA[j][0], R_uA[j][1], R_slotw[g]], [Region()]))

        barrier()
        hTe = [uA[:, :, 0:CAP], uB[:, :, 0:CAP]]
        actE = [mT[:, 0:4, 0:CAP], mT[:, 4:8, 0:CAP]]
        hbt = [hT[:, i, :] for i in (0, 1, 2, 5, 6, 7)]
        yts = [hT[:, 3 + i, :] for i in range(2)]
        R_hTe = [[Region() for _ in range(NBLK)] for _ in range(2)]
        R_actE = [[Region() for _ in range(4)] for _ in range(2)]
        R_hbt = [Region() for _ in range(6)]
        R_yts = [Region() for _ in range(2)]
        ystore_toks = []
        cnt = {"hbt": 0, "yt": 0}
        def p2_T(ex):
            eb = ex % 2
            for blk in range(NBLK):
                hi = cnt["hbt"] % 6
                cnt["hbt"] += 1
                r0 = ex * CAP + blk * 128
                dma("sp", hbt[hi], buf_d[r0:r0 + 128, :], (), [R_hbt[hi]])
                b = next_bank()
                pT = banks[b][:].bitcast(BF16).rearrange("p (c t) -> p c t", c=8)
                for c in range(8):
                    S.op("pe", lambda e, c=c, pT=pT, hi=hi: e.transpose(out=pT[:, c, :], in_=hbt[hi][:, c * 128:(c + 1) * 128], identity=identb[:]),
                         [R_hbt[hi], R_const], [R_bank[b]])
                if blk % 2 == 0:
                    act(hTe[eb][:, :, blk * 128:(blk + 1) * 128], pT, AF.Copy, [R_bank[b]], [R_hTe[eb][blk]])
                else:
                    copy("dve", hTe[eb][:, :, blk * 128:(blk + 1) * 128], pT, [R_bank[b]], [R_hTe[eb][blk]])

        def p2_GU(ex, s):
            eb = ex % 2
            for q in range(4):
                for cg in range(CAP // CG):
                    cs = slice(cg * CG, (cg + 1) * CG)
                    bg = next_bank()
                    bu = next_bank()
                    for k in range(8):
                        mm(banks[bg][:, 0:CG], slots[s][:, k * 512 + q * 128: k * 512 + (q + 1) * 128], hTe[eb][:, k, cs],
                           k == 0, k == 7, [R_slot[s]] + R_hTe[eb], [R_bank[bg]])
                    for k in range(8):
                        mm(banks[bu][:, 0:CG], slots[s][:, 4096 + k * 512 + q * 128: 4096 + k * 512 + (q + 1) * 128], hTe[eb][:, k, cs],
                           k == 0, k == 7, [R_slot[s]] + R_hTe[eb], [R_bank[bu]])
                    tg = next_t()
                    act(t512[tg][:, 0:CG], banks[bg][:, 0:CG], AF.Silu, [R_bank[bg]], [R_t[tg]])
                    tt("dve", actE[eb][:, q, cs], banks[bu][:, 0:CG], t512[tg][:, 0:CG], ALU.mult, [R_bank[bu], R_t[tg]], [R_actE[eb][q]])

        def p2_DN(ex, s):
            eb = ex % 2
            for blk in range(NBLK):
                yi = cnt["yt"] % 2
                cnt["yt"] += 1
                for h in range(2):
                    b = next_bank()
                    for q in range(4):
                        mm(banks[b][:], actE[eb][:, q, blk * 128:(blk + 1) * 128],
                           slots[s][:, 8192 + q * 1024 + h * 512: 8192 + q * 1024 + (h + 1) * 512],
                           q == 0, q == 3, [R_slot[s], R_actE[eb][q]], [R_bank[b]])
                    if h == 0:
                        act(yts[yi][:, 0:512], banks[b][:], AF.Copy, [R_bank[b]], [R_yts[yi]])
                    else:
                        copy("dve", yts[yi][:, 512:1024], banks[b][:], [R_bank[b]], [R_yts[yi]])
                r0 = ex * CAP + blk * 128
                ystore_toks.append(dma("sp", ybuf_d[r0:r0 + 128, :], yts[yi], [R_yts[yi]], [Region()]))

        slots.append(x_tok[:, 2:8, :].rearrange("p a b -> p (a b)").bitcast(BF16))
        s0 = nxt
        pre1 = (n_exp == NE)
        seq = [s0, 1 - s0, 2] if pre1 else [s0, 2, 1 - s0]
        if n_exp > 0:
            p2_T(0)
        if n_exp > 1 and not pre1:
            load_unit(exp_parts(1), seq[1])
        for ex in range(n_exp):
            s = seq[ex % 3]
            if ex + 2 < n_exp:
                load_unit(exp_parts(ex + 2), seq[(ex + 2) % 3])
            elif pre1 and ex + 1 == n_exp:
                nxt = load_unit(ple_parts, seq[(ex - 1) % 3])
            elif (not pre1) and ex + 2 == n_exp:
                free01 = [q for q in (0, 1) if q not in (seq[ex % 3], seq[(ex + 1) % 3])]
                nxt = load_unit(ple_parts, free01[0])
            p2_GU(ex, s)
            if ex + 1 < n_exp:
                p2_T(ex + 1)
            p2_DN(ex, s)
        if n_exp < 2:
            nxt = load_unit(ple_parts, 1 - s0)

        barrier()
        s = nxt
        dma("sp", gbc[:], fin_g.partition_broadcast(128), (), [R_gbc])
        xt3 = [x_tok[:, i, :] for i in range(7)]
        y3 = [[uB[:, 2 * i, :], uB[:, 2 * i + 1, :]] for i in range(3)]
        ot3 = [accs[0][:], accs[1][:]]
        hT3 = [hT[:, :, i * 128:(i + 1) * 128] for i in range(3)]
        pT3 = [uA[:, 0:2, i * 128:(i + 1) * 128] for i in range(3)]
        R_xt3 = [[Region(), Region()] for _ in range(7)]
        R_y3 = [[Region(), Region()] for _ in range(3)]
        R_ot3 = [R_acc[0], R_acc[1]]
        R_hT3 = [Region(), Region(), Region()]
        R_pT3 = [Region(), Region(), Region()]
        n_tiles = n_pass * NJ

        def p3_s0(g):
            i, i3, iy = g % 2, g % 7, g % 3
            r0 = g * 128
            dma("sp", xt3[i3], xs_d[r0:r0 + 128, :], (), R_xt3[i3])
            dma("sp", ptile[iy][:], p_d[r0:r0 + 128, :], (), [R_pt[iy]])
            for kk in range(2):
                idx = slot_g[:, 2 * g + kk:2 * g + kk + 1]
                S.dma("pool", lambda e, idx=idx, dst=y3[iy][kk]: e.indirect_dma_start(
                    out=dst, out_offset=None, in_=ybuf_d, in_offset=bass.IndirectOffsetOnAxis(ap=idx, axis=0),
                    bounds_check=bcheck(e), oob_is_err=False), [R_slotw[g]], [R_y3[iy][kk]])

        junk3 = mT[:, 7, :]
        R_junk3 = [Region()]

        def p3_s1a(g):
            i, i3, iy = g % 2, g % 7, g % 3
            for kk in range(2):
                for h in range(2):
                    hs = slice(h * 512, (h + 1) * 512)
                    stt(xt3[i3][:, hs], y3[iy][kk][:, hs], w12[:, 2 * g + kk:2 * g + kk + 1], xt3[i3][:, hs], ALU.mult, ALU.add,
                        [R_y3[iy][kk], R_slotw[g], R_xt3[i3][h]], [R_xt3[i3][h]])
            si = rstd_of(xt3[i3], R_xt3[i3], junk3, R_junk3)
            ts("dve", hn[i][:], xt3[i3], stat[:, si:si + 1], None, ALU.mult, None, R_xt3[i3] + [R_stat[si]], [R_hn[i]])
            copy("pool", ptb[i][:], ptile[iy][:], [R_pt[iy]], [R_ptb[i]])

        def p3_s1b(g):
            i, i3, iy = g % 2, g % 7, g % 3
            b = next_bank()
            pT = banks[b][:].bitcast(BF16).rearrange("p (c t) -> p c t", c=8)
            for c in range(8):
                S.op("pe", lambda e, c=c: e.transpose(out=pT[:, c, :], in_=hn[i][:, c * 128:(c + 1) * 128], identity=identb[:]),
                     [R_hn[i], R_const], [R_bank[b]])
            b2 = next_bank()
            pT2 = banks[b2][:].bitcast(BF16).rearrange("p (c t) -> p c t", c=8)
            for c in range(2):
                S.op("pe", lambda e, c=c: e.transpose(out=pT2[:, c, :], in_=ptb[i][:, c * 128:(c + 1) * 128], identity=identb[:]),
                     [R_ptb[i], R_const], [R_bank[b2]])
            tt("dve", hT3[iy], pT, cv[:, :, GPLE:GPLE + 1].to_broadcast([128, 8, 128]), ALU.mult, [R_bank[b], R_cv], [R_hT3[iy]])
            act(pT3[iy], pT2[:, 0:2, :], AF.Copy, [R_bank[b2]], [R_pT3[iy]])

        def p3_s2(g):
            i, i3, iy = g % 2, g % 7, g % 3
            for h in range(2):
                hs = slice(h * 512, (h + 1) * 512)
                bg = next_bank()
                bp = next_bank()
                for k in range(8):
                    mm(banks[bg][:], hT3[iy][:, k, :], slots[s][:, k * 1024 + h * 512: k * 1024 + (h + 1) * 512],
                       k == 0, k == 7, [R_slot[s], R_hT3[iy]], [R_bank[bg]])
                for k in range(2):
                    mm(banks[bp][:], pT3[iy][:, k, :], slots[s][:, 8192 + k * 1024 + h * 512: 8192 + k * 1024 + (h + 1) * 512],
                       k == 0, k == 1, [R_slot[s], R_pT3[iy]], [R_bank[bp]])
                tg, tp = next_t(), next_t()
                act(t512[tg][:], banks[bg][:], AF.Sigmoid, [R_bank[bg]], [R_t[tg]])
                tt("dve", t512[tp][:], banks[bp][:], t512[tg][:], ALU.mult, [R_bank[bp], R_t[tg]], [R_t[tp]])
                tt("pool", xt3[i3][:, hs], xt3[i3][:, hs], t512[tp][:], ALU.add, [R_xt3[i3][h], R_t[tp]], [R_xt3[i3][h]])

        def p3_s3(g):
            i, i3, iy = g % 2, g % 7, g % 3
            r0 = g * 128
            si = rstd_of(xt3[i3], R_xt3[i3], junk3, R_junk3)
            stt(ot3[i], xt3[i3], stat[:, si:si + 1], gbc[:], ALU.mult, ALU.mult, R_xt3[i3] + [R_stat[si], R_gbc], [R_ot3[i]])
            out_toks.append(dma("sp", out_d[r0:r0 + 128, :], ot3[i], [R_ot3[i]], [Region()]))

        p3_s0(0)
        if n_tiles > 1:
            p3_s0(1)
        for step in range(n_tiles + 4):
            if 0 <= step - 4 < n_tiles:
                p3_s3(step - 4)
            if 0 <= step - 2 < n_tiles:
                p3_s2(step - 2)
            if 0 <= step - 1 < n_tiles:
                p3_s1b(step - 1)
            if step + 2 < n_tiles:
                p3_s0(step + 2)
            if step < n_tiles:
                p3_s1a(step)

        S.final_wait("sp", out_toks)
        S.emit()
    return nc


_NC_CACHE = {}


def make_in_maps(inputs, ncores=NCORES):
    f = lambda a: np.ascontiguousarray(np.asarray(a, dtype=np.float32))
    x = f(inputs["x"]).reshape(NCORES, TOK_CORE, D)
    p = f(inputs["p"]).reshape(NCORES, TOK_CORE, PLE)
    shared = {"ident": np.eye(128, dtype=np.float32),
              "ustrict": np.triu(np.ones((128, 128), dtype=np.float32), k=1),
              "ebase": np.ascontiguousarray(np.broadcast_to((np.arange(NE, dtype=np.float32) * CAP)[None, :], (128, NE)))}
    for name in ("mix_norm_g", "w_in", "conv_a_w", "w_out_a", "conv_b_w", "conv_b_b", "ln_b_g", "ln_b_b", "w_out_b",
                 "b_gate", "w_o", "ffn_norm_g", "w_router_group", "b_router_group", "w_router_expert", "b_router_expert",
                 "w_exp_gate", "w_exp_up", "w_exp_down", "ple_norm_g", "w_ple_gate", "w_ple_proj"):
        a = f(inputs[name])
        shared[name] = np.ascontiguousarray(a.reshape(a.shape[1:]))
    shared["final_norm_g"] = f(inputs["final_norm_g"])
    in_maps = []
    for c in range(ncores):
        m = dict(shared)
        m["x"] = x[c]
        m["p"] = p[c]
        in_maps.append(m)
    return in_maps


def kernel(**inputs):
    in_maps = make_in_maps(inputs)
    if "nc" not in _NC_CACHE:
        _NC_CACHE["nc"] = build_nc()
    nc = _NC_CACHE["nc"]
    res = run_bass_kernel_spmd(nc, in_maps, core_ids=list(range(NCORES)))
    out = np.stack([np.asarray(r["out"]) for r in res.results], axis=0)
    return out.reshape(16, SEQ, D).astype(np.float32)
```

```python
import numpy as np
import concourse.bass as bass
import concourse.mybir as mybir
from concourse.bass_utils import run_bass_kernel_spmd
from contextlib import ExitStack

F32 = mybir.dt.float32
BF16 = mybir.dt.bfloat16
ALU = mybir.AluOpType
AF = mybir.ActivationFunctionType
AX = mybir.AxisListType

NCORES = 8
D = 1024
SEQ = 2048
TOK_CORE = 4096
PT = 1024
NPASS = TOK_CORE // PT
NJ = PT // 128
NTT = PT // 512
NE = 32
DE = 512
PLE = 256
EPS = 1e-6
NV = 42
NT5 = 8
CAP = 512
CG = 512
NBLK = CAP // 128
NG = TOK_CORE // 128
ENG_KEYS = ("pe", "act", "dve", "pool", "sp")
STRICT_SYNC = [True]


class Region:
    __slots__ = ("w", "rs", "name")

    def __init__(self, name=""):
        self.w = None
        self.rs = []
        self.name = name


class Sched:
    def __init__(self, nc, n_dma_sems=24):
        self.nc = nc
        self.ops = {k: [] for k in ENG_KEYS}
        self.count = {k: 0 for k in ENG_KEYS}
        self.seen = {k: {} for k in ENG_KEYS}
        self.n_dma_sems = n_dma_sems
        self.ring = n_dma_sems // 2
        self.dma_k = {"sw": 0, "hw": 0}
        self.needed = set()

    def _deps(self, reads, writes):
        deps = set()
        for r in reads:
            if r.w is not None:
                deps.add(r.w)
        for w in writes:
            if w.w is not None:
                deps.add(w.w)
            for t in w.rs:
                deps.add(t)
        return deps

    def _filter(self, eng, deps, raw, is_dma=False):
        waits = []
        seen = self.seen[eng]
        for t in sorted(deps):
            kind, key, val = t
            if kind == "e" and key == eng and not is_dma and not (STRICT_SYNC[0] and eng != "pe"):
                if eng == "pe" or t not in raw:
                    continue
            sk = (kind, key)
            if seen.get(sk, 0) >= val:
                continue
            seen[sk] = val
            waits.append(t)
            self.needed.add(t)
        return waits

    def _finish(self, tok, reads, writes):
        for r in reads:
            r.rs.append(tok)
            if len(r.rs) > 64:
                best = {}
                for t in r.rs:
                    k = (t[0], t[1])
                    if k not in best or best[k][2] < t[2]:
                        best[k] = t
                r.rs = list(best.values())
        for w in writes:
            w.w = tok
            w.rs = []

    def op(self, eng, fn, reads=(), writes=()):
        deps = self._deps(reads, writes)
        raw = set(r.w for r in reads if r.w is not None)
        waits = self._filter(eng, deps, raw)
        self.count[eng] += 1
        tok = ("e", eng, self.count[eng])
        self.ops[eng].append((waits, fn, tok))
        self._finish(tok, reads, writes)
        return tok

    def dma(self, eng, fn, reads=(), writes=()):
        deps = self._deps(reads, writes)
        rk = "sw" if eng == "pool" else "hw"
        k = self.dma_k[rk]
        self.dma_k[rk] += 1
        s = k % self.ring + (0 if rk == "sw" else self.ring)
        val = 16 * (k // self.ring + 1)
        if val > 16:
            deps.add(("d", s, val - 16))
        raw = set(r.w for r in reads if r.w is not None)
        waits = self._filter(eng, deps, raw, is_dma=True)
        tok = ("d", s, val)
        self.ops[eng].append((waits, fn, tok))
        self._finish(tok, reads, writes)
        return tok

    def final_wait(self, eng, toks):
        waits = self._filter(eng, set(toks), set())
        self.ops[eng].append((waits, None, None))

    def emit(self):
        nc = self.nc
        rank = {}
        for k in ENG_KEYS:
            idxs = sorted(v for (kind, key, v) in self.needed if kind == "e" and key == k)
            for i, v in enumerate(idxs):
                rank[(k, v)] = i + 1
        with ExitStack() as es:
            esem = {k: es.enter_context(nc.semaphore("s_" + k)) for k in ENG_KEYS}
            dsem = [es.enter_context(nc.semaphore("d_%d" % i)) for i in range(self.n_dma_sems)]
            block = es.enter_context(nc.Block())

            def run(eng_key):
                def body(e):
                    for waits, fn, tok in self.ops[eng_key]:
                        for (kind, key, val) in waits:
                            if kind == "e":
                                e.wait_ge(esem[key], rank[(key, val)])
                            else:
                                e.wait_ge(dsem[key], val)
                        if fn is None:
                            continue
                        ins = fn(e)
                        if tok[0] == "d":
                            ins.then_inc(dsem[tok[1]], 16)
                        elif tok in self.needed:
                            ins.then_inc(esem[eng_key], 1)
                return body

            block.tensor(run("pe"))
            block.scalar(run("act"))
            block.vector(run("dve"))
            block.gpsimd(run("pool"))
            block.sync(run("sp"))


def build_nc(n_pass=NPASS, n_exp=NE):
    nc = bass.Bass("TRN2", target_bir_lowering=False)

    def din(name, shape):
        return nc.dram_tensor(name, list(shape), F32, kind="ExternalInput").ap()

    x_d = din("x", [TOK_CORE, D])
    p_d = din("p", [TOK_CORE, PLE])
    ident_d = din("ident", [128, 128])
    ustr_d = din("ustrict", [128, 128])
    ebase_d = din("ebase", [128, NE])
    mix_g = din("mix_norm_g", [D])
    w_in = din("w_in", [D, 7168])
    conv_a_w = din("conv_a_w", [3, D])
    w_out_a = din("w_out_a", [D, D])
    conv_b_w = din("conv_b_w", [31, D])
    conv_b_b = din("conv_b_b", [D])
    ln_b_g = din("ln_b_g", [D])
    ln_b_b = din("ln_b_b", [D])
    w_out_b = din("w_out_b", [D, D])
    b_gate = din("b_gate", [2 * D])
    w_o = din("w_o", [D, D])
    ffn_g = din("ffn_norm_g", [D])
    w_rg = din("w_router_group", [D, 4])
    b_rg = din("b_router_group", [4])
    w_re = din("w_router_expert", [D, NE])
    b_re = din("b_router_expert", [NE])
    w_eg = din("w_exp_gate", [NE, D, DE])
    w_eu = din("w_exp_up", [NE, D, DE])
    w_ed = din("w_exp_down", [NE, DE, D])
    ple_g = din("ple_norm_g", [D])
    w_pg = din("w_ple_gate", [D, D])
    w_pp = din("w_ple_proj", [PLE, D])
    fin_g = din("final_norm_g", [D])
    out_d = nc.dram_tensor("out", [TOK_CORE, D], F32, kind="ExternalOutput").ap()
    xs_d = nc.dram_tensor("xs_scratch", [TOK_CORE, D], F32).ap()
    buf_d = nc.dram_tensor("buf_scratch", [NE * CAP, D], BF16).ap()
    ybuf_d = nc.dram_tensor("ybuf_scratch", [NE * CAP, D], BF16).ap()

    es = ExitStack()
    with es:
        def sb(name, shape, dt):
            return es.enter_context(nc.sbuf_tensor(name, list(shape), dt))

        S = Sched(nc)

        x_tok = sb("x_tok", [128, NJ, D], F32)
        hT = sb("hT", [128, 8, PT], BF16)
        uA = sb("uA", [128, 8, PT], BF16)
        uB = sb("uB", [128, 8, PT], BF16)
        mT = sb("mT", [128, 8, PT], BF16)
        slots = [sb("wslot%d" % i, [128, 12288], BF16) for i in range(2)]
        haloA = sb("haloA", [128, 8, 2], F32)
        haloB = sb("haloB", [128, 8, 30], BF16)
        identf = sb("identf", [128, 128], F32)
        identb = sb("identb", [128, 128], BF16)
        onesm = sb("onesm", [128, 128], BF16)
        ones_row = sb("ones_row", [1, 128], BF16)
        epsc = sb("epsc", [128, 1], F32)
        cv = sb("cv", [128, 8, NV], F32)
        gbc = sb("gbc", [128, D], F32)
        wr = sb("wr", [128, 8, 36], BF16)
        rb = sb("rb", [1, 36], BF16)
        ustr_b = sb("ustr_b", [128, 128], BF16)
        ones128 = sb("ones128", [128, 128], BF16)
        ebase_t = sb("ebase_t", [128, NE], F32)
        cnt_bc = sb("cnt_bc", [128, NE], F32)
        slot_i = sb("slot_i", [128, 2 * NG], mybir.dt.int32)
        slot_g = sb("slot_g", [128, 2 * NG], mybir.dt.int32)
        w12 = sb("w12", [128, 2 * NG], F32)
        maskb = sb("maskb", [128, NJ, NE], BF16)
        hn = [sb("hn%d" % i, [128, D], BF16) for i in range(2)]
        stat = sb("stat", [128, 64], F32)
        uGb = [sb("uGb%d" % i, [128, 30 + PT], BF16) for i in range(2)]
        vA = sb("vA", [128, 2 + PT], F32)
        abuf = sb("abuf", [128, PT], F32)
        vrows = abuf
        accs = [sb("acc%d" % i, [128, PT], F32) for i in range(3)]
        t512 = [sb("t512_%d" % i, [128, 512], F32) for i in range(NT5)]
        sqb = [sb("sqb%d" % i, [128, 512], BF16) for i in range(2)]
        ptile = [sb("ptile%d" % i, [128, PLE], F32) for i in range(3)]
        ptb = [sb("ptb%d" % i, [128, PLE], BF16) for i in range(2)]
        rt = sb("rt", [128, 192], F32)

        banks = [es.enter_context(nc.psum_tensor("bank%d" % i, [128, 512], F32)) for i in range(8)]

        R_x = [[Region() for _ in range(2)] for _ in range(NJ)]
        R_hT = [Region() for _ in range(NJ)]
        R_uA = [[Region() for _ in range(NTT)] for _ in range(8)]
        R_uB = [[Region() for _ in range(NTT)] for _ in range(8)]
        R_m = [[Region() for _ in range(NTT)] for _ in range(8)]
        R_slot = [Region(), Region(), Region()]
        R_haloA = [Region() for _ in range(8)]
        R_haloB = [Region() for _ in range(8)]
        R_const = Region()
        R_cv = Region()
        R_cnt = Region()
        R_slotw = [Region() for _ in range(NG)]
        R_gbc = Region()
        R_hn = [Region(), Region()]
        R_stat = [Region() for _ in range(64)]
        R_uG = [Region(), Region()]
        R_vA = Region()
        R_ab = Region()
        R_vrows = R_ab
        R_acc = [Region() for _ in range(3)]
        R_t = [Region() for _ in range(NT5)]
        R_sq = [Region(), Region()]
        R_pt = [Region(), Region(), Region()]
        R_ptb = [Region(), Region()]
        R_rt = Region()
        R_bank = [Region() for _ in range(8)]
        out_toks = []
        state = {"bank": 0, "slot": 0, "t": 0, "stat": 0, "hn": 0}

        resv = set()

        def next_bank():
            while True:
                b = state["bank"]
                state["bank"] = (b + 1) % 8
                if b not in resv:
                    return b

        t_resv = set()

        def next_t():
            while True:
                t = state["t"]
                state["t"] = (t + 1) % NT5
                if t not in t_resv:
                    return t

        def next_stat():
            t = state["stat"]
            state["stat"] = (t + 1) % 64
            return t

        def mm(out, lhsT, rhs, start, stop, reads, writes):
            S.op("pe", lambda e: e.matmul(out=out, lhsT=lhsT, rhs=rhs, start=start, stop=stop), reads, writes)

        def act(out, in_, func, reads, writes, bias=None, scale=None, accum_out=None):
            kw = {}
            if bias is not None:
                kw["bias"] = bias
            if scale is not None:
                kw["scale"] = scale
            if accum_out is not None:
                kw["accum_out"] = accum_out
            S.op("act", lambda e: e.activation(out=out, in_=in_, func=func, **kw), reads, writes)

        def tt(eng, out, in0, in1, op, reads, writes):
            S.op(eng, lambda e: e.tensor_tensor(out=out, in0=in0, in1=in1, op=op), reads, writes)

        def ts(eng, out, in0, s1, s2, op0, op1, reads, writes):
            if s2 is None:
                S.op(eng, lambda e: e.tensor_scalar(out=out, in0=in0, scalar1=s1, scalar2=None, op0=op0), reads, writes)
            else:
                S.op(eng, lambda e: e.tensor_scalar(out=out, in0=in0, scalar1=s1, scalar2=s2, op0=op0, op1=op1), reads, writes)

        def stt(out, in0, scalar, in1, op0, op1, reads, writes):
            S.op("dve", lambda e: e.scalar_tensor_tensor(out=out, in0=in0, scalar=scalar, in1=in1, op0=op0, op1=op1), reads, writes)

        def copy(eng, out, in_, reads, writes):
            S.op(eng, lambda e: e.tensor_copy(out=out, in_=in_), reads, writes)

        def memset(eng, ap, val, writes):
            S.op(eng, lambda e: e.memset(ap, val), (), writes)

        def dma(eng, out, in_, reads, writes, slow=False):
            if slow:
                return S.dma(eng, lambda e: e.dma_start(out=out, in_=in_, allow_slow_non_contiguous=True), reads, writes)
            return S.dma(eng, lambda e: e.dma_start(out=out, in_=in_), reads, writes)

        for j in range(NJ):
            dma("sp", x_tok[:, j, :], x_d[j * 128:(j + 1) * 128, :], (), [R_x[j][0], R_x[j][1]])

        dma("act", identf[:], ident_d, (), [R_const])
        copy("dve", identb[:], identf[:], [R_const], [R_const])
        memset("pool", onesm[:], 1.0 / 1024.0, [R_const])
        memset("pool", ones_row[:], 1.0, [R_const])
        memset("pool", epsc[:], EPS, [R_const])
        dma("act", vrows[0:31, :], conv_b_w, (), [R_vrows])
        dma("act", vrows[31:34, :], conv_a_w, (), [R_vrows])
        for r, v in ((34, conv_b_b), (35, ln_b_g), (36, ln_b_b), (39, mix_g), (40, ffn_g), (41, ple_g)):
            dma("act", vrows[r:r + 1, :], v.rearrange("(o d) -> o d", o=1), (), [R_vrows])
        dma("act", vrows[37:39, :], b_gate.rearrange("(o d) -> o d", o=2), (), [R_vrows])
        dma("act", gbc[:], ffn_g.partition_broadcast(128), (), [R_gbc])
        dma("act", accs[0][:, 0:128], ustr_d, (), [R_acc[0]])
        copy("dve", ustr_b[:], accs[0][:, 0:128], [R_acc[0]], [R_const])
        dma("act", ebase_t[:], ebase_d, (), [R_const])
        memset("pool", ones128[:], 1.0, [R_const])
        memset("pool", cnt_bc[:], 0.0, [R_cnt])
        for c in range(8):
            b = next_bank()
            mm(banks[b][:, 0:NV], vrows[0:NV, c * 128:(c + 1) * 128], identf[0:NV, 0:NV], True, True,
               [R_vrows, R_const], [R_bank[b]])
            copy("dve", cv[:, c, :], banks[b][:, 0:NV], [R_bank[b]], [R_cv])
        CB0, CA0, CBB, LNG, LNB, BGA, BGB, GMIX, GFFN, GPLE = 0, 31, 34, 35, 36, 37, 38, 39, 40, 41

        def load_unit(parts, s=None):
            if s is None:
                s = state["slot"]
                state["slot"] = 1 - s
            for (off, k, n, src) in parts:
                dst = slots[s][:, off:off + k * n].rearrange("p (k n) -> p k n", k=k)
                dma("pool", dst, src.rearrange("(k p) n -> p k n", p=128), (), [R_slot[s]])
            return s

        def rstd_of(xap, xregs, junk_ap=None, junk_regs=None):
            si = next_stat()
            hb = state["hn"]
            if junk_ap is None:
                junk_ap, junk_regs = hn[hb][:], [R_hn[hb]]
            act(junk_ap, xap, AF.Square, xregs + [R_stat[si]], junk_regs + [R_stat[si]], accum_out=stat[:, si:si + 1])
            act(stat[:, si:si + 1], stat[:, si:si + 1], AF.Sqrt, [R_stat[si], R_const], [R_stat[si]], bias=epsc[:, 0:1], scale=1.0 / D)
            S.op("dve", lambda e: e.reciprocal(out=stat[:, si:si + 1], in_=stat[:, si:si + 1]), [R_stat[si]], [R_stat[si]])
            return si

        def norm_batch(tiles, gcol=None, gain_bc=False):
            sis = []
            for (xap, xregs, dst, dregs, hn_ap, hn_regs) in tiles:
                si = next_stat()
                sis.append(si)
                act(hn[0][:], xap, AF.Square, xregs + [R_stat[si]], [R_hn[0], R_stat[si]], accum_out=stat[:, si:si + 1])
            for si in sis:
                act(stat[:, si:si + 1], stat[:, si:si + 1], AF.Sqrt, [R_stat[si], R_const], [R_stat[si]], bias=epsc[:, 0:1], scale=1.0 / D)
            for si in sis:
                S.op("dve", lambda e, si=si: e.reciprocal(out=stat[:, si:si + 1], in_=stat[:, si:si + 1]), [R_stat[si]], [R_stat[si]])
            for si, (xap, xregs, dst, dregs, hn_ap, hn_regs) in zip(sis, tiles):
                if gain_bc:
                    stt(hn_ap, xap, stat[:, si:si + 1], gbc[:], ALU.mult, ALU.mult, xregs + [R_stat[si], R_gbc], hn_regs)
                else:
                    ts("dve", hn_ap, xap, stat[:, si:si + 1], None, ALU.mult, None, xregs + [R_stat[si]], hn_regs)
            pts = []

            def evac(i_):
                (b, pT) = pts[i_]
                (xap, xregs, dst, dregs, hn_ap, hn_regs) = tiles[i_]
                if gcol is not None:
                    tt("dve", dst, pT, cv[:, :, gcol:gcol + 1].to_broadcast([128, 8, 128]), ALU.mult, [R_bank[b], R_cv], dregs)
                elif i_ % 2 == 0:
                    act(dst, pT, AF.Copy, [R_bank[b]], dregs)
                else:
                    copy("dve", dst, pT, [R_bank[b]], dregs)

            for i_, (xap, xregs, dst, dregs, hn_ap, hn_regs) in enumerate(tiles):
                b = next_bank()
                pT = banks[b][:].bitcast(BF16).rearrange("p (c t) -> p c t", c=8)
                pts.append((b, pT))
                for c in range(8):
                    S.op("pe", lambda e, c=c, pT=pT, hn_ap=hn_ap: e.transpose(out=pT[:, c, :], in_=hn_ap[:, c * 128:(c + 1) * 128], identity=identb[:]),
                         hn_regs + [R_const], [R_bank[b]])
                if i_ >= 2:
                    evac(i_ - 2)
            for i_ in range(max(0, len(tiles) - 2), len(tiles)):
                evac(i_)

        def norm_T(xap, xregs, dst, dregs, gcol=None, gain_bc=False, hn_ap=None, hn_regs=None):
            si = rstd_of(xap, xregs)
            hb = state["hn"]
            state["hn"] = 1 - hb
            if hn_ap is None:
                hn_ap, hn_regs = hn[hb][:], [R_hn[hb]]
            if gain_bc:
                stt(hn_ap, xap, stat[:, si:si + 1], gbc[:], ALU.mult, ALU.mult, xregs + [R_stat[si], R_gbc], hn_regs)
            else:
                ts("dve", hn_ap, xap, stat[:, si:si + 1], None, ALU.mult, None, xregs + [R_stat[si]], hn_regs)
            b = next_bank()
            pT = banks[b][:].bitcast(BF16).rearrange("p (c t) -> p c t", c=8)
            for c in range(8):
                S.op("pe", lambda e, c=c: e.transpose(out=pT[:, c, :], in_=hn_ap[:, c * 128:(c + 1) * 128], identity=identb[:]),
                     hn_regs + [R_const], [R_bank[b]])
            if gcol is not None:
                tt("dve", dst, pT, cv[:, :, gcol:gcol + 1].to_broadcast([128, 8, 128]), ALU.mult, [R_bank[b], R_cv], dregs)
            else:
                act(dst, pT, AF.Copy, [R_bank[b]], dregs)
            return hb

        def barrier():
            toks = [("e", k, S.count[k]) for k in ("pe", "act", "dve", "pool") if S.count[k] > 0]
            for rk, base in (("sw", 0), ("hw", S.ring)):
                nk = S.dma_k[rk]
                for s_ in range(min(nk, S.ring)):
                    k_last = ((nk - 1 - s_) // S.ring) * S.ring + s_
                    toks.append(("d", base + s_, 16 * (k_last // S.ring + 1)))
            for eng in ENG_KEYS:
                S.final_wait(eng, toks)

        def hT_reads(t):
            return [R_hT[4 * t + i] for i in range(4)]

        zero_toks = []
        _breg = {}

        def bcheck(e):
            if "r" not in _breg:
                _breg["r"] = e.to_reg(NE * CAP - 1)
            return _breg["r"]

        scat_toks, xs_toks = [], []
        nxt = None
        for ps in range(n_pass):
            tok0 = ps * PT
            first_half = (ps % 2 == 0)
            for j in range(NJ):
                if ps > 0:
                    dma("sp", x_tok[:, j, :], x_d[tok0 + j * 128: tok0 + (j + 1) * 128, :], (), [R_x[j][0], R_x[j][1]])

            def s1_parts(g_):
                return [(j * 2048, 8, 256, w_in[:, j * 1024 + g_ * 256: j * 1024 + (g_ + 1) * 256]) for j in range(5)]

            def ypartsA(g_):
                return [(0, 8, 512, w_out_a[:, g_ * 512:(g_ + 1) * 512]),
                        (4096, 8, 512, w_in[:, 5120 + g_ * 512: 5120 + (g_ + 1) * 512])]

            def ypartsB(g_):
                return [(0, 8, 512, w_out_b[:, g_ * 512:(g_ + 1) * 512]),
                        (4096, 8, 512, w_in[:, 6144 + g_ * 512: 6144 + (g_ + 1) * 512])]

            if nxt is None:
                nxt = load_unit(s1_parts(0))
            if ps == 0:
                dma("pool", wr[:, :, 0:4], w_rg.rearrange("(k p) n -> p k n", p=128), (), [R_const])
                dma("pool", wr[:, :, 4:36], w_re.rearrange("(k p) n -> p k n", p=128), (), [R_const])
                dma("pool", rb[0:1, 0:4], b_rg.rearrange("(o d) -> o d", o=1), (), [R_const])
                dma("pool", rb[0:1, 4:36], b_re.rearrange("(o d) -> o d", o=1), (), [R_const])
            norm_batch([(x_tok[:, j, :], [R_x[j][0], R_x[j][1]], hT[:, :, j * 128:(j + 1) * 128], [R_hT[j]],
                         mT[:, j, :], [R_m[j][0], R_m[j][1]]) for j in range(NJ)], gcol=GMIX)

            if ps == 0:
                for c in range(8):
                    memset("pool", mT[:, c, :], 0.0, [R_m[c][0], R_m[c][1]])
                buf_v = buf_d.rearrange("(n p) d -> p n d", p=128)
                zero_pending = list(range(NE * CAP // 128 // 8))

                def zero_some(n_):
                    for _ in range(n_):
                        if zero_pending:
                            i = zero_pending.pop(0)
                            zero_toks.append(dma("act", buf_v[:, 8 * i:8 * i + 8, :], mT[:],
                                                 [R_m[c_][t_] for c_ in range(8) for t_ in range(NTT)], [Region()]))


            dgA = accs[0][:].bitcast(BF16).rearrange("p (k m) -> p k m", m=128)
            dgB = accs[1][:].bitcast(BF16).rearrange("p (k m) -> p k m", m=128)[:, 0:15, :]

            KPE = 23

            def dg_build(c):
                tt("dve", dgA, identb[:].unsqueeze(1).to_broadcast([128, 16, 128]),
                   cv[:, c, CB0:CB0 + 16].unsqueeze(2).to_broadcast([128, 16, 128]), ALU.mult, [R_const, R_cv], [R_acc[0]])
                tt("dve", dgB[:, 0:KPE - 16, :], identb[:].unsqueeze(1).to_broadcast([128, KPE - 16, 128]),
                   cv[:, c, CB0 + 16:CB0 + KPE].unsqueeze(2).to_broadcast([128, KPE - 16, 128]), ALU.mult, [R_const, R_cv], [R_acc[1]])

            def conv_pe(c, ub):
                bks = []
                for t in range(NTT):
                    b = next_bank()
                    bks.append(b)
                    for k in range(KPE):
                        lhsT = dgA[:, k, :] if k < 16 else dgB[:, k - 16, :]
                        mm(banks[b][:], lhsT, uGb[ub][:, k + t * 512: k + (t + 1) * 512], k == 0, k == KPE - 1,
                           [R_acc[0], R_acc[1], R_uG[ub]], [R_bank[b]])
                tmps = [next_t() for _ in range(NTT)]
                for idx, k in enumerate(range(KPE, 31)):
                    for t in range(NTT):
                        if idx == 0:
                            in1, in1_regs = banks[bks[t]][:], [R_bank[bks[t]]]
                        else:
                            in1, in1_regs = t512[tmps[t]][:], [R_t[tmps[t]]]
                        stt(t512[tmps[t]][:], uGb[ub][:, k + t * 512: k + (t + 1) * 512], cv[:, c, CB0 + k:CB0 + k + 1], in1,
                            ALU.mult, ALU.add, [R_uG[ub], R_cv] + in1_regs, [R_t[tmps[t]]])
                for t in range(NTT):
                    act(uB[:, c, t * 512:(t + 1) * 512], t512[tmps[t]][:], AF.Identity, [R_t[tmps[t]], R_cv], [R_uB[c][t]],
                        bias=cv[:, c, CBB:CBB + 1])

            for c in range(8):
                ub = c % 2
                uG = uGb[ub]
                if c % 2 == 0:
                    s = nxt
                    nxt = load_unit(s1_parts(c // 2 + 1) if c < 6 else ypartsA(0))
                if ps == 0:
                    zero_some(3 if c < 7 else 99)
                if c >= 1:
                    dg_build(c - 1)
                if first_half:
                    memset("pool", uG[:, 0:30], 0.0, [R_uG[ub]])
                    memset("pool", vA[:, 0:2], 0.0, [R_vA])
                else:
                    copy("pool", uG[:, 0:30], haloB[:, c, :], [R_haloB[c]], [R_uG[ub]])
                    copy("pool", vA[:, 0:2], haloA[:, c, :], [R_haloA[c]], [R_vA])
                for t in range(NTT):
                    bk = []
                    for j in range(5):
                        b = next_bank()
                        bk.append(b)
                        for k in range(8):
                            mm(banks[b][:], slots[s][:, j * 2048 + k * 256 + ub * 128: j * 2048 + k * 256 + (ub + 1) * 128],
                               hT[:, k, t * 512:(t + 1) * 512], k == 0, k == 7,
                               [R_slot[s]] + hT_reads(t), [R_bank[b]])
                    sl = slice(t * 512, (t + 1) * 512)
                    act(abuf[:, sl], banks[bk[0]][:], AF.Copy, [R_bank[bk[0]]], [R_ab])
                    tx = next_t()
                    act(t512[tx][:], banks[bk[2]][:], AF.Copy, [R_bank[bk[2]]], [R_t[tx]])
                    tt("dve", vA[:, 2 + t * 512: 2 + (t + 1) * 512], banks[bk[1]][:], t512[tx][:], ALU.mult,
                       [R_bank[bk[1]], R_t[tx]], [R_vA])
                    tg = next_t()
                    act(t512[tg][:], banks[bk[4]][:], AF.Sigmoid, [R_bank[bk[4]]], [R_t[tg]])
                    tt("dve", uG[:, 30 + t * 512: 30 + (t + 1) * 512], banks[bk[3]][:], t512[tg][:], ALU.mult,
                       [R_bank[bk[3]], R_t[tg]], [R_uG[ub]])
                if c >= 1:
                    conv_pe(c - 1, 1 - ub)
                a2 = accs[2]
                ts("dve", a2[:], vA[:, 0:PT], cv[:, c, CA0:CA0 + 1], None, ALU.mult, None, [R_vA, R_cv], [R_acc[2]])
                for k in range(1, 3):
                    stt(a2[:], vA[:, k:k + PT], cv[:, c, CA0 + k:CA0 + k + 1], a2[:], ALU.mult, ALU.add,
                        [R_vA, R_cv, R_acc[2]], [R_acc[2]])
                tt("dve", uA[:, c, :], a2[:], abuf[:], ALU.mult, [R_acc[2], R_ab], [R_uA[c][0], R_uA[c][1]])
                if first_half:
                    copy("pool", haloB[:, c, :], uG[:, PT:PT + 30], [R_uG[ub]], [R_haloB[c]])
                    copy("pool", haloA[:, c, :], vA[:, PT:PT + 2], [R_vA], [R_haloA[c]])
            dg_build(7)
            conv_pe(7, 1)

            ln_tm, ln_tr = [None] * NTT, [None] * NTT

            def ln_stats():
                for t in range(NTT):
                    sl = slice(t * 512, (t + 1) * 512)
                    bm = next_bank()
                    bq = next_bank()
                    for c in range(8):
                        q = c % 2
                        act(sqb[q][:], uB[:, c, sl], AF.Square, [R_uB[c][t]], [R_sq[q]])
                        mm(banks[bm][:], onesm[:], uB[:, c, sl], c == 0, c == 7, [R_const, R_uB[c][t]], [R_bank[bm]])
                        mm(banks[bq][:], onesm[:], sqb[q][:], c == 0, c == 7, [R_const, R_sq[q]], [R_bank[bq]])
                    tm, tq, tr = next_t(), next_t(), next_t()
                    t_resv.add(tm)
                    t_resv.add(tr)
                    act(t512[tm][:], banks[bm][:], AF.Copy, [R_bank[bm]], [R_t[tm]])
                    act(t512[tq][:], banks[bm][:], AF.Square, [R_bank[bm]], [R_t[tq]])
                    stt(t512[tr][:], banks[bq][:], EPS, t512[tq][:], ALU.add, ALU.subtract, [R_bank[bq], R_t[tq]], [R_t[tr]])
                    act(t512[tr][:], t512[tr][:], AF.Sqrt, [R_t[tr]], [R_t[tr]])
                    S.op("dve", lambda e, tr=tr: e.reciprocal(out=t512[tr][:], in_=t512[tr][:]), [R_t[tr]], [R_t[tr]])
                    ln_tm[t], ln_tr[t] = tm, tr

            def ln_norm(c):
                for t in range(NTT):
                    sl = slice(t * 512, (t + 1) * 512)
                    tm, tr = ln_tm[t], ln_tr[t]
                    ta = next_t()
                    tt("dve", t512[ta][:], uB[:, c, sl], t512[tm][:], ALU.subtract, [R_uB[c][t], R_t[tm]], [R_t[ta]])
                    tt("dve", t512[ta][:], t512[ta][:], t512[tr][:], ALU.mult, [R_t[ta], R_t[tr]], [R_t[ta]])
                    act(uB[:, c, sl], t512[ta][:], AF.Silu, [R_t[ta], R_cv], [R_uB[c][t]],
                        bias=cv[:, c, LNB:LNB + 1], scale=cv[:, c, LNG:LNG + 1])

            def sweep(c, s, src_, rr_, bcol, first):
                cc = c % 4
                for t in range(NTT):
                    sl = slice(t * 512, (t + 1) * 512)
                    by = next_bank()
                    bg = next_bank()
                    for k in range(8):
                        mm(banks[by][:], slots[s][:, k * 512 + cc * 128: k * 512 + (cc + 1) * 128], src_[:, k, sl], k == 0, k == 7,
                           [R_slot[s], rr_[k][t]], [R_bank[by]])
                    for k in range(8):
                        mm(banks[bg][:], slots[s][:, 4096 + k * 512 + cc * 128: 4096 + k * 512 + (cc + 1) * 128], hT[:, k, sl],
                           k == 0, k == 7, [R_slot[s]] + hT_reads(t), [R_bank[bg]])
                    sg = next_t()
                    act(t512[sg][:], banks[bg][:], AF.Sigmoid, [R_bank[bg], R_cv], [R_t[sg]], bias=cv[:, c, bcol:bcol + 1])
                    if first:
                        tt("dve", mT[:, c, sl], banks[by][:], t512[sg][:], ALU.mult, [R_bank[by], R_t[sg]], [R_m[c][t]])
                    else:
                        t2 = next_t()
                        tt("dve", t512[t2][:], banks[by][:], t512[sg][:], ALU.mult, [R_bank[by], R_t[sg]], [R_t[t2]])
                        tt("pool", mT[:, c, sl], mT[:, c, sl], t512[t2][:], ALU.add, [R_m[c][t], R_t[t2]], [R_m[c][t]])

            for c in range(8):
                if c % 4 == 0:
                    s = nxt
                    nxt = load_unit(ypartsA(1) if c == 0 else ypartsB(0))
                sweep(c, s, uA, R_uA, BGA, True)
                if c == 0:
                    ln_stats()
                else:
                    ln_norm(c - 1)
            ln_norm(7)
            t_resv.clear()
            for c in range(8):
                if c % 4 == 0:
                    s = nxt
                    nxt = load_unit(ypartsB(1) if c == 0 else [(0, 8, 1024, w_o)])
                sweep(c, s, uB, R_uB, BGB, False)

            s = nxt

            def exp_parts(e):
                return [(0, 8, 512, w_eg[e]), (4096, 8, 512, w_eu[e]), (8192, 4, 1024, w_ed[e])]

            ple_parts = [(0, 8, 1024, w_pg), (8192, 2, 1024, w_pp)]
            if ps + 1 < n_pass:
                nxt = load_unit(s1_parts(0))
            else:
                nxt = load_unit(exp_parts(0) if n_exp > 0 else ple_parts)
            for j in range(NJ):
                for h in range(2):
                    b = next_bank()
                    for k in range(8):
                        mm(banks[b][:], mT[:, k, j * 128:(j + 1) * 128], slots[s][:, k * 1024 + h * 512: k * 1024 + (h + 1) * 512],
                           k == 0, k == 7, [R_slot[s], R_m[k][j // 4]], [R_bank[b]])
                    tt("dve", x_tok[:, j, h * 512:(h + 1) * 512], x_tok[:, j, h * 512:(h + 1) * 512], banks[b][:], ALU.add,
                       [R_x[j][h], R_bank[b]], [R_x[j][h]])

            bL = next_bank()
            resv.add(bL)
            Lps = banks[bL][:].rearrange("p (j c) -> p j c", j=NJ)
            for j in range(NJ):
                xs_toks.append(dma("sp", xs_d[tok0 + j * 128: tok0 + (j + 1) * 128, :], x_tok[:, j, :], [R_x[j][0], R_x[j][1]], [Region()]))
            norm_batch([(x_tok[:, j, :], [R_x[j][0], R_x[j][1]], hT[:, :, j * 128:(j + 1) * 128], [R_hT[j]],
                         uA[:, j, :], [R_uA[j][0], R_uA[j][1]]) for j in range(NJ)], gain_bc=True)
            for j in range(NJ):
                for k in range(8):
                    mm(Lps[:, j, 0:36], hT[:, k, j * 128:(j + 1) * 128], wr[:, k, :], k == 0, False,
                       [R_hT[j], R_const], [R_bank[bL]])
                mm(Lps[:, j, 0:36], ones_row[0:1, :], rb[0:1, :], False, True, [R_const], [R_bank[bL]])
            A0, A1 = accs[0], accs[1]
            rr = [R_acc[0], R_acc[1]]
            rw = rr
            v3 = lambda ap, n: ap.rearrange("p (j c) -> p j c", j=NJ)
            L = v3(A0[:, 0:288], 36)
            sel = v3(A0[:, 288:544], 32)
            mask1 = v3(A0[:, 544:800], 32)
            sm = lambda i: A0[:, 800 + 8 * i: 808 + 8 * i]
            gmax, gsum, pgrp, m1, m2, dd, e2, den, w1, w2, s1f, s2f, ov1, ov2 = (sm(i) for i in range(14))
            gmask = v3(A0[:, 912:944], 4)
            pen = v3(A0[:, 944:976], 4)
            gex = v3(A0[:, 976:1008], 4)
            mask2 = v3(A1[:, 0:256], 32)
            rank = v3(A1[:, 256:512], 32)
            over = v3(A1[:, 512:768], 32)
            sel2 = v3(A1[:, 768:1024], 32)
            tmp = sel2
            bc = lambda ap, n: ap.unsqueeze(2).to_broadcast([128, NJ, n])
            red = lambda out, in_, op: S.op("dve", lambda e: e.tensor_reduce(out=out, in_=in_, axis=AX.X, op=op), rr, rw)
            act(L, Lps[:, :, 0:36], AF.Copy, [R_bank[bL]], rw)
            resv.discard(bL)
            red(gmax, L[:, :, 0:4], ALU.max)
            tt("dve", gmask, L[:, :, 0:4], bc(gmax, 4), ALU.is_ge, rr, rw)
            tt("dve", gex, L[:, :, 0:4], bc(gmax, 4), ALU.subtract, rr, rw)
            act(gex, gex, AF.Exp, rr, rw)
            red(gsum, gex, ALU.add)
            S.op("dve", lambda e: e.reciprocal(out=pgrp, in_=gsum), rr, rw)
            ts("dve", pen, gmask, 1.0, 1e30, ALU.subtract, ALU.mult, rr, rw)
            tt("dve", sel.rearrange("p j (g e) -> p j g e", g=4), L[:, :, 4:36].rearrange("p j (g e) -> p j g e", g=4),
               pen.unsqueeze(3).to_broadcast([128, NJ, 4, 8]), ALU.add, rr, rw)
            red(m1, sel, ALU.max)
            tt("dve", mask1, sel, bc(m1, 32), ALU.is_ge, rr, rw)
            stt(sel2, mask1, -1e30, sel, ALU.mult, ALU.add, rr, rw)
            red(m2, sel2, ALU.max)
            tt("dve", mask2, sel2, bc(m2, 32), ALU.is_ge, rr, rw)
            tt("dve", dd, m2, m1, ALU.subtract, rr, rw)
            act(e2, dd, AF.Exp, rr, rw)
            ts("dve", den, e2, 1.0, None, ALU.add, None, rr, rw)
            S.op("dve", lambda e: e.reciprocal(out=den, in_=den), rr, rw)
            tt("dve", w1, den, pgrp, ALU.mult, rr, rw)
            tt("dve", w2, w1, e2, ALU.mult, rr, rw)
            R_mb = Region()
            tt("dve", maskb[:], mask1, mask2, ALU.add, rr, [R_mb])
            b2 = next_bank()
            Rps = banks[b2][:].rearrange("p (j c) -> p j c", j=NJ)
            for j in range(NJ):
                mm(Rps[:, j, 0:NE], ustr_b[:], maskb[:, j, :], True, j == 0, [R_const, R_mb], [R_bank[b2]])
                for j2 in range(j):
                    mm(Rps[:, j, 0:NE], ones128[:], maskb[:, j2, :], False, j2 == j - 1, [R_const, R_mb], [R_bank[b2]])
            for j2 in range(NJ):
                mm(Rps[:, NJ - 1, NE:2 * NE], ones128[:], maskb[:, j2, :], j2 == 0, j2 == NJ - 1, [R_const, R_mb], [R_bank[b2]])
            tt("dve", rank, Rps[:, :, 0:NE], cnt_bc[:].unsqueeze(1).to_broadcast([128, NJ, NE]), ALU.add, rr + [R_bank[b2], R_cnt], rw)
            tt("dve", cnt_bc[:], cnt_bc[:], Rps[:, NJ - 1, NE:2 * NE], ALU.add, [R_cnt, R_bank[b2]], [R_cnt])
            ts("dve", over, rank, float(CAP), None, ALU.is_ge, None, rr, rw)
            tt("dve", rank, rank, ebase_t[:].unsqueeze(1).to_broadcast([128, NJ, NE]), ALU.add, rr + [R_const], rw)
            stt(rank, over, 1.0e6, rank, ALU.mult, ALU.add, rr, rw)
            for (mk, src_, dst_) in ((mask1, rank, s1f), (mask2, rank, s2f), (mask1, over, ov1), (mask2, over, ov2)):
                tt("dve", tmp, mk, src_, ALU.mult, rr, rw)
                red(dst_, tmp, ALU.add)
            ts("dve", ov1, ov1, -1.0, 1.0, ALU.mult, ALU.add, rr, rw)
            ts("dve", ov2, ov2, -1.0, 1.0, ALU.mult, ALU.add, rr, rw)
            g0 = ps * NJ
            R_sw = [R_slotw[g0 + j] for j in range(NJ)]
            w12v = w12[:, 2 * g0:2 * g0 + 2 * NJ].rearrange("p (j k) -> p j k", k=2)
            siv = slot_i[:, 2 * g0:2 * g0 + 2 * NJ].rearrange("p (j k) -> p j k", k=2)
            tt("dve", w12v[:, :, 0], w1, ov1, ALU.mult, rr, R_sw)
            tt("dve", w12v[:, :, 1], w2, ov2, ALU.mult, rr, R_sw)
            copy("dve", siv[:, :, 0], s1f, rr, R_sw)
            copy("dve", siv[:, :, 1], s2f, rr, R_sw)
            sgv = slot_g[:, 2 * g0:2 * g0 + 2 * NJ].rearrange("p (j k) -> p j k", k=2)
            ts("dve", s1f, s1f, float(NE * CAP - 1), None, ALU.min, None, rr, rw)
            ts("dve", s2f, s2f, float(NE * CAP - 1), None, ALU.min, None, rr, rw)
            copy("dve", sgv[:, :, 0], s1f, rr, R_sw)
            copy("dve", sgv[:, :, 1], s2f, rr, R_sw)
            if ps == 0:
                S.final_wait("pool", zero_toks)
            for j in range(NJ):
                g = g0 + j
                for kk in range(2):
                    idx = slot_i[:, 2 * g + kk:2 * g + kk + 1]
                    scat_toks.append(S.dma("pool", lambda e, idx=idx, j=j: e.indirect_dma_start(
                        out=buf_d, out_offset=bass.IndirectOffsetOnAxis(ap=idx, axis=0), in_=uA[:, j, :], in_offset=None,
                        bounds_check=bcheck(e), oob_is_err=False), [R_uA[j][0], R_uA[j][1], R_slotw[g]], [Region()]))

        barrier()
        hTe = [uA[:, :, 0:CAP], uB[:, :, 0:CAP]]
        actE = [mT[:, 0:4, 0:CAP], mT[:, 4:8, 0:CAP]]
        hbt = [hT[:, i, :] for i in (0, 1, 2, 5, 6, 7)]
        yts = [hT[:, 3 + i, :] for i in range(2)]
        R_hTe = [[Region() for _ in range(NBLK)] for _ in range(2)]
        R_actE = [[Region() for _ in range(4)] for _ in range(2)]
        R_hbt = [Region() for _ in range(6)]
        R_yts = [Region() for _ in range(2)]
        ystore_toks = []
        cnt = {"hbt": 0, "yt": 0}
        def p2_T(ex):
            eb = ex % 2
            for blk in range(NBLK):
                hi = cnt["hbt"] % 6
                cnt["hbt"] += 1
                r0 = ex * CAP + blk * 128
                dma("sp", hbt[hi], buf_d[r0:r0 + 128, :], (), [R_hbt[hi]])
                b = next_bank()
                pT = banks[b][:].bitcast(BF16).rearrange("p (c t) -> p c t", c=8)
                for c in range(8):
                    S.op("pe", lambda e, c=c, pT=pT, hi=hi: e.transpose(out=pT[:, c, :], in_=hbt[hi][:, c * 128:(c + 1) * 128], identity=identb[:]),
                         [R_hbt[hi], R_const], [R_bank[b]])
                if blk % 2 == 0:
                    act(hTe[eb][:, :, blk * 128:(blk + 1) * 128], pT, AF.Copy, [R_bank[b]], [R_hTe[eb][blk]])
                else:
                    copy("dve", hTe[eb][:, :, blk * 128:(blk + 1) * 128], pT, [R_bank[b]], [R_hTe[eb][blk]])

        def p2_GU(ex, s):
            eb = ex % 2
            for q in range(4):
                for cg in range(CAP // CG):
                    cs = slice(cg * CG, (cg + 1) * CG)
                    bg = next_bank()
                    bu = next_bank()
                    for k in range(8):
                        mm(banks[bg][:, 0:CG], slots[s][:, k * 512 + q * 128: k * 512 + (q + 1) * 128], hTe[eb][:, k, cs],
                           k == 0, k == 7, [R_slot[s]] + R_hTe[eb], [R_bank[bg]])
                    for k in range(8):
                        mm(banks[bu][:, 0:CG], slots[s][:, 4096 + k * 512 + q * 128: 4096 + k * 512 + (q + 1) * 128], hTe[eb][:, k, cs],
                           k == 0, k == 7, [R_slot[s]] + R_hTe[eb], [R_bank[bu]])
                    tg = next_t()
                    act(t512[tg][:, 0:CG], banks[bg][:, 0:CG], AF.Silu, [R_bank[bg]], [R_t[tg]])
                    tt("dve", actE[eb][:, q, cs], banks[bu][:, 0:CG], t512[tg][:, 0:CG], ALU.mult, [R_bank[bu], R_t[tg]], [R_actE[eb][q]])

        def p2_DN(ex, s):
            eb = ex % 2
            for blk in range(NBLK):
                yi = cnt["yt"] % 2
                cnt["yt"] += 1
                for h in range(2):
                    b = next_bank()
                    for q in range(4):
                        mm(banks[b][:], actE[eb][:, q, blk * 128:(blk + 1) * 128],
                           slots[s][:, 8192 + q * 1024 + h * 512: 8192 + q * 1024 + (h + 1) * 512],
                           q == 0, q == 3, [R_slot[s], R_actE[eb][q]], [R_bank[b]])
                    if h == 0:
                        act(yts[yi][:, 0:512], banks[b][:], AF.Copy, [R_bank[b]], [R_yts[yi]])
                    else:
                        copy("dve", yts[yi][:, 512:1024], banks[b][:], [R_bank[b]], [R_yts[yi]])
                r0 = ex * CAP + blk * 128
                ystore_toks.append(dma("sp", ybuf_d[r0:r0 + 128, :], yts[yi], [R_yts[yi]], [Region()]))

        slots.append(x_tok[:, 2:8, :].rearrange("p a b -> p (a b)").bitcast(BF16))
        s0 = nxt
        seq = [s0, 2, 1 - s0]
        if n_exp > 0:
            p2_T(0)
        if n_exp > 1:
            load_unit(exp_parts(1), seq[1])
        for ex in range(n_exp):
            s = seq[ex % 3]
            if ex + 2 < n_exp:
                load_unit(exp_parts(ex + 2), seq[(ex + 2) % 3])
            elif ex + 2 == n_exp:
                free01 = [q for q in (0, 1) if q not in (seq[ex % 3], seq[(ex + 1) % 3])]
                nxt = load_unit(ple_parts, free01[0])
            p2_GU(ex, s)
            if ex + 1 < n_exp:
                p2_T(ex + 1)
            p2_DN(ex, s)
        if n_exp < 2:
            nxt = load_unit(ple_parts, 1 - s0)

        barrier()
        s = nxt
        dma("sp", gbc[:], fin_g.partition_broadcast(128), (), [R_gbc])
        xt3 = [x_tok[:, i, :] for i in range(7)]
        y3 = [[uB[:, 2 * i, :], uB[:, 2 * i + 1, :]] for i in range(3)]
        ot3 = [accs[0][:], accs[1][:]]
        hT3 = [hT[:, :, i * 128:(i + 1) * 128] for i in range(3)]
        pT3 = [uA[:, 0:2, i * 128:(i + 1) * 128] for i in range(3)]
        R_xt3 = [[Region(), Region()] for _ in range(7)]
        R_y3 = [[Region(), Region()] for _ in range(3)]
        R_ot3 = [R_acc[0], R_acc[1]]
        R_hT3 = [Region(), Region(), Region()]
        R_pT3 = [Region(), Region(), Region()]
        n_tiles = n_pass * NJ

        def p3_s0(g):
            i, i3, iy = g % 2, g % 7, g % 3
            r0 = g * 128
            dma("sp", xt3[i3], xs_d[r0:r0 + 128, :], (), R_xt3[i3])
            dma("sp", ptile[iy][:], p_d[r0:r0 + 128, :], (), [R_pt[iy]])
            for kk in range(2):
                idx = slot_g[:, 2 * g + kk:2 * g + kk + 1]
                S.dma("pool", lambda e, idx=idx, dst=y3[iy][kk]: e.indirect_dma_start(
                    out=dst, out_offset=None, in_=ybuf_d, in_offset=bass.IndirectOffsetOnAxis(ap=idx, axis=0),
                    bounds_check=bcheck(e), oob_is_err=False), [R_slotw[g]], [R_y3[iy][kk]])

        junk3 = mT[:, 7, :]
        R_junk3 = [Region()]

        def p3_s1a(g):
            i, i3, iy = g % 2, g % 7, g % 3
            for kk in range(2):
                for h in range(2):
                    hs = slice(h * 512, (h + 1) * 512)
                    stt(xt3[i3][:, hs], y3[iy][kk][:, hs], w12[:, 2 * g + kk:2 * g + kk + 1], xt3[i3][:, hs], ALU.mult, ALU.add,
                        [R_y3[iy][kk], R_slotw[g], R_xt3[i3][h]], [R_xt3[i3][h]])
            si = rstd_of(xt3[i3], R_xt3[i3], junk3, R_junk3)
            ts("dve", hn[i][:], xt3[i3], stat[:, si:si + 1], None, ALU.mult, None, R_xt3[i3] + [R_stat[si]], [R_hn[i]])
            copy("pool", ptb[i][:], ptile[iy][:], [R_pt[iy]], [R_ptb[i]])

        def p3_s1b(g):
            i, i3, iy = g % 2, g % 7, g % 3
            b = next_bank()
            pT = banks[b][:].bitcast(BF16).rearrange("p (c t) -> p c t", c=8)
            for c in range(8):
                S.op("pe", lambda e, c=c: e.transpose(out=pT[:, c, :], in_=hn[i][:, c * 128:(c + 1) * 128], identity=identb[:]),
                     [R_hn[i], R_const], [R_bank[b]])
            b2 = next_bank()
            pT2 = banks[b2][:].bitcast(BF16).rearrange("p (c t) -> p c t", c=8)
            for c in range(2):
                S.op("pe", lambda e, c=c: e.transpose(out=pT2[:, c, :], in_=ptb[i][:, c * 128:(c + 1) * 128], identity=identb[:]),
                     [R_ptb[i], R_const], [R_bank[b2]])
            tt("dve", hT3[iy], pT, cv[:, :, GPLE:GPLE + 1].to_broadcast([128, 8, 128]), ALU.mult, [R_bank[b], R_cv], [R_hT3[iy]])
            act(pT3[iy], pT2[:, 0:2, :], AF.Copy, [R_bank[b2]], [R_pT3[iy]])

        def p3_s2(g):
            i, i3, iy = g % 2, g % 7, g % 3
            for h in range(2):
                hs = slice(h * 512, (h + 1) * 512)
                bg = next_bank()
                bp = next_bank()
                for k in range(8):
                    mm(banks[bg][:], hT3[iy][:, k, :], slots[s][:, k * 1024 + h * 512: k * 1024 + (h + 1) * 512],
                       k == 0, k == 7, [R_slot[s], R_hT3[iy]], [R_bank[bg]])
                for k in range(2):
                    mm(banks[bp][:], pT3[iy][:, k, :], slots[s][:, 8192 + k * 1024 + h * 512: 8192 + k * 1024 + (h + 1) * 512],
                       k == 0, k == 1, [R_slot[s], R_pT3[iy]], [R_bank[bp]])
                tg, tp = next_t(), next_t()
                act(t512[tg][:], banks[bg][:], AF.Sigmoid, [R_bank[bg]], [R_t[tg]])
                tt("dve", t512[tp][:], banks[bp][:], t512[tg][:], ALU.mult, [R_bank[bp], R_t[tg]], [R_t[tp]])
                tt("pool", xt3[i3][:, hs], xt3[i3][:, hs], t512[tp][:], ALU.add, [R_xt3[i3][h], R_t[tp]], [R_xt3[i3][h]])

        def p3_s3(g):
            i, i3, iy = g % 2, g % 7, g % 3
            r0 = g * 128
            si = rstd_of(xt3[i3], R_xt3[i3], junk3, R_junk3)
            stt(ot3[i], xt3[i3], stat[:, si:si + 1], gbc[:], ALU.mult, ALU.mult, R_xt3[i3] + [R_stat[si], R_gbc], [R_ot3[i]])
            out_toks.append(dma("sp", out_d[r0:r0 + 128, :], ot3[i], [R_ot3[i]], [Region()]))

        p3_s0(0)
        if n_tiles > 1:
            p3_s0(1)
        for step in range(n_tiles + 4):
            if 0 <= step - 4 < n_tiles:
                p3_s3(step - 4)
            if 0 <= step - 2 < n_tiles:
                p3_s2(step - 2)
            if 0 <= step - 1 < n_tiles:
                p3_s1b(step - 1)
            if step + 2 < n_tiles:
                p3_s0(step + 2)
            if step < n_tiles:
                p3_s1a(step)

        S.final_wait("sp", out_toks)
        S.emit()
    return nc


_NC_CACHE = {}


def make_in_maps(inputs, ncores=NCORES):
    f = lambda a: np.ascontiguousarray(np.asarray(a, dtype=np.float32))
    x = f(inputs["x"]).reshape(NCORES, TOK_CORE, D)
    p = f(inputs["p"]).reshape(NCORES, TOK_CORE, PLE)
    shared = {"ident": np.eye(128, dtype=np.float32),
              "ustrict": np.triu(np.ones((128, 128), dtype=np.float32), k=1),
              "ebase": np.ascontiguousarray(np.broadcast_to((np.arange(NE, dtype=np.float32) * CAP)[None, :], (128, NE)))}
    for name in ("mix_norm_g", "w_in", "conv_a_w", "w_out_a", "conv_b_w", "conv_b_b", "ln_b_g", "ln_b_b", "w_out_b",
                 "b_gate", "w_o", "ffn_norm_g", "w_router_group", "b_router_group", "w_router_expert", "b_router_expert",
                 "w_exp_gate", "w_exp_up", "w_exp_down", "ple_norm_g", "w_ple_gate", "w_ple_proj"):
        a = f(inputs[name])
        shared[name] = np.ascontiguousarray(a.reshape(a.shape[1:]))
    shared["final_norm_g"] = f(inputs["final_norm_g"])
    in_maps = []
    for c in range(ncores):
        m = dict(shared)
        m["x"] = x[c]
        m["p"] = p[c]
        in_maps.append(m)
    return in_maps


def kernel(**inputs):
    in_maps = make_in_maps(inputs)
    if "nc" not in _NC_CACHE:
        _NC_CACHE["nc"] = build_nc()
    nc = _NC_CACHE["nc"]
    res = run_bass_kernel_spmd(nc, in_maps, core_ids=list(range(NCORES)))
    out = np.stack([np.asarray(r["out"]) for r in res.results], axis=0)
    return out.reshape(16, SEQ, D).astype(np.float32)
```

```python
import numpy as np
import concourse.bass as bass
import concourse.mybir as mybir
from concourse.bass_utils import run_bass_kernel_spmd
from contextlib import ExitStack

F32 = mybir.dt.float32
BF16 = mybir.dt.bfloat16
ALU = mybir.AluOpType
AF = mybir.ActivationFunctionType
AX = mybir.AxisListType

NCORES = 8
D = 1024
SEQ = 2048
TOK_CORE = 4096
PT = 1024
NPASS = TOK_CORE // PT
NJ = PT // 128
NTT = PT // 512
NE = 32
DE = 512
PLE = 256
EPS = 1e-6
NV = 42
NT5 = 8
CAP = 512
CG = 512
NBLK = CAP // 128
NG = TOK_CORE // 128
ENG_KEYS = ("pe", "act", "dve", "pool", "sp")
STRICT_SYNC = [True]


class Region:
    __slots__ = ("w", "rs", "name")

    def __init__(self, name=""):
        self.w = None
        self.rs = []
        self.name = name


class Sched:
    def __init__(self, nc, n_dma_sems=24):
        self.nc = nc
        self.ops = {k: [] for k in ENG_KEYS}
        self.count = {k: 0 for k in ENG_KEYS}
        self.seen = {k: {} for k in ENG_KEYS}
        self.n_dma_sems = n_dma_sems
        self.ring = n_dma_sems // 2
        self.dma_k = {"sw": 0, "hw": 0}
        self.needed = set()

    def _deps(self, reads, writes):
        deps = set()
        for r in reads:
            if r.w is not None:
                deps.add(r.w)
        for w in writes:
            if w.w is not None:
                deps.add(w.w)
            for t in w.rs:
                deps.add(t)
        return deps

    def _filter(self, eng, deps, raw, is_dma=False):
        waits = []
        seen = self.seen[eng]
        for t in sorted(deps):
            kind, key, val = t
            if kind == "e" and key == eng and not is_dma and not (STRICT_SYNC[0] and eng != "pe"):
                if eng == "pe" or t not in raw:
                    continue
            sk = (kind, key)
            if seen.get(sk, 0) >= val:
                continue
            seen[sk] = val
            waits.append(t)
            self.needed.add(t)
        return waits

    def _finish(self, tok, reads, writes):
        for r in reads:
            r.rs.append(tok)
            if len(r.rs) > 64:
                best = {}
                for t in r.rs:
                    k = (t[0], t[1])
                    if k not in best or best[k][2] < t[2]:
                        best[k] = t
                r.rs = list(best.values())
        for w in writes:
            w.w = tok
            w.rs = []

    def op(self, eng, fn, reads=(), writes=()):
        deps = self._deps(reads, writes)
        raw = set(r.w for r in reads if r.w is not None)
        waits = self._filter(eng, deps, raw)
        self.count[eng] += 1
        tok = ("e", eng, self.count[eng])
        self.ops[eng].append((waits, fn, tok))
        self._finish(tok, reads, writes)
        return tok

    def dma(self, eng, fn, reads=(), writes=()):
        deps = self._deps(reads, writes)
        rk = "sw" if eng == "pool" else "hw"
        k = self.dma_k[rk]
        self.dma_k[rk] += 1
        s = k % self.ring + (0 if rk == "sw" else self.ring)
        val = 16 * (k // self.ring + 1)
        if val > 16:
            deps.add(("d", s, val - 16))
        raw = set(r.w for r in reads if r.w is not None)
        waits = self._filter(eng, deps, raw, is_dma=True)
        tok = ("d", s, val)
        self.ops[eng].append((waits, fn, tok))
        self._finish(tok, reads, writes)
        return tok

    def final_wait(self, eng, toks):
        waits = self._filter(eng, set(toks), set())
        self.ops[eng].append((waits, None, None))

    def emit(self):
        nc = self.nc
        rank = {}
        for k in ENG_KEYS:
            idxs = sorted(v for (kind, key, v) in self.needed if kind == "e" and key == k)
            for i, v in enumerate(idxs):
                rank[(k, v)] = i + 1
        with ExitStack() as es:
            esem = {k: es.enter_context(nc.semaphore("s_" + k)) for k in ENG_KEYS}
            dsem = [es.enter_context(nc.semaphore("d_%d" % i)) for i in range(self.n_dma_sems)]
            block = es.enter_context(nc.Block())

            def run(eng_key):
                def body(e):
                    for waits, fn, tok in self.ops[eng_key]:
                        for (kind, key, val) in waits:
                            if kind == "e":
                                e.wait_ge(esem[key], rank[(key, val)])
                            else:
                                e.wait_ge(dsem[key], val)
                        if fn is None:
                            continue
                        ins = fn(e)
                        if tok[0] == "d":
                            ins.then_inc(dsem[tok[1]], 16)
                        elif tok in self.needed:
                            ins.then_inc(esem[eng_key], 1)
                return body

            block.tensor(run("pe"))
            block.scalar(run("act"))
            block.vector(run("dve"))
            block.gpsimd(run("pool"))
            block.sync(run("sp"))


def build_nc(n_pass=NPASS, n_exp=NE):
    nc = bass.Bass("TRN2", target_bir_lowering=False)

    def din(name, shape):
        return nc.dram_tensor(name, list(shape), F32, kind="ExternalInput").ap()

    x_d = din("x", [TOK_CORE, D])
    p_d = din("p", [TOK_CORE, PLE])
    ident_d = din("ident", [128, 128])
    ustr_d = din("ustrict", [128, 128])
    ebase_d = din("ebase", [128, NE])
    mix_g = din("mix_norm_g", [D])
    w_in = din("w_in", [D, 7168])
    conv_a_w = din("conv_a_w", [3, D])
    w_out_a = din("w_out_a", [D, D])
    conv_b_w = din("conv_b_w", [31, D])
    conv_b_b = din("conv_b_b", [D])
    ln_b_g = din("ln_b_g", [D])
    ln_b_b = din("ln_b_b", [D])
    w_out_b = din("w_out_b", [D, D])
    b_gate = din("b_gate", [2 * D])
    w_o = din("w_o", [D, D])
    ffn_g = din("ffn_norm_g", [D])
    w_rg = din("w_router_group", [D, 4])
    b_rg = din("b_router_group", [4])
    w_re = din("w_router_expert", [D, NE])
    b_re = din("b_router_expert", [NE])
    w_eg = din("w_exp_gate", [NE, D, DE])
    w_eu = din("w_exp_up", [NE, D, DE])
    w_ed = din("w_exp_down", [NE, DE, D])
    ple_g = din("ple_norm_g", [D])
    w_pg = din("w_ple_gate", [D, D])
    w_pp = din("w_ple_proj", [PLE, D])
    fin_g = din("final_norm_g", [D])
    out_d = nc.dram_tensor("out", [TOK_CORE, D], F32, kind="ExternalOutput").ap()
    xs_d = nc.dram_tensor("xs_scratch", [TOK_CORE, D], F32).ap()
    buf_d = nc.dram_tensor("buf_scratch", [NE * CAP, D], BF16).ap()
    ybuf_d = nc.dram_tensor("ybuf_scratch", [NE * CAP, D], BF16).ap()

    es = ExitStack()
    with es:
        def sb(name, shape, dt):
            return es.enter_context(nc.sbuf_tensor(name, list(shape), dt))

        S = Sched(nc)

        x_tok = sb("x_tok", [128, NJ, D], F32)
        hT = sb("hT", [128, 8, PT], BF16)
        uA = sb("uA", [128, 8, PT], BF16)
        uB = sb("uB", [128, 8, PT], BF16)
        mT = sb("mT", [128, 8, PT], BF16)
        slots = [sb("wslot%d" % i, [128, 12288], BF16) for i in range(2)]
        haloA = sb("haloA", [128, 8, 2], F32)
        haloB = sb("haloB", [128, 8, 30], BF16)
        identf = sb("identf", [128, 128], F32)
        identb = sb("identb", [128, 128], BF16)
        onesm = sb("onesm", [128, 128], BF16)
        ones_row = sb("ones_row", [1, 128], BF16)
        epsc = sb("epsc", [128, 1], F32)
        cv = sb("cv", [128, 8, NV], F32)
        gbc = sb("gbc", [128, D], F32)
        wr = sb("wr", [128, 8, 36], BF16)
        rb = sb("rb", [1, 36], BF16)
        ustr_b = sb("ustr_b", [128, 128], BF16)
        ones128 = sb("ones128", [128, 128], BF16)
        ebase_t = sb("ebase_t", [128, NE], F32)
        cnt_bc = sb("cnt_bc", [128, NE], F32)
        slot_i = sb("slot_i", [128, 2 * NG], mybir.dt.int32)
        slot_g = sb("slot_g", [128, 2 * NG], mybir.dt.int32)
        w12 = sb("w12", [128, 2 * NG], F32)
        maskb = sb("maskb", [128, NJ, NE], BF16)
        hn = [sb("hn%d" % i, [128, D], BF16) for i in range(2)]
        stat = sb("stat", [128, 64], F32)
        uGb = [sb("uGb%d" % i, [128, 30 + PT], BF16) for i in range(2)]
        vA = sb("vA", [128, 2 + PT], F32)
        abuf = sb("abuf", [128, PT], F32)
        vrows = abuf
        accs = [sb("acc%d" % i, [128, PT], F32) for i in range(3)]
        t512 = [sb("t512_%d" % i, [128, 512], F32) for i in range(NT5)]
        sqb = [sb("sqb%d" % i, [128, 512], BF16) for i in range(2)]
        ptile = [sb("ptile%d" % i, [128, PLE], F32) for i in range(3)]
        ptb = [sb("ptb%d" % i, [128, PLE], BF16) for i in range(2)]
        rt = sb("rt", [128, 192], F32)

        banks = [es.enter_context(nc.psum_tensor("bank%d" % i, [128, 512], F32)) for i in range(8)]

        R_x = [[Region() for _ in range(2)] for _ in range(NJ)]
        R_hT = [Region() for _ in range(NJ)]
        R_uA = [[Region() for _ in range(NTT)] for _ in range(8)]
        R_uB = [[Region() for _ in range(NTT)] for _ in range(8)]
        R_m = [[Region() for _ in range(NTT)] for _ in range(8)]
        R_slot = [Region(), Region(), Region()]
        R_haloA = [Region() for _ in range(8)]
        R_haloB = [Region() for _ in range(8)]
        R_const = Region()
        R_ustr = Region()
        R_eps = Region()
        R_rw = Region()
        R_cv = Region()
        R_cnt = Region()
        R_slotw = [Region() for _ in range(NG)]
        R_gbc = Region()
        R_hn = [Region(), Region()]
        R_stat = [Region() for _ in range(64)]
        R_uG = [Region(), Region()]
        R_vA = Region()
        R_ab = Region()
        R_vrows = R_ab
        R_acc = [Region() for _ in range(3)]
        R_t = [Region() for _ in range(NT5)]
        R_sq = [Region(), Region()]
        R_pt = [Region(), Region(), Region()]
        R_ptb = [Region(), Region()]
        R_rt = Region()
        R_bank = [Region() for _ in range(8)]
        out_toks = []
        state = {"bank": 0, "slot": 0, "t": 0, "stat": 0, "hn": 0}

        resv = set()

        def next_bank():
            while True:
                b = state["bank"]
                state["bank"] = (b + 1) % 8
                if b not in resv:
                    return b

        t_resv = set()

        def next_t():
            while True:
                t = state["t"]
                state["t"] = (t + 1) % NT5
                if t not in t_resv:
                    return t

        def next_stat():
            t = state["stat"]
            state["stat"] = (t + 1) % 64
            return t

        def mm(out, lhsT, rhs, start, stop, reads, writes):
            S.op("pe", lambda e: e.matmul(out=out, lhsT=lhsT, rhs=rhs, start=start, stop=stop), reads, writes)

        def act(out, in_, func, reads, writes, bias=None, scale=None, accum_out=None):
            kw = {}
            if bias is not None:
                kw["bias"] = bias
            if scale is not None:
                kw["scale"] = scale
            if accum_out is not None:
                kw["accum_out"] = accum_out
            S.op("act", lambda e: e.activation(out=out, in_=in_, func=func, **kw), reads, writes)

        def tt(eng, out, in0, in1, op, reads, writes):
            S.op(eng, lambda e: e.tensor_tensor(out=out, in0=in0, in1=in1, op=op), reads, writes)

        def ts(eng, out, in0, s1, s2, op0, op1, reads, writes):
            if s2 is None:
                S.op(eng, lambda e: e.tensor_scalar(out=out, in0=in0, scalar1=s1, scalar2=None, op0=op0), reads, writes)
            else:
                S.op(eng, lambda e: e.tensor_scalar(out=out, in0=in0, scalar1=s1, scalar2=s2, op0=op0, op1=op1), reads, writes)

        def stt(out, in0, scalar, in1, op0, op1, reads, writes):
            S.op("dve", lambda e: e.scalar_tensor_tensor(out=out, in0=in0, scalar=scalar, in1=in1, op0=op0, op1=op1), reads, writes)

        def copy(eng, out, in_, reads, writes):
            S.op(eng, lambda e: e.tensor_copy(out=out, in_=in_), reads, writes)

        def memset(eng, ap, val, writes):
            S.op(eng, lambda e: e.memset(ap, val), (), writes)

        def dma(eng, out, in_, reads, writes, slow=False):
            if slow:
                return S.dma(eng, lambda e: e.dma_start(out=out, in_=in_, allow_slow_non_contiguous=True), reads, writes)
            return S.dma(eng, lambda e: e.dma_start(out=out, in_=in_), reads, writes)

        for j in range(NJ):
            dma("sp", x_tok[:, j, :], x_d[j * 128:(j + 1) * 128, :], (), [R_x[j][0], R_x[j][1]])

        dma("sp", identf[:], ident_d, (), [R_const])
        copy("dve", identb[:], identf[:], [R_const], [R_const])
        memset("pool", onesm[:], 1.0 / 1024.0, [R_const])
        memset("pool", ones_row[:], 1.0, [R_const])
        memset("pool", epsc[:], EPS, [R_eps])
        dma("sp", vrows[0:31, :], conv_b_w, (), [R_vrows])
        dma("sp", vrows[31:34, :], conv_a_w, (), [R_vrows])
        for r, v in ((34, conv_b_b), (35, ln_b_g), (36, ln_b_b), (39, mix_g), (40, ffn_g), (41, ple_g)):
            dma("sp", vrows[r:r + 1, :], v.rearrange("(o d) -> o d", o=1), (), [R_vrows])
        dma("sp", vrows[37:39, :], b_gate.rearrange("(o d) -> o d", o=2), (), [R_vrows])
        dma("sp", gbc[:], ffn_g.partition_broadcast(128), (), [R_gbc])
        dma("sp", accs[0][:, 0:128], ustr_d, (), [R_acc[0]])
        copy("dve", ustr_b[:], accs[0][:, 0:128], [R_acc[0]], [R_ustr])
        dma("sp", ebase_t[:], ebase_d, (), [R_ustr])
        memset("pool", ones128[:], 1.0, [R_ustr])
        memset("pool", cnt_bc[:], 0.0, [R_cnt])
        for c in range(8):
            b = next_bank()
            mm(banks[b][:, 0:NV], vrows[0:NV, c * 128:(c + 1) * 128], identf[0:NV, 0:NV], True, True,
               [R_vrows, R_const], [R_bank[b]])
            copy("dve", cv[:, c, :], banks[b][:, 0:NV], [R_bank[b]], [R_cv])
        CB0, CA0, CBB, LNG, LNB, BGA, BGB, GMIX, GFFN, GPLE = 0, 31, 34, 35, 36, 37, 38, 39, 40, 41

        def load_unit(parts, s=None):
            if s is None:
                s = state["slot"]
                state["slot"] = 1 - s
            for (off, k, n, src) in parts:
                dst = slots[s][:, off:off + k * n].rearrange("p (k n) -> p k n", k=k)
                dma("pool", dst, src.rearrange("(k p) n -> p k n", p=128), (), [R_slot[s]])
            return s

        def rstd_of(xap, xregs, junk_ap=None, junk_regs=None):
            si = next_stat()
            hb = state["hn"]
            if junk_ap is None:
                junk_ap, junk_regs = hn[hb][:], [R_hn[hb]]
            act(junk_ap, xap, AF.Square, xregs + [R_stat[si]], junk_regs + [R_stat[si]], accum_out=stat[:, si:si + 1])
            act(stat[:, si:si + 1], stat[:, si:si + 1], AF.Sqrt, [R_stat[si], R_eps], [R_stat[si]], bias=epsc[:, 0:1], scale=1.0 / D)
            S.op("dve", lambda e: e.reciprocal(out=stat[:, si:si + 1], in_=stat[:, si:si + 1]), [R_stat[si]], [R_stat[si]])
            return si

        def norm_batch(tiles, gcol=None, gain_bc=False):
            sis = []
            for (xap, xregs, dst, dregs, hn_ap, hn_regs) in tiles:
                si = next_stat()
                sis.append(si)
                act(hn[0][:], xap, AF.Square, xregs + [R_stat[si]], [R_hn[0], R_stat[si]], accum_out=stat[:, si:si + 1])
            for si in sis:
                act(stat[:, si:si + 1], stat[:, si:si + 1], AF.Sqrt, [R_stat[si], R_eps], [R_stat[si]], bias=epsc[:, 0:1], scale=1.0 / D)
            for si in sis:
                S.op("dve", lambda e, si=si: e.reciprocal(out=stat[:, si:si + 1], in_=stat[:, si:si + 1]), [R_stat[si]], [R_stat[si]])
            for si, (xap, xregs, dst, dregs, hn_ap, hn_regs) in zip(sis, tiles):
                if gain_bc:
                    stt(hn_ap, xap, stat[:, si:si + 1], gbc[:], ALU.mult, ALU.mult, xregs + [R_stat[si], R_gbc], hn_regs)
                else:
                    ts("dve", hn_ap, xap, stat[:, si:si + 1], None, ALU.mult, None, xregs + [R_stat[si]], hn_regs)
            pts = []

            def evac(i_):
                (b, pT) = pts[i_]
                (xap, xregs, dst, dregs, hn_ap, hn_regs) = tiles[i_]
                if gcol is not None:
                    tt("dve", dst, pT, cv[:, :, gcol:gcol + 1].to_broadcast([128, 8, 128]), ALU.mult, [R_bank[b], R_cv], dregs)
                elif i_ % 2 == 0:
                    act(dst, pT, AF.Copy, [R_bank[b]], dregs)
                else:
                    copy("dve", dst, pT, [R_bank[b]], dregs)

            for i_, (xap, xregs, dst, dregs, hn_ap, hn_regs) in enumerate(tiles):
                b = next_bank()
                pT = banks[b][:].bitcast(BF16).rearrange("p (c t) -> p c t", c=8)
                pts.append((b, pT))
                for c in range(8):
                    S.op("pe", lambda e, c=c, pT=pT, hn_ap=hn_ap: e.transpose(out=pT[:, c, :], in_=hn_ap[:, c * 128:(c + 1) * 128], identity=identb[:]),
                         hn_regs + [R_const], [R_bank[b]])
                if i_ >= 2:
                    evac(i_ - 2)
            for i_ in range(max(0, len(tiles) - 2), len(tiles)):
                evac(i_)

        def norm_T(xap, xregs, dst, dregs, gcol=None, gain_bc=False, hn_ap=None, hn_regs=None):
            si = rstd_of(xap, xregs)
            hb = state["hn"]
            state["hn"] = 1 - hb
            if hn_ap is None:
                hn_ap, hn_regs = hn[hb][:], [R_hn[hb]]
            if gain_bc:
                stt(hn_ap, xap, stat[:, si:si + 1], gbc[:], ALU.mult, ALU.mult, xregs + [R_stat[si], R_gbc], hn_regs)
            else:
                ts("dve", hn_ap, xap, stat[:, si:si + 1], None, ALU.mult, None, xregs + [R_stat[si]], hn_regs)
            b = next_bank()
            pT = banks[b][:].bitcast(BF16).rearrange("p (c t) -> p c t", c=8)
            for c in range(8):
                S.op("pe", lambda e, c=c: e.transpose(out=pT[:, c, :], in_=hn_ap[:, c * 128:(c + 1) * 128], identity=identb[:]),
                     hn_regs + [R_const], [R_bank[b]])
            if gcol is not None:
                tt("dve", dst, pT, cv[:, :, gcol:gcol + 1].to_broadcast([128, 8, 128]), ALU.mult, [R_bank[b], R_cv], dregs)
            else:
                act(dst, pT, AF.Copy, [R_bank[b]], dregs)
            return hb

        def barrier():
            toks = [("e", k, S.count[k]) for k in ("pe", "act", "dve", "pool") if S.count[k] > 0]
            for rk, base in (("sw", 0), ("hw", S.ring)):
                nk = S.dma_k[rk]
                for s_ in range(min(nk, S.ring)):
                    k_last = ((nk - 1 - s_) // S.ring) * S.ring + s_
                    toks.append(("d", base + s_, 16 * (k_last // S.ring + 1)))
            for eng in ENG_KEYS:
                S.final_wait(eng, toks)

        def hT_reads(t):
            return [R_hT[4 * t + i] for i in range(4)]

        zero_toks = []
        _breg = {}

        def bcheck(e):
            if "r" not in _breg:
                _breg["r"] = e.to_reg(NE * CAP - 1)
            return _breg["r"]

        scat_toks, xs_toks = [], []
        nxt = None
        for ps in range(n_pass):
            tok0 = ps * PT
            first_half = (ps % 2 == 0)
            for j in range(NJ):
                if ps > 0:
                    dma("sp", x_tok[:, j, :], x_d[tok0 + j * 128: tok0 + (j + 1) * 128, :], (), [R_x[j][0], R_x[j][1]])

            def s1_parts(g_):
                return [(j * 2048, 8, 256, w_in[:, j * 1024 + g_ * 256: j * 1024 + (g_ + 1) * 256]) for j in range(5)]

            def ypartsA(g_):
                return [(0, 8, 512, w_out_a[:, g_ * 512:(g_ + 1) * 512]),
                        (4096, 8, 512, w_in[:, 5120 + g_ * 512: 5120 + (g_ + 1) * 512])]

            def ypartsB(g_):
                return [(0, 8, 512, w_out_b[:, g_ * 512:(g_ + 1) * 512]),
                        (4096, 8, 512, w_in[:, 6144 + g_ * 512: 6144 + (g_ + 1) * 512])]

            if nxt is None:
                nxt = load_unit(s1_parts(0))
            if ps == 0:
                dma("pool", wr[:, :, 0:4], w_rg.rearrange("(k p) n -> p k n", p=128), (), [R_rw])
                dma("pool", wr[:, :, 4:36], w_re.rearrange("(k p) n -> p k n", p=128), (), [R_rw])
                dma("pool", rb[0:1, 0:4], b_rg.rearrange("(o d) -> o d", o=1), (), [R_rw])
                dma("pool", rb[0:1, 4:36], b_re.rearrange("(o d) -> o d", o=1), (), [R_rw])
            norm_batch([(x_tok[:, j, :], [R_x[j][0], R_x[j][1]], hT[:, :, j * 128:(j + 1) * 128], [R_hT[j]],
                         mT[:, j, :], [R_m[j][0], R_m[j][1]]) for j in range(NJ)], gcol=GMIX)

            if ps == 0:
                for c in range(8):
                    memset("pool", mT[:, c, :], 0.0, [R_m[c][0], R_m[c][1]])
                buf_v = buf_d.rearrange("(n p) d -> p n d", p=128)
                zero_pending = list(range(NE * CAP // 128 // 8))

                def zero_some(n_):
                    for _ in range(n_):
                        if zero_pending:
                            i = zero_pending.pop(0)
                            zero_toks.append(dma("act", buf_v[:, 8 * i:8 * i + 8, :], mT[:],
                                                 [R_m[c_][t_] for c_ in range(8) for t_ in range(NTT)], [Region()]))


            dgA = accs[0][:].bitcast(BF16).rearrange("p (k m) -> p k m", m=128)
            dgB = accs[1][:].bitcast(BF16).rearrange("p (k m) -> p k m", m=128)[:, 0:15, :]

            KPE = 23

            def dg_build(c):
                tt("dve", dgA, identb[:].unsqueeze(1).to_broadcast([128, 16, 128]),
                   cv[:, c, CB0:CB0 + 16].unsqueeze(2).to_broadcast([128, 16, 128]), ALU.mult, [R_const, R_cv], [R_acc[0]])
                tt("dve", dgB[:, 0:KPE - 16, :], identb[:].unsqueeze(1).to_broadcast([128, KPE - 16, 128]),
                   cv[:, c, CB0 + 16:CB0 + KPE].unsqueeze(2).to_broadcast([128, KPE - 16, 128]), ALU.mult, [R_const, R_cv], [R_acc[1]])

            def conv_pe(c, ub):
                bks = []
                for t in range(NTT):
                    b = next_bank()
                    bks.append(b)
                    for k in range(KPE):
                        lhsT = dgA[:, k, :] if k < 16 else dgB[:, k - 16, :]
                        mm(banks[b][:], lhsT, uGb[ub][:, k + t * 512: k + (t + 1) * 512], k == 0, k == KPE - 1,
                           [R_acc[0], R_acc[1], R_uG[ub]], [R_bank[b]])
                tmps = [next_t() for _ in range(NTT)]
                for idx, k in enumerate(range(KPE, 31)):
                    for t in range(NTT):
                        if idx == 0:
                            in1, in1_regs = banks[bks[t]][:], [R_bank[bks[t]]]
                        else:
                            in1, in1_regs = t512[tmps[t]][:], [R_t[tmps[t]]]
                        stt(t512[tmps[t]][:], uGb[ub][:, k + t * 512: k + (t + 1) * 512], cv[:, c, CB0 + k:CB0 + k + 1], in1,
                            ALU.mult, ALU.add, [R_uG[ub], R_cv] + in1_regs, [R_t[tmps[t]]])
                for t in range(NTT):
                    act(uB[:, c, t * 512:(t + 1) * 512], t512[tmps[t]][:], AF.Identity, [R_t[tmps[t]], R_cv], [R_uB[c][t]],
                        bias=cv[:, c, CBB:CBB + 1])

            for c in range(8):
                ub = c % 2
                uG = uGb[ub]
                if c % 2 == 0:
                    s = nxt
                    nxt = load_unit(s1_parts(c // 2 + 1) if c < 6 else ypartsA(0))
                if ps == 0:
                    zero_some(3 if c < 7 else 99)
                if c >= 1:
                    dg_build(c - 1)
                if first_half:
                    memset("pool", uG[:, 0:30], 0.0, [R_uG[ub]])
                    memset("pool", vA[:, 0:2], 0.0, [R_vA])
                else:
                    copy("pool", uG[:, 0:30], haloB[:, c, :], [R_haloB[c]], [R_uG[ub]])
                    copy("pool", vA[:, 0:2], haloA[:, c, :], [R_haloA[c]], [R_vA])
                for t in range(NTT):
                    bk = []
                    for j in range(5):
                        b = next_bank()
                        bk.append(b)
                        for k in range(8):
                            mm(banks[b][:], slots[s][:, j * 2048 + k * 256 + ub * 128: j * 2048 + k * 256 + (ub + 1) * 128],
                               hT[:, k, t * 512:(t + 1) * 512], k == 0, k == 7,
                               [R_slot[s]] + hT_reads(t), [R_bank[b]])
                    sl = slice(t * 512, (t + 1) * 512)
                    act(abuf[:, sl], banks[bk[0]][:], AF.Copy, [R_bank[bk[0]]], [R_ab])
                    tx = next_t()
                    act(t512[tx][:], banks[bk[2]][:], AF.Copy, [R_bank[bk[2]]], [R_t[tx]])
                    tt("dve", vA[:, 2 + t * 512: 2 + (t + 1) * 512], banks[bk[1]][:], t512[tx][:], ALU.mult,
                       [R_bank[bk[1]], R_t[tx]], [R_vA])
                    tg = next_t()
                    act(t512[tg][:], banks[bk[4]][:], AF.Sigmoid, [R_bank[bk[4]]], [R_t[tg]])
                    tt("dve", uG[:, 30 + t * 512: 30 + (t + 1) * 512], banks[bk[3]][:], t512[tg][:], ALU.mult,
                       [R_bank[bk[3]], R_t[tg]], [R_uG[ub]])
                if c >= 1:
                    conv_pe(c - 1, 1 - ub)
                a2 = accs[2]
                ts("dve", a2[:], vA[:, 0:PT], cv[:, c, CA0:CA0 + 1], None, ALU.mult, None, [R_vA, R_cv], [R_acc[2]])
                for k in range(1, 3):
                    stt(a2[:], vA[:, k:k + PT], cv[:, c, CA0 + k:CA0 + k + 1], a2[:], ALU.mult, ALU.add,
                        [R_vA, R_cv, R_acc[2]], [R_acc[2]])
                tt("dve", uA[:, c, :], a2[:], abuf[:], ALU.mult, [R_acc[2], R_ab], [R_uA[c][0], R_uA[c][1]])
                if first_half:
                    copy("pool", haloB[:, c, :], uG[:, PT:PT + 30], [R_uG[ub]], [R_haloB[c]])
                    copy("pool", haloA[:, c, :], vA[:, PT:PT + 2], [R_vA], [R_haloA[c]])
            dg_build(7)
            conv_pe(7, 1)

            ln_tm, ln_tr = [None] * NTT, [None] * NTT

            def ln_stats():
                for t in range(NTT):
                    sl = slice(t * 512, (t + 1) * 512)
                    bm = next_bank()
                    bq = next_bank()
                    for c in range(8):
                        q = c % 2
                        act(sqb[q][:], uB[:, c, sl], AF.Square, [R_uB[c][t]], [R_sq[q]])
                        mm(banks[bm][:], onesm[:], uB[:, c, sl], c == 0, c == 7, [R_const, R_uB[c][t]], [R_bank[bm]])
                        mm(banks[bq][:], onesm[:], sqb[q][:], c == 0, c == 7, [R_const, R_sq[q]], [R_bank[bq]])
                    tm, tq, tr = next_t(), next_t(), next_t()
                    t_resv.add(tm)
                    t_resv.add(tr)
                    act(t512[tm][:], banks[bm][:], AF.Copy, [R_bank[bm]], [R_t[tm]])
                    act(t512[tq][:], banks[bm][:], AF.Square, [R_bank[bm]], [R_t[tq]])
                    stt(t512[tr][:], banks[bq][:], EPS, t512[tq][:], ALU.add, ALU.subtract, [R_bank[bq], R_t[tq]], [R_t[tr]])
                    act(t512[tr][:], t512[tr][:], AF.Sqrt, [R_t[tr]], [R_t[tr]])
                    S.op("dve", lambda e, tr=tr: e.reciprocal(out=t512[tr][:], in_=t512[tr][:]), [R_t[tr]], [R_t[tr]])
                    ln_tm[t], ln_tr[t] = tm, tr

            def ln_norm(c):
                for t in range(NTT):
                    sl = slice(t * 512, (t + 1) * 512)
                    tm, tr = ln_tm[t], ln_tr[t]
                    ta = next_t()
                    tt("dve", t512[ta][:], uB[:, c, sl], t512[tm][:], ALU.subtract, [R_uB[c][t], R_t[tm]], [R_t[ta]])
                    tt("dve", t512[ta][:], t512[ta][:], t512[tr][:], ALU.mult, [R_t[ta], R_t[tr]], [R_t[ta]])
                    act(uB[:, c, sl], t512[ta][:], AF.Silu, [R_t[ta], R_cv], [R_uB[c][t]],
                        bias=cv[:, c, LNB:LNB + 1], scale=cv[:, c, LNG:LNG + 1])

            def sweep(c, s, src_, rr_, bcol, first):
                cc = c % 4
                for t in range(NTT):
                    sl = slice(t * 512, (t + 1) * 512)
                    by = next_bank()
                    bg = next_bank()
                    for k in range(8):
                        mm(banks[by][:], slots[s][:, k * 512 + cc * 128: k * 512 + (cc + 1) * 128], src_[:, k, sl], k == 0, k == 7,
                           [R_slot[s], rr_[k][t]], [R_bank[by]])
                    for k in range(8):
                        mm(banks[bg][:], slots[s][:, 4096 + k * 512 + cc * 128: 4096 + k * 512 + (cc + 1) * 128], hT[:, k, sl],
                           k == 0, k == 7, [R_slot[s]] + hT_reads(t), [R_bank[bg]])
                    sg = next_t()
                    act(t512[sg][:], banks[bg][:], AF.Sigmoid, [R_bank[bg], R_cv], [R_t[sg]], bias=cv[:, c, bcol:bcol + 1])
                    if first:
                        tt("dve", mT[:, c, sl], banks[by][:], t512[sg][:], ALU.mult, [R_bank[by], R_t[sg]], [R_m[c][t]])
                    else:
                        t2 = next_t()
                        tt("dve", t512[t2][:], banks[by][:], t512[sg][:], ALU.mult, [R_bank[by], R_t[sg]], [R_t[t2]])
                        tt("pool", mT[:, c, sl], mT[:, c, sl], t512[t2][:], ALU.add, [R_m[c][t], R_t[t2]], [R_m[c][t]])

            for c in range(8):
                if c % 4 == 0:
                    s = nxt
                    nxt = load_unit(ypartsA(1) if c == 0 else ypartsB(0))
                sweep(c, s, uA, R_uA, BGA, True)
                if c == 0:
                    ln_stats()
                else:
                    ln_norm(c - 1)
            ln_norm(7)
            t_resv.clear()
            for c in range(8):
                if c % 4 == 0:
                    s = nxt
                    nxt = load_unit(ypartsB(1) if c == 0 else [(0, 8, 1024, w_o)])
                sweep(c, s, uB, R_uB, BGB, False)

            s = nxt

            def exp_parts(e):
                return [(0, 8, 512, w_eg[e]), (4096, 8, 512, w_eu[e]), (8192, 4, 1024, w_ed[e])]

            ple_parts = [(0, 8, 1024, w_pg), (8192, 2, 1024, w_pp)]
            if ps + 1 < n_pass:
                nxt = load_unit(s1_parts(0))
            else:
                nxt = load_unit(exp_parts(0) if n_exp > 0 else ple_parts)
            for j in range(NJ):
                for h in range(2):
                    b = next_bank()
                    for k in range(8):
                        mm(banks[b][:], mT[:, k, j * 128:(j + 1) * 128], slots[s][:, k * 1024 + h * 512: k * 1024 + (h + 1) * 512],
                           k == 0, k == 7, [R_slot[s], R_m[k][j // 4]], [R_bank[b]])
                    tt("dve", x_tok[:, j, h * 512:(h + 1) * 512], x_tok[:, j, h * 512:(h + 1) * 512], banks[b][:], ALU.add,
                       [R_x[j][h], R_bank[b]], [R_x[j][h]])

            bL = next_bank()
            resv.add(bL)
            Lps = banks[bL][:].rearrange("p (j c) -> p j c", j=NJ)
            for j in range(NJ):
                xs_toks.append(dma("sp", xs_d[tok0 + j * 128: tok0 + (j + 1) * 128, :], x_tok[:, j, :], [R_x[j][0], R_x[j][1]], [Region()]))
            norm_batch([(x_tok[:, j, :], [R_x[j][0], R_x[j][1]], hT[:, :, j * 128:(j + 1) * 128], [R_hT[j]],
                         uA[:, j, :], [R_uA[j][0], R_uA[j][1]]) for j in range(NJ)], gain_bc=True)
            for j in range(NJ):
                for k in range(8):
                    mm(Lps[:, j, 0:36], hT[:, k, j * 128:(j + 1) * 128], wr[:, k, :], k == 0, False,
                       [R_hT[j], R_const, R_rw], [R_bank[bL]])
                mm(Lps[:, j, 0:36], ones_row[0:1, :], rb[0:1, :], False, True, [R_const, R_rw], [R_bank[bL]])
            A0, A1 = accs[0], accs[1]
            rr = [R_acc[0], R_acc[1]]
            rw = rr
            v3 = lambda ap, n: ap.rearrange("p (j c) -> p j c", j=NJ)
            L = v3(A0[:, 0:288], 36)
            sel = v3(A0[:, 288:544], 32)
            mask1 = v3(A0[:, 544:800], 32)
            sm = lambda i: A0[:, 800 + 8 * i: 808 + 8 * i]
            gmax, gsum, pgrp, m1, m2, dd, e2, den, w1, w2, s1f, s2f, ov1, ov2 = (sm(i) for i in range(14))
            gmask = v3(A0[:, 912:944], 4)
            pen = v3(A0[:, 944:976], 4)
            gex = v3(A0[:, 976:1008], 4)
            mask2 = v3(A1[:, 0:256], 32)
            rank = v3(A1[:, 256:512], 32)
            over = v3(A1[:, 512:768], 32)
            sel2 = v3(A1[:, 768:1024], 32)
            tmp = sel2
            bc = lambda ap, n: ap.unsqueeze(2).to_broadcast([128, NJ, n])
            red = lambda out, in_, op: S.op("dve", lambda e: e.tensor_reduce(out=out, in_=in_, axis=AX.X, op=op), rr, rw)
            act(L, Lps[:, :, 0:36], AF.Copy, [R_bank[bL]], rw)
            resv.discard(bL)
            red(gmax, L[:, :, 0:4], ALU.max)
            tt("dve", gmask, L[:, :, 0:4], bc(gmax, 4), ALU.is_ge, rr, rw)
            tt("dve", gex, L[:, :, 0:4], bc(gmax, 4), ALU.subtract, rr, rw)
            act(gex, gex, AF.Exp, rr, rw)
            red(gsum, gex, ALU.add)
            S.op("dve", lambda e: e.reciprocal(out=pgrp, in_=gsum), rr, rw)
            ts("dve", pen, gmask, 1.0, 1e30, ALU.subtract, ALU.mult, rr, rw)
            tt("dve", sel.rearrange("p j (g e) -> p j g e", g=4), L[:, :, 4:36].rearrange("p j (g e) -> p j g e", g=4),
               pen.unsqueeze(3).to_broadcast([128, NJ, 4, 8]), ALU.add, rr, rw)
            red(m1, sel, ALU.max)
            tt("dve", mask1, sel, bc(m1, 32), ALU.is_ge, rr, rw)
            stt(sel2, mask1, -1e30, sel, ALU.mult, ALU.add, rr, rw)
            red(m2, sel2, ALU.max)
            tt("dve", mask2, sel2, bc(m2, 32), ALU.is_ge, rr, rw)
            tt("dve", dd, m2, m1, ALU.subtract, rr, rw)
            act(e2, dd, AF.Exp, rr, rw)
            ts("dve", den, e2, 1.0, None, ALU.add, None, rr, rw)
            S.op("dve", lambda e: e.reciprocal(out=den, in_=den), rr, rw)
            tt("dve", w1, den, pgrp, ALU.mult, rr, rw)
            tt("dve", w2, w1, e2, ALU.mult, rr, rw)
            R_mb = Region()
            tt("dve", maskb[:], mask1, mask2, ALU.add, rr, [R_mb])
            b2 = next_bank()
            Rps = banks[b2][:].rearrange("p (j c) -> p j c", j=NJ)
            for j in range(NJ):
                mm(Rps[:, j, 0:NE], ustr_b[:], maskb[:, j, :], True, j == 0, [R_const, R_ustr, R_mb], [R_bank[b2]])
                for j2 in range(j):
                    mm(Rps[:, j, 0:NE], ones128[:], maskb[:, j2, :], False, j2 == j - 1, [R_const, R_ustr, R_mb], [R_bank[b2]])
            for j2 in range(NJ):
                mm(Rps[:, NJ - 1, NE:2 * NE], ones128[:], maskb[:, j2, :], j2 == 0, j2 == NJ - 1, [R_const, R_ustr, R_mb], [R_bank[b2]])
            tt("dve", rank, Rps[:, :, 0:NE], cnt_bc[:].unsqueeze(1).to_broadcast([128, NJ, NE]), ALU.add, rr + [R_bank[b2], R_cnt], rw)
            tt("dve", cnt_bc[:], cnt_bc[:], Rps[:, NJ - 1, NE:2 * NE], ALU.add, [R_cnt, R_bank[b2]], [R_cnt])
            ts("dve", over, rank, float(CAP), None, ALU.is_ge, None, rr, rw)
            tt("dve", rank, rank, ebase_t[:].unsqueeze(1).to_broadcast([128, NJ, NE]), ALU.add, rr + [R_const, R_ustr], rw)
            stt(rank, over, 1.0e6, rank, ALU.mult, ALU.add, rr, rw)
            for (mk, src_, dst_) in ((mask1, rank, s1f), (mask2, rank, s2f), (mask1, over, ov1), (mask2, over, ov2)):
                tt("dve", tmp, mk, src_, ALU.mult, rr, rw)
                red(dst_, tmp, ALU.add)
            ts("dve", ov1, ov1, -1.0, 1.0, ALU.mult, ALU.add, rr, rw)
            ts("dve", ov2, ov2, -1.0, 1.0, ALU.mult, ALU.add, rr, rw)
            g0 = ps * NJ
            R_sw = [R_slotw[g0 + j] for j in range(NJ)]
            w12v = w12[:, 2 * g0:2 * g0 + 2 * NJ].rearrange("p (j k) -> p j k", k=2)
            siv = slot_i[:, 2 * g0:2 * g0 + 2 * NJ].rearrange("p (j k) -> p j k", k=2)
            tt("dve", w12v[:, :, 0], w1, ov1, ALU.mult, rr, R_sw)
            tt("dve", w12v[:, :, 1], w2, ov2, ALU.mult, rr, R_sw)
            copy("dve", siv[:, :, 0], s1f, rr, R_sw)
            copy("dve", siv[:, :, 1], s2f, rr, R_sw)
            sgv = slot_g[:, 2 * g0:2 * g0 + 2 * NJ].rearrange("p (j k) -> p j k", k=2)
            ts("dve", s1f, s1f, float(NE * CAP - 1), None, ALU.min, None, rr, rw)
            ts("dve", s2f, s2f, float(NE * CAP - 1), None, ALU.min, None, rr, rw)
            copy("dve", sgv[:, :, 0], s1f, rr, R_sw)
            copy("dve", sgv[:, :, 1], s2f, rr, R_sw)
            if ps == 0:
                S.final_wait("pool", zero_toks)
            for j in range(NJ):
                g = g0 + j
                for kk in range(2):
                    idx = slot_i[:, 2 * g + kk:2 * g + kk + 1]
                    scat_toks.append(S.dma("pool", lambda e, idx=idx, j=j: e.indirect_dma_start(
                        out=buf_d, out_offset=bass.IndirectOffsetOnAxis(ap=idx, axis=0), in_=uA[:, j, :], in_offset=None,
                        bounds_check=bcheck(e), oob_is_err=False), [R_uA[j][0], R_uA[j][1], R_slotw[g]], [Region()]))

        barrier()
        hTe = [uA[:, :, 0:CAP], uB[:, :, 0:CAP]]
        actE = [mT[:, 0:4, 0:CAP], mT[:, 4:8, 0:CAP]]
        hbt = [hT[:, i, :] for i in (0, 1, 2, 5, 6, 7)]
        yts = [hT[:, 3 + i, :] for i in range(2)]
        R_hTe = [[Region() for _ in range(NBLK)] for _ in range(2)]
        R_actE = [[Region() for _ in range(4)] for _ in range(2)]
        R_hbt = [Region() for _ in range(6)]
        R_yts = [Region() for _ in range(2)]
        ystore_toks = []
        cnt = {"hbt": 0, "yt": 0}
        def p2_T(ex):
            eb = ex % 2
            for blk in range(NBLK):
                hi = cnt["hbt"] % 6
                cnt["hbt"] += 1
                r0 = ex * CAP + blk * 128
                dma("sp", hbt[hi], buf_d[r0:r0 + 128, :], (), [R_hbt[hi]])
                b = next_bank()
                pT = banks[b][:].bitcast(BF16).rearrange("p (c t) -> p c t", c=8)
                for c in range(8):
                    S.op("pe", lambda e, c=c, pT=pT, hi=hi: e.transpose(out=pT[:, c, :], in_=hbt[hi][:, c * 128:(c + 1) * 128], identity=identb[:]),
                         [R_hbt[hi], R_const], [R_bank[b]])
                if blk % 2 == 0:
                    act(hTe[eb][:, :, blk * 128:(blk + 1) * 128], pT, AF.Copy, [R_bank[b]], [R_hTe[eb][blk]])
                else:
                    copy("dve", hTe[eb][:, :, blk * 128:(blk + 1) * 128], pT, [R_bank[b]], [R_hTe[eb][blk]])

        def p2_GU(ex, s):
            eb = ex % 2
            for q in range(4):
                for cg in range(CAP // CG):
                    cs = slice(cg * CG, (cg + 1) * CG)
                    bg = next_bank()
                    bu = next_bank()
                    for k in range(8):
                        mm(banks[bg][:, 0:CG], slots[s][:, k * 512 + q * 128: k * 512 + (q + 1) * 128], hTe[eb][:, k, cs],
                           k == 0, k == 7, [R_slot[s]] + R_hTe[eb], [R_bank[bg]])
                    for k in range(8):
                        mm(banks[bu][:, 0:CG], slots[s][:, 4096 + k * 512 + q * 128: 4096 + k * 512 + (q + 1) * 128], hTe[eb][:, k, cs],
                           k == 0, k == 7, [R_slot[s]] + R_hTe[eb], [R_bank[bu]])
                    tg = next_t()
                    act(t512[tg][:, 0:CG], banks[bg][:, 0:CG], AF.Silu, [R_bank[bg]], [R_t[tg]])
                    tt("dve", actE[eb][:, q, cs], banks[bu][:, 0:CG], t512[tg][:, 0:CG], ALU.mult, [R_bank[bu], R_t[tg]], [R_actE[eb][q]])

        def p2_DN(ex, s):
            eb = ex % 2
            for blk in range(NBLK):
                yi = cnt["yt"] % 2
                cnt["yt"] += 1
                for h in range(2):
                    b = next_bank()
                    for q in range(4):
                        mm(banks[b][:], actE[eb][:, q, blk * 128:(blk + 1) * 128],
                           slots[s][:, 8192 + q * 1024 + h * 512: 8192 + q * 1024 + (h + 1) * 512],
                           q == 0, q == 3, [R_slot[s], R_actE[eb][q]], [R_bank[b]])
                    if h == 0:
                        act(yts[yi][:, 0:512], banks[b][:], AF.Copy, [R_bank[b]], [R_yts[yi]])
                    else:
                        copy("dve", yts[yi][:, 512:1024], banks[b][:], [R_bank[b]], [R_yts[yi]])
                r0 = ex * CAP + blk * 128
                ystore_toks.append(dma("sp", ybuf_d[r0:r0 + 128, :], yts[yi], [R_yts[yi]], [Region()]))

        slots.append(x_tok[:, 2:8, :].rearrange("p a b -> p (a b)").bitcast(BF16))
        s0 = nxt
        seq = [s0, 2, 1 - s0]
        if n_exp > 0:
            p2_T(0)
        if n_exp > 1:
            load_unit(exp_parts(1), seq[1])
        for ex in range(n_exp):
            s = seq[ex % 3]
            if ex + 2 < n_exp:
                load_unit(exp_parts(ex + 2), seq[(ex + 2) % 3])
            elif ex + 2 == n_exp:
                free01 = [q for q in (0, 1) if q not in (seq[ex % 3], seq[(ex + 1) % 3])]
                nxt = load_unit(ple_parts, free01[0])
            p2_GU(ex, s)
            if ex + 1 < n_exp:
                p2_T(ex + 1)
            p2_DN(ex, s)
        if n_exp < 2:
            nxt = load_unit(ple_parts, 1 - s0)

        barrier()
        s = nxt
        dma("sp", gbc[:], fin_g.partition_broadcast(128), (), [R_gbc])
        xt3 = [x_tok[:, i, :] for i in range(7)]
        y3 = [[uB[:, 2 * i, :], uB[:, 2 * i + 1, :]] for i in range(3)]
        ot3 = [accs[0][:], accs[1][:]]
        hT3 = [hT[:, :, i * 128:(i + 1) * 128] for i in range(3)]
        pT3 = [uA[:, 0:2, i * 128:(i + 1) * 128] for i in range(3)]
        R_xt3 = [[Region(), Region()] for _ in range(7)]
        R_y3 = [[Region(), Region()] for _ in range(3)]
        R_ot3 = [R_acc[0], R_acc[1]]
        R_hT3 = [Region(), Region(), Region()]
        R_pT3 = [Region(), Region(), Region()]
        n_tiles = n_pass * NJ

        def p3_s0(g):
            i, i3, iy = g % 2, g % 7, g % 3
            r0 = g * 128
            dma("sp", xt3[i3], xs_d[r0:r0 + 128, :], (), R_xt3[i3])
            dma("sp", ptile[iy][:], p_d[r0:r0 + 128, :], (), [R_pt[iy]])
            for kk in range(2):
                idx = slot_g[:, 2 * g + kk:2 * g + kk + 1]
                S.dma("pool", lambda e, idx=idx, dst=y3[iy][kk]: e.indirect_dma_start(
                    out=dst, out_offset=None, in_=ybuf_d, in_offset=bass.IndirectOffsetOnAxis(ap=idx, axis=0),
                    bounds_check=bcheck(e), oob_is_err=False), [R_slotw[g]], [R_y3[iy][kk]])

        junk3 = mT[:, 7, :]
        R_junk3 = [Region()]

        def p3_s1a(g):
            i, i3, iy = g % 2, g % 7, g % 3
            for kk in range(2):
                for h in range(2):
                    hs = slice(h * 512, (h + 1) * 512)
                    stt(xt3[i3][:, hs], y3[iy][kk][:, hs], w12[:, 2 * g + kk:2 * g + kk + 1], xt3[i3][:, hs], ALU.mult, ALU.add,
                        [R_y3[iy][kk], R_slotw[g], R_xt3[i3][h]], [R_xt3[i3][h]])
            si = rstd_of(xt3[i3], R_xt3[i3], junk3, R_junk3)
            ts("dve", hn[i][:], xt3[i3], stat[:, si:si + 1], None, ALU.mult, None, R_xt3[i3] + [R_stat[si]], [R_hn[i]])
            copy("pool", ptb[i][:], ptile[iy][:], [R_pt[iy]], [R_ptb[i]])

        def p3_s1b(g):
            i, i3, iy = g % 2, g % 7, g % 3
            b = next_bank()
            pT = banks[b][:].bitcast(BF16).rearrange("p (c t) -> p c t", c=8)
            for c in range(8):
                S.op("pe", lambda e, c=c: e.transpose(out=pT[:, c, :], in_=hn[i][:, c * 128:(c + 1) * 128], identity=identb[:]),
                     [R_hn[i], R_const], [R_bank[b]])
            b2 = next_bank()
            pT2 = banks[b2][:].bitcast(BF16).rearrange("p (c t) -> p c t", c=8)
            for c in range(2):
                S.op("pe", lambda e, c=c: e.transpose(out=pT2[:, c, :], in_=ptb[i][:, c * 128:(c + 1) * 128], identity=identb[:]),
                     [R_ptb[i], R_const], [R_bank[b2]])
            tt("dve", hT3[iy], pT, cv[:, :, GPLE:GPLE + 1].to_broadcast([128, 8, 128]), ALU.mult, [R_bank[b], R_cv], [R_hT3[iy]])
            act(pT3[iy], pT2[:, 0:2, :], AF.Copy, [R_bank[b2]], [R_pT3[iy]])

        def p3_s2(g):
            i, i3, iy = g % 2, g % 7, g % 3
            for h in range(2):
                hs = slice(h * 512, (h + 1) * 512)
                bg = next_bank()
                bp = next_bank()
                for k in range(8):
                    mm(banks[bg][:], hT3[iy][:, k, :], slots[s][:, k * 1024 + h * 512: k * 1024 + (h + 1) * 512],
                       k == 0, k == 7, [R_slot[s], R_hT3[iy]], [R_bank[bg]])
                for k in range(2):
                    mm(banks[bp][:], pT3[iy][:, k, :], slots[s][:, 8192 + k * 1024 + h * 512: 8192 + k * 1024 + (h + 1) * 512],
                       k == 0, k == 1, [R_slot[s], R_pT3[iy]], [R_bank[bp]])
                tg, tp = next_t(), next_t()
                act(t512[tg][:], banks[bg][:], AF.Sigmoid, [R_bank[bg]], [R_t[tg]])
                tt("dve", t512[tp][:], banks[bp][:], t512[tg][:], ALU.mult, [R_bank[bp], R_t[tg]], [R_t[tp]])
                tt("pool", xt3[i3][:, hs], xt3[i3][:, hs], t512[tp][:], ALU.add, [R_xt3[i3][h], R_t[tp]], [R_xt3[i3][h]])

        def p3_s3(g):
            i, i3, iy = g % 2, g % 7, g % 3
            r0 = g * 128
            si = rstd_of(xt3[i3], R_xt3[i3], junk3, R_junk3)
            stt(ot3[i], xt3[i3], stat[:, si:si + 1], gbc[:], ALU.mult, ALU.mult, R_xt3[i3] + [R_stat[si], R_gbc], [R_ot3[i]])
            out_toks.append(dma("sp", out_d[r0:r0 + 128, :], ot3[i], [R_ot3[i]], [Region()]))

        p3_s0(0)
        if n_tiles > 1:
            p3_s0(1)
        for step in range(n_tiles + 4):
            if 0 <= step - 4 < n_tiles:
                p3_s3(step - 4)
            if 0 <= step - 2 < n_tiles:
                p3_s2(step - 2)
            if 0 <= step - 1 < n_tiles:
                p3_s1b(step - 1)
            if step + 2 < n_tiles:
                p3_s0(step + 2)
            if step < n_tiles:
                p3_s1a(step)

        S.final_wait("sp", out_toks)
        S.emit()
    return nc


_NC_CACHE = {}


def make_in_maps(inputs, ncores=NCORES):
    f = lambda a: np.ascontiguousarray(np.asarray(a, dtype=np.float32))
    x = f(inputs["x"]).reshape(NCORES, TOK_CORE, D)
    p = f(inputs["p"]).reshape(NCORES, TOK_CORE, PLE)
    shared = {"ident": np.eye(128, dtype=np.float32),
              "ustrict": np.triu(np.ones((128, 128), dtype=np.float32), k=1),
              "ebase": np.ascontiguousarray(np.broadcast_to((np.arange(NE, dtype=np.float32) * CAP)[None, :], (128, NE)))}
    for name in ("mix_norm_g", "w_in", "conv_a_w", "w_out_a", "conv_b_w", "conv_b_b", "ln_b_g", "ln_b_b", "w_out_b",
                 "b_gate", "w_o", "ffn_norm_g", "w_router_group", "b_router_group", "w_router_expert", "b_router_expert",
                 "w_exp_gate", "w_exp_up", "w_exp_down", "ple_norm_g", "w_ple_gate", "w_ple_proj"):
        a = f(inputs[name])
        shared[name] = np.ascontiguousarray(a.reshape(a.shape[1:]))
    shared["final_norm_g"] = f(inputs["final_norm_g"])
    in_maps = []
    for c in range(ncores):
        m = dict(shared)
        m["x"] = x[c]
        m["p"] = p[c]
        in_maps.append(m)
    return in_maps


def kernel(**inputs):
    in_maps = make_in_maps(inputs)
    if "nc" not in _NC_CACHE:
        _NC_CACHE["nc"] = build_nc()
    nc = _NC_CACHE["nc"]
    res = run_bass_kernel_spmd(nc, in_maps, core_ids=list(range(NCORES)))
    out = np.stack([np.asarray(r["out"]) for r in res.results], axis=0)
    return out.reshape(16, SEQ, D).astype(np.float32)
```

```python
import numpy as np
import concourse.bass as bass
import concourse.mybir as mybir
from concourse.bass_utils import run_bass_kernel_spmd
from contextlib import ExitStack

F32 = mybir.dt.float32
BF16 = mybir.dt.bfloat16
ALU = mybir.AluOpType
AF = mybir.ActivationFunctionType
AX = mybir.AxisListType

NCORES = 8
D = 1024
SEQ = 2048
TOK_CORE = 4096
PT = 1024
NPASS = TOK_CORE // PT
NJ = PT // 128
NTT = PT // 512
NE = 32
DE = 512
PLE = 256
EPS = 1e-6
NV = 42
NT5 = 8
CAP = 512
CG = 512
NBLK = CAP // 128
NG = TOK_CORE // 128
ENG_KEYS = ("pe", "act", "dve", "pool", "sp")
STRICT_SYNC = [True]


class Region:
    __slots__ = ("w", "rs", "name")

    def __init__(self, name=""):
        self.w = None
        self.rs = []
        self.name = name


class Sched:
    def __init__(self, nc, n_dma_sems=24):
        self.nc = nc
        self.ops = {k: [] for k in ENG_KEYS}
        self.count = {k: 0 for k in ENG_KEYS}
        self.seen = {k: {} for k in ENG_KEYS}
        self.n_dma_sems = n_dma_sems
        self.ring = n_dma_sems // 2
        self.dma_k = {"sw": 0, "hw": 0}
        self.needed = set()

    def _deps(self, reads, writes):
        deps = set()
        for r in reads:
            if r.w is not None:
                deps.add(r.w)
        for w in writes:
            if w.w is not None:
                deps.add(w.w)
            for t in w.rs:
                deps.add(t)
        return deps

    def _filter(self, eng, deps, raw, is_dma=False):
        waits = []
        seen = self.seen[eng]
        for t in sorted(deps):
            kind, key, val = t
            if kind == "e" and key == eng and not is_dma and not (STRICT_SYNC[0] and eng != "pe"):
                if eng == "pe" or t not in raw:
                    continue
            sk = (kind, key)
            if seen.get(sk, 0) >= val:
                continue
            seen[sk] = val
            waits.append(t)
            self.needed.add(t)
        return waits

    def _finish(self, tok, reads, writes):
        for r in reads:
            r.rs.append(tok)
            if len(r.rs) > 64:
                best = {}
                for t in r.rs:
                    k = (t[0], t[1])
                    if k not in best or best[k][2] < t[2]:
                        best[k] = t
                r.rs = list(best.values())
        for w in writes:
            w.w = tok
            w.rs = []

    def op(self, eng, fn, reads=(), writes=()):
        deps = self._deps(reads, writes)
        raw = set(r.w for r in reads if r.w is not None)
        waits = self._filter(eng, deps, raw)
        self.count[eng] += 1
        tok = ("e", eng, self.count[eng])
        self.ops[eng].append((waits, fn, tok))
        self._finish(tok, reads, writes)
        return tok

    def dma(self, eng, fn, reads=(), writes=()):
        deps = self._deps(reads, writes)
        rk = "sw" if eng == "pool" else "hw"
        k = self.dma_k[rk]
        self.dma_k[rk] += 1
        s = k % self.ring + (0 if rk == "sw" else self.ring)
        val = 16 * (k // self.ring + 1)
        if val > 16:
            deps.add(("d", s, val - 16))
        raw = set(r.w for r in reads if r.w is not None)
        waits = self._filter(eng, deps, raw, is_dma=True)
        tok = ("d", s, val)
        self.ops[eng].append((waits, fn, tok))
        self._finish(tok, reads, writes)
        return tok

    def final_wait(self, eng, toks):
        waits = self._filter(eng, set(toks), set())
        self.ops[eng].append((waits, None, None))

    def emit(self):
        nc = self.nc
        rank = {}
        for k in ENG_KEYS:
            idxs = sorted(v for (kind, key, v) in self.needed if kind == "e" and key == k)
            for i, v in enumerate(idxs):
                rank[(k, v)] = i + 1
        with ExitStack() as es:
            esem = {k: es.enter_context(nc.semaphore("s_" + k)) for k in ENG_KEYS}
            dsem = [es.enter_context(nc.semaphore("d_%d" % i)) for i in range(self.n_dma_sems)]
            block = es.enter_context(nc.Block())

            def run(eng_key):
                def body(e):
                    for waits, fn, tok in self.ops[eng_key]:
                        for (kind, key, val) in waits:
                            if kind == "e":
                                e.wait_ge(esem[key], rank[(key, val)])
                            else:
                                e.wait_ge(dsem[key], val)
                        if fn is None:
                            continue
                        ins = fn(e)
                        if tok[0] == "d":
                            ins.then_inc(dsem[tok[1]], 16)
                        elif tok in self.needed:
                            ins.then_inc(esem[eng_key], 1)
                return body

            block.tensor(run("pe"))
            block.scalar(run("act"))
            block.vector(run("dve"))
            block.gpsimd(run("pool"))
            block.sync(run("sp"))


def build_nc(n_pass=NPASS, n_exp=NE):
    nc = bass.Bass("TRN2", target_bir_lowering=False)

    def din(name, shape):
        return nc.dram_tensor(name, list(shape), F32, kind="ExternalInput").ap()

    x_d = din("x", [TOK_CORE, D])
    p_d = din("p", [TOK_CORE, PLE])
    ident_d = din("ident", [128, 128])
    ustr_d = din("ustrict", [128, 128])
    ebase_d = din("ebase", [128, NE])
    mix_g = din("mix_norm_g", [D])
    w_in = din("w_in", [D, 7168])
    conv_a_w = din("conv_a_w", [3, D])
    w_out_a = din("w_out_a", [D, D])
    conv_b_w = din("conv_b_w", [31, D])
    conv_b_b = din("conv_b_b", [D])
    ln_b_g = din("ln_b_g", [D])
    ln_b_b = din("ln_b_b", [D])
    w_out_b = din("w_out_b", [D, D])
    b_gate = din("b_gate", [2 * D])
    w_o = din("w_o", [D, D])
    ffn_g = din("ffn_norm_g", [D])
    w_rg = din("w_router_group", [D, 4])
    b_rg = din("b_router_group", [4])
    w_re = din("w_router_expert", [D, NE])
    b_re = din("b_router_expert", [NE])
    w_eg = din("w_exp_gate", [NE, D, DE])
    w_eu = din("w_exp_up", [NE, D, DE])
    w_ed = din("w_exp_down", [NE, DE, D])
    ple_g = din("ple_norm_g", [D])
    w_pg = din("w_ple_gate", [D, D])
    w_pp = din("w_ple_proj", [PLE, D])
    fin_g = din("final_norm_g", [D])
    out_d = nc.dram_tensor("out", [TOK_CORE, D], F32, kind="ExternalOutput").ap()
    xs_d = nc.dram_tensor("xs_scratch", [TOK_CORE, D], F32).ap()
    buf_d = nc.dram_tensor("buf_scratch", [NE * CAP, D], BF16).ap()
    ybuf_d = nc.dram_tensor("ybuf_scratch", [NE * CAP, D], BF16).ap()

    es = ExitStack()
    with es:
        def sb(name, shape, dt):
            return es.enter_context(nc.sbuf_tensor(name, list(shape), dt))

        S = Sched(nc)

        x_tok = sb("x_tok", [128, NJ, D], F32)
        hT = sb("hT", [128, 8, PT], BF16)
        uA = sb("uA", [128, 8, PT], BF16)
        uB = sb("uB", [128, 8, PT], BF16)
        mT = sb("mT", [128, 8, PT], BF16)
        slots = [sb("wslot%d" % i, [128, 12288], BF16) for i in range(2)]
        haloA = sb("haloA", [128, 8, 2], F32)
        haloB = sb("haloB", [128, 8, 30], BF16)
        identf = sb("identf", [128, 128], F32)
        identb = sb("identb", [128, 128], BF16)
        onesm = sb("onesm", [128, 128], BF16)
        ones_row = sb("ones_row", [1, 128], BF16)
        epsc = sb("epsc", [128, 1], F32)
        cv = sb("cv", [128, 8, NV], F32)
        gbc = sb("gbc", [128, D], F32)
        wr = sb("wr", [128, 8, 36], BF16)
        rb = sb("rb", [1, 36], BF16)
        ustr_b = sb("ustr_b", [128, 128], BF16)
        ones128 = sb("ones128", [128, 128], BF16)
        ebase_t = sb("ebase_t", [128, NE], F32)
        cnt_bc = sb("cnt_bc", [128, NE], F32)
        slot_i = sb("slot_i", [128, 2 * NG], mybir.dt.int32)
        slot_g = sb("slot_g", [128, 2 * NG], mybir.dt.int32)
        w12 = sb("w12", [128, 2 * NG], F32)
        maskb = sb("maskb", [128, NJ, NE], BF16)
        hn = [sb("hn%d" % i, [128, D], BF16) for i in range(2)]
        stat = sb("stat", [128, 64], F32)
        uGb = [sb("uGb%d" % i, [128, 30 + PT], BF16) for i in range(2)]
        vA = sb("vA", [128, 2 + PT], F32)
        abuf = sb("abuf", [128, PT], F32)
        vrows = abuf
        accs = [sb("acc%d" % i, [128, PT], F32) for i in range(3)]
        t512 = [sb("t512_%d" % i, [128, 512], F32) for i in range(NT5)]
        sqb = [sb("sqb%d" % i, [128, 512], BF16) for i in range(2)]
        ptile = [sb("ptile%d" % i, [128, PLE], F32) for i in range(3)]
        ptb = [sb("ptb%d" % i, [128, PLE], BF16) for i in range(2)]
        rt = sb("rt", [128, 192], F32)

        banks = [es.enter_context(nc.psum_tensor("bank%d" % i, [128, 512], F32)) for i in range(8)]

        R_x = [[Region() for _ in range(2)] for _ in range(NJ)]
        R_hT = [Region() for _ in range(NJ)]
        R_uA = [[Region() for _ in range(NTT)] for _ in range(8)]
        R_uB = [[Region() for _ in range(NTT)] for _ in range(8)]
        R_m = [[Region() for _ in range(NTT)] for _ in range(8)]
        R_slot = [Region(), Region(), Region()]
        R_haloA = [Region() for _ in range(8)]
        R_haloB = [Region() for _ in range(8)]
        R_const = Region()
        R_eps = Region()
        R_rw = Region()
        R_cv = Region()
        R_cnt = Region()
        R_slotw = [Region() for _ in range(NG)]
        R_gbc = Region()
        R_hn = [Region(), Region()]
        R_stat = [Region() for _ in range(64)]
        R_uG = [Region(), Region()]
        R_vA = Region()
        R_ab = Region()
        R_vrows = R_ab
        R_acc = [Region() for _ in range(3)]
        R_t = [Region() for _ in range(NT5)]
        R_sq = [Region(), Region()]
        R_pt = [Region(), Region(), Region()]
        R_ptb = [Region(), Region()]
        R_rt = Region()
        R_bank = [Region() for _ in range(8)]
        out_toks = []
        state = {"bank": 0, "slot": 0, "t": 0, "stat": 0, "hn": 0}

        resv = set()

        def next_bank():
            while True:
                b = state["bank"]
                state["bank"] = (b + 1) % 8
                if b not in resv:
                    return b

        t_resv = set()

        def next_t():
            while True:
                t = state["t"]
                state["t"] = (t + 1) % NT5
                if t not in t_resv:
                    return t

        def next_stat():
            t = state["stat"]
            state["stat"] = (t + 1) % 64
            return t

        def mm(out, lhsT, rhs, start, stop, reads, writes):
            S.op("pe", lambda e: e.matmul(out=out, lhsT=lhsT, rhs=rhs, start=start, stop=stop), reads, writes)

        def act(out, in_, func, reads, writes, bias=None, scale=None, accum_out=None):
            kw = {}
            if bias is not None:
                kw["bias"] = bias
            if scale is not None:
                kw["scale"] = scale
            if accum_out is not None:
                kw["accum_out"] = accum_out
            S.op("act", lambda e: e.activation(out=out, in_=in_, func=func, **kw), reads, writes)

        def tt(eng, out, in0, in1, op, reads, writes):
            S.op(eng, lambda e: e.tensor_tensor(out=out, in0=in0, in1=in1, op=op), reads, writes)

        def ts(eng, out, in0, s1, s2, op0, op1, reads, writes):
            if s2 is None:
                S.op(eng, lambda e: e.tensor_scalar(out=out, in0=in0, scalar1=s1, scalar2=None, op0=op0), reads, writes)
            else:
                S.op(eng, lambda e: e.tensor_scalar(out=out, in0=in0, scalar1=s1, scalar2=s2, op0=op0, op1=op1), reads, writes)

        def stt(out, in0, scalar, in1, op0, op1, reads, writes):
            S.op("dve", lambda e: e.scalar_tensor_tensor(out=out, in0=in0, scalar=scalar, in1=in1, op0=op0, op1=op1), reads, writes)

        def copy(eng, out, in_, reads, writes):
            S.op(eng, lambda e: e.tensor_copy(out=out, in_=in_), reads, writes)

        def memset(eng, ap, val, writes):
            S.op(eng, lambda e: e.memset(ap, val), (), writes)

        def dma(eng, out, in_, reads, writes, slow=False):
            if slow:
                return S.dma(eng, lambda e: e.dma_start(out=out, in_=in_, allow_slow_non_contiguous=True), reads, writes)
            return S.dma(eng, lambda e: e.dma_start(out=out, in_=in_), reads, writes)

        for j in range(NJ):
            dma("sp", x_tok[:, j, :], x_d[j * 128:(j + 1) * 128, :], (), [R_x[j][0], R_x[j][1]])

        dma("act", identf[:], ident_d, (), [R_const])
        copy("dve", identb[:], identf[:], [R_const], [R_const])
        memset("pool", onesm[:], 1.0 / 1024.0, [R_const])
        memset("pool", ones_row[:], 1.0, [R_const])
        memset("pool", epsc[:], EPS, [R_eps])
        dma("act", vrows[0:31, :], conv_b_w, (), [R_vrows])
        dma("act", vrows[31:34, :], conv_a_w, (), [R_vrows])
        for r, v in ((34, conv_b_b), (35, ln_b_g), (36, ln_b_b), (39, mix_g), (40, ffn_g), (41, ple_g)):
            dma("act", vrows[r:r + 1, :], v.rearrange("(o d) -> o d", o=1), (), [R_vrows])
        dma("act", vrows[37:39, :], b_gate.rearrange("(o d) -> o d", o=2), (), [R_vrows])
        dma("act", gbc[:], ffn_g.partition_broadcast(128), (), [R_gbc])
        dma("act", accs[0][:, 0:128], ustr_d, (), [R_acc[0]])
        copy("dve", ustr_b[:], accs[0][:, 0:128], [R_acc[0]], [R_const])
        dma("act", ebase_t[:], ebase_d, (), [R_const])
        memset("pool", ones128[:], 1.0, [R_const])
        memset("pool", cnt_bc[:], 0.0, [R_cnt])
        for c in range(8):
            b = next_bank()
            mm(banks[b][:, 0:NV], vrows[0:NV, c * 128:(c + 1) * 128], identf[0:NV, 0:NV], True, True,
               [R_vrows, R_const], [R_bank[b]])
            copy("dve", cv[:, c, :], banks[b][:, 0:NV], [R_bank[b]], [R_cv])
        CB0, CA0, CBB, LNG, LNB, BGA, BGB, GMIX, GFFN, GPLE = 0, 31, 34, 35, 36, 37, 38, 39, 40, 41

        def load_unit(parts, s=None):
            if s is None:
                s = state["slot"]
                state["slot"] = 1 - s
            for (off, k, n, src) in parts:
                dst = slots[s][:, off:off + k * n].rearrange("p (k n) -> p k n", k=k)
                dma("pool", dst, src.rearrange("(k p) n -> p k n", p=128), (), [R_slot[s]])
            return s

        def rstd_of(xap, xregs, junk_ap=None, junk_regs=None):
            si = next_stat()
            hb = state["hn"]
            if junk_ap is None:
                junk_ap, junk_regs = hn[hb][:], [R_hn[hb]]
            act(junk_ap, xap, AF.Square, xregs + [R_stat[si]], junk_regs + [R_stat[si]], accum_out=stat[:, si:si + 1])
            act(stat[:, si:si + 1], stat[:, si:si + 1], AF.Sqrt, [R_stat[si], R_eps], [R_stat[si]], bias=epsc[:, 0:1], scale=1.0 / D)
            S.op("dve", lambda e: e.reciprocal(out=stat[:, si:si + 1], in_=stat[:, si:si + 1]), [R_stat[si]], [R_stat[si]])
            return si

        def norm_batch(tiles, gcol=None, gain_bc=False):
            sis = []
            for (xap, xregs, dst, dregs, hn_ap, hn_regs) in tiles:
                si = next_stat()
                sis.append(si)
                act(hn[0][:], xap, AF.Square, xregs + [R_stat[si]], [R_hn[0], R_stat[si]], accum_out=stat[:, si:si + 1])
            for si in sis:
                act(stat[:, si:si + 1], stat[:, si:si + 1], AF.Sqrt, [R_stat[si], R_eps], [R_stat[si]], bias=epsc[:, 0:1], scale=1.0 / D)
            for si in sis:
                S.op("dve", lambda e, si=si: e.reciprocal(out=stat[:, si:si + 1], in_=stat[:, si:si + 1]), [R_stat[si]], [R_stat[si]])
            for si, (xap, xregs, dst, dregs, hn_ap, hn_regs) in zip(sis, tiles):
                if gain_bc:
                    stt(hn_ap, xap, stat[:, si:si + 1], gbc[:], ALU.mult, ALU.mult, xregs + [R_stat[si], R_gbc], hn_regs)
                else:
                    ts("dve", hn_ap, xap, stat[:, si:si + 1], None, ALU.mult, None, xregs + [R_stat[si]], hn_regs)
            pts = []

            def evac(i_):
                (b, pT) = pts[i_]
                (xap, xregs, dst, dregs, hn_ap, hn_regs) = tiles[i_]
                if gcol is not None:
                    tt("dve", dst, pT, cv[:, :, gcol:gcol + 1].to_broadcast([128, 8, 128]), ALU.mult, [R_bank[b], R_cv], dregs)
                elif i_ % 2 == 0:
                    act(dst, pT, AF.Copy, [R_bank[b]], dregs)
                else:
                    copy("dve", dst, pT, [R_bank[b]], dregs)

            for i_, (xap, xregs, dst, dregs, hn_ap, hn_regs) in enumerate(tiles):
                b = next_bank()
                pT = banks[b][:].bitcast(BF16).rearrange("p (c t) -> p c t", c=8)
                pts.append((b, pT))
                for c in range(8):
                    S.op("pe", lambda e, c=c, pT=pT, hn_ap=hn_ap: e.transpose(out=pT[:, c, :], in_=hn_ap[:, c * 128:(c + 1) * 128], identity=identb[:]),
                         hn_regs + [R_const], [R_bank[b]])
                if i_ >= 2:
                    evac(i_ - 2)
            for i_ in range(max(0, len(tiles) - 2), len(tiles)):
                evac(i_)

        def norm_T(xap, xregs, dst, dregs, gcol=None, gain_bc=False, hn_ap=None, hn_regs=None):
            si = rstd_of(xap, xregs)
            hb = state["hn"]
            state["hn"] = 1 - hb
            if hn_ap is None:
                hn_ap, hn_regs = hn[hb][:], [R_hn[hb]]
            if gain_bc:
                stt(hn_ap, xap, stat[:, si:si + 1], gbc[:], ALU.mult, ALU.mult, xregs + [R_stat[si], R_gbc], hn_regs)
            else:
                ts("dve", hn_ap, xap, stat[:, si:si + 1], None, ALU.mult, None, xregs + [R_stat[si]], hn_regs)
            b = next_bank()
            pT = banks[b][:].bitcast(BF16).rearrange("p (c t) -> p c t", c=8)
            for c in range(8):
                S.op("pe", lambda e, c=c: e.transpose(out=pT[:, c, :], in_=hn_ap[:, c * 128:(c + 1) * 128], identity=identb[:]),
                     hn_regs + [R_const], [R_bank[b]])
            if gcol is not None:
                tt("dve", dst, pT, cv[:, :, gcol:gcol + 1].to_broadcast([128, 8, 128]), ALU.mult, [R_bank[b], R_cv], dregs)
            else:
                act(dst, pT, AF.Copy, [R_bank[b]], dregs)
            return hb

        def barrier():
            toks = [("e", k, S.count[k]) for k in ("pe", "act", "dve", "pool") if S.count[k] > 0]
            for rk, base in (("sw", 0), ("hw", S.ring)):
                nk = S.dma_k[rk]
                for s_ in range(min(nk, S.ring)):
                    k_last = ((nk - 1 - s_) // S.ring) * S.ring + s_
                    toks.append(("d", base + s_, 16 * (k_last // S.ring + 1)))
            for eng in ENG_KEYS:
                S.final_wait(eng, toks)

        def hT_reads(t):
            return [R_hT[4 * t + i] for i in range(4)]

        zero_toks = []
        _breg = {}

        def bcheck(e):
            if "r" not in _breg:
                _breg["r"] = e.to_reg(NE * CAP - 1)
            return _breg["r"]

        scat_toks, xs_toks = [], []
        nxt = None
        for ps in range(n_pass):
            tok0 = ps * PT
            first_half = (ps % 2 == 0)
            for j in range(NJ):
                if ps > 0:
                    dma("sp", x_tok[:, j, :], x_d[tok0 + j * 128: tok0 + (j + 1) * 128, :], (), [R_x[j][0], R_x[j][1]])

            def s1_parts(g_):
                return [(j * 2048, 8, 256, w_in[:, j * 1024 + g_ * 256: j * 1024 + (g_ + 1) * 256]) for j in range(5)]

            def ypartsA(g_):
                return [(0, 8, 512, w_out_a[:, g_ * 512:(g_ + 1) * 512]),
                        (4096, 8, 512, w_in[:, 5120 + g_ * 512: 5120 + (g_ + 1) * 512])]

            def ypartsB(g_):
                return [(0, 8, 512, w_out_b[:, g_ * 512:(g_ + 1) * 512]),
                        (4096, 8, 512, w_in[:, 6144 + g_ * 512: 6144 + (g_ + 1) * 512])]

            if nxt is None:
                nxt = load_unit(s1_parts(0))
            if ps == 0:
                dma("pool", wr[:, :, 0:4], w_rg.rearrange("(k p) n -> p k n", p=128), (), [R_rw])
                dma("pool", wr[:, :, 4:36], w_re.rearrange("(k p) n -> p k n", p=128), (), [R_rw])
                dma("pool", rb[0:1, 0:4], b_rg.rearrange("(o d) -> o d", o=1), (), [R_rw])
                dma("pool", rb[0:1, 4:36], b_re.rearrange("(o d) -> o d", o=1), (), [R_rw])
            norm_batch([(x_tok[:, j, :], [R_x[j][0], R_x[j][1]], hT[:, :, j * 128:(j + 1) * 128], [R_hT[j]],
                         mT[:, j, :], [R_m[j][0], R_m[j][1]]) for j in range(NJ)], gcol=GMIX)

            if ps == 0:
                for c in range(8):
                    memset("pool", mT[:, c, :], 0.0, [R_m[c][0], R_m[c][1]])
                buf_v = buf_d.rearrange("(n p) d -> p n d", p=128)
                zero_pending = list(range(NE * CAP // 128 // 8))

                def zero_some(n_):
                    for _ in range(n_):
                        if zero_pending:
                            i = zero_pending.pop(0)
                            zero_toks.append(dma("act", buf_v[:, 8 * i:8 * i + 8, :], mT[:],
                                                 [R_m[c_][t_] for c_ in range(8) for t_ in range(NTT)], [Region()]))


            dgA = accs[0][:].bitcast(BF16).rearrange("p (k m) -> p k m", m=128)
            dgB = accs[1][:].bitcast(BF16).rearrange("p (k m) -> p k m", m=128)[:, 0:15, :]

            KPE = 23

            def dg_build(c):
                tt("dve", dgA, identb[:].unsqueeze(1).to_broadcast([128, 16, 128]),
                   cv[:, c, CB0:CB0 + 16].unsqueeze(2).to_broadcast([128, 16, 128]), ALU.mult, [R_const, R_cv], [R_acc[0]])
                tt("dve", dgB[:, 0:KPE - 16, :], identb[:].unsqueeze(1).to_broadcast([128, KPE - 16, 128]),
                   cv[:, c, CB0 + 16:CB0 + KPE].unsqueeze(2).to_broadcast([128, KPE - 16, 128]), ALU.mult, [R_const, R_cv], [R_acc[1]])

            def conv_pe(c, ub):
                bks = []
                for t in range(NTT):
                    b = next_bank()
                    bks.append(b)
                    for k in range(KPE):
                        lhsT = dgA[:, k, :] if k < 16 else dgB[:, k - 16, :]
                        mm(banks[b][:], lhsT, uGb[ub][:, k + t * 512: k + (t + 1) * 512], k == 0, k == KPE - 1,
                           [R_acc[0], R_acc[1], R_uG[ub]], [R_bank[b]])
                tmps = [next_t() for _ in range(NTT)]
                for idx, k in enumerate(range(KPE, 31)):
                    for t in range(NTT):
                        if idx == 0:
                            in1, in1_regs = banks[bks[t]][:], [R_bank[bks[t]]]
                        else:
                            in1, in1_regs = t512[tmps[t]][:], [R_t[tmps[t]]]
                        stt(t512[tmps[t]][:], uGb[ub][:, k + t * 512: k + (t + 1) * 512], cv[:, c, CB0 + k:CB0 + k + 1], in1,
                            ALU.mult, ALU.add, [R_uG[ub], R_cv] + in1_regs, [R_t[tmps[t]]])
                for t in range(NTT):
                    act(uB[:, c, t * 512:(t + 1) * 512], t512[tmps[t]][:], AF.Identity, [R_t[tmps[t]], R_cv], [R_uB[c][t]],
                        bias=cv[:, c, CBB:CBB + 1])

            for c in range(8):
                ub = c % 2
                uG = uGb[ub]
                if c % 2 == 0:
                    s = nxt
                    nxt = load_unit(s1_parts(c // 2 + 1) if c < 6 else ypartsA(0))
                if ps == 0:
                    zero_some(3 if c < 7 else 99)
                if c >= 1:
                    dg_build(c - 1)
                if first_half:
                    memset("pool", uG[:, 0:30], 0.0, [R_uG[ub]])
                    memset("pool", vA[:, 0:2], 0.0, [R_vA])
                else:
                    copy("pool", uG[:, 0:30], haloB[:, c, :], [R_haloB[c]], [R_uG[ub]])
                    copy("pool", vA[:, 0:2], haloA[:, c, :], [R_haloA[c]], [R_vA])
                for t in range(NTT):
                    bk = []
                    for j in range(5):
                        b = next_bank()
                        bk.append(b)
                        for k in range(8):
                            mm(banks[b][:], slots[s][:, j * 2048 + k * 256 + ub * 128: j * 2048 + k * 256 + (ub + 1) * 128],
                               hT[:, k, t * 512:(t + 1) * 512], k == 0, k == 7,
                               [R_slot[s]] + hT_reads(t), [R_bank[b]])
                    sl = slice(t * 512, (t + 1) * 512)
                    act(abuf[:, sl], banks[bk[0]][:], AF.Copy, [R_bank[bk[0]]], [R_ab])
                    tx = next_t()
                    act(t512[tx][:], banks[bk[2]][:], AF.Copy, [R_bank[bk[2]]], [R_t[tx]])
                    tt("dve", vA[:, 2 + t * 512: 2 + (t + 1) * 512], banks[bk[1]][:], t512[tx][:], ALU.mult,
                       [R_bank[bk[1]], R_t[tx]], [R_vA])
                    tg = next_t()
                    act(t512[tg][:], banks[bk[4]][:], AF.Sigmoid, [R_bank[bk[4]]], [R_t[tg]])
                    tt("dve", uG[:, 30 + t * 512: 30 + (t + 1) * 512], banks[bk[3]][:], t512[tg][:], ALU.mult,
                       [R_bank[bk[3]], R_t[tg]], [R_uG[ub]])
                if c >= 1:
                    conv_pe(c - 1, 1 - ub)
                a2 = accs[2]
                ts("dve", a2[:], vA[:, 0:PT], cv[:, c, CA0:CA0 + 1], None, ALU.mult, None, [R_vA, R_cv], [R_acc[2]])
                for k in range(1, 3):
                    stt(a2[:], vA[:, k:k + PT], cv[:, c, CA0 + k:CA0 + k + 1], a2[:], ALU.mult, ALU.add,
                        [R_vA, R_cv, R_acc[2]], [R_acc[2]])
                tt("dve", uA[:, c, :], a2[:], abuf[:], ALU.mult, [R_acc[2], R_ab], [R_uA[c][0], R_uA[c][1]])
                if first_half:
                    copy("pool", haloB[:, c, :], uG[:, PT:PT + 30], [R_uG[ub]], [R_haloB[c]])
                    copy("pool", haloA[:, c, :], vA[:, PT:PT + 2], [R_vA], [R_haloA[c]])
            dg_build(7)
            conv_pe(7, 1)

            ln_tm, ln_tr = [None] * NTT, [None] * NTT

            def ln_stats():
                for t in range(NTT):
                    sl = slice(t * 512, (t + 1) * 512)
                    bm = next_bank()
                    bq = next_bank()
                    for c in range(8):
                        q = c % 2
                        act(sqb[q][:], uB[:, c, sl], AF.Square, [R_uB[c][t]], [R_sq[q]])
                        mm(banks[bm][:], onesm[:], uB[:, c, sl], c == 0, c == 7, [R_const, R_uB[c][t]], [R_bank[bm]])
                        mm(banks[bq][:], onesm[:], sqb[q][:], c == 0, c == 7, [R_const, R_sq[q]], [R_bank[bq]])
                    tm, tq, tr = next_t(), next_t(), next_t()
                    t_resv.add(tm)
                    t_resv.add(tr)
                    act(t512[tm][:], banks[bm][:], AF.Copy, [R_bank[bm]], [R_t[tm]])
                    act(t512[tq][:], banks[bm][:], AF.Square, [R_bank[bm]], [R_t[tq]])
                    stt(t512[tr][:], banks[bq][:], EPS, t512[tq][:], ALU.add, ALU.subtract, [R_bank[bq], R_t[tq]], [R_t[tr]])
                    act(t512[tr][:], t512[tr][:], AF.Sqrt, [R_t[tr]], [R_t[tr]])
                    S.op("dve", lambda e, tr=tr: e.reciprocal(out=t512[tr][:], in_=t512[tr][:]), [R_t[tr]], [R_t[tr]])
                    ln_tm[t], ln_tr[t] = tm, tr

            def ln_norm(c):
                for t in range(NTT):
                    sl = slice(t * 512, (t + 1) * 512)
                    tm, tr = ln_tm[t], ln_tr[t]
                    ta = next_t()
                    tt("dve", t512[ta][:], uB[:, c, sl], t512[tm][:], ALU.subtract, [R_uB[c][t], R_t[tm]], [R_t[ta]])
                    tt("dve", t512[ta][:], t512[ta][:], t512[tr][:], ALU.mult, [R_t[ta], R_t[tr]], [R_t[ta]])
                    act(uB[:, c, sl], t512[ta][:], AF.Silu, [R_t[ta], R_cv], [R_uB[c][t]],
                        bias=cv[:, c, LNB:LNB + 1], scale=cv[:, c, LNG:LNG + 1])

            def sweep(c, s, src_, rr_, bcol, first):
                cc = c % 4
                for t in range(NTT):
                    sl = slice(t * 512, (t + 1) * 512)
                    by = next_bank()
                    bg = next_bank()
                    for k in range(8):
                        mm(banks[by][:], slots[s][:, k * 512 + cc * 128: k * 512 + (cc + 1) * 128], src_[:, k, sl], k == 0, k == 7,
                           [R_slot[s], rr_[k][t]], [R_bank[by]])
                    for k in range(8):
                        mm(banks[bg][:], slots[s][:, 4096 + k * 512 + cc * 128: 4096 + k * 512 + (cc + 1) * 128], hT[:, k, sl],
                           k == 0, k == 7, [R_slot[s]] + hT_reads(t), [R_bank[bg]])
                    sg = next_t()
                    act(t512[sg][:], banks[bg][:], AF.Sigmoid, [R_bank[bg], R_cv], [R_t[sg]], bias=cv[:, c, bcol:bcol + 1])
                    if first:
                        tt("dve", mT[:, c, sl], banks[by][:], t512[sg][:], ALU.mult, [R_bank[by], R_t[sg]], [R_m[c][t]])
                    else:
                        t2 = next_t()
                        tt("dve", t512[t2][:], banks[by][:], t512[sg][:], ALU.mult, [R_bank[by], R_t[sg]], [R_t[t2]])
                        tt("pool", mT[:, c, sl], mT[:, c, sl], t512[t2][:], ALU.add, [R_m[c][t], R_t[t2]], [R_m[c][t]])

            for c in range(8):
                if c % 4 == 0:
                    s = nxt
                    nxt = load_unit(ypartsA(1) if c == 0 else ypartsB(0))
                sweep(c, s, uA, R_uA, BGA, True)
                if c == 0:
                    ln_stats()
                else:
                    ln_norm(c - 1)
            ln_norm(7)
            t_resv.clear()
            for c in range(8):
                if c % 4 == 0:
                    s = nxt
                    nxt = load_unit(ypartsB(1) if c == 0 else [(0, 8, 1024, w_o)])
                sweep(c, s, uB, R_uB, BGB, False)

            s = nxt

            def exp_parts(e):
                return [(0, 8, 512, w_eg[e]), (4096, 8, 512, w_eu[e]), (8192, 4, 1024, w_ed[e])]

            ple_parts = [(0, 8, 1024, w_pg), (8192, 2, 1024, w_pp)]
            if ps + 1 < n_pass:
                nxt = load_unit(s1_parts(0))
            else:
                nxt = load_unit(exp_parts(0) if n_exp > 0 else ple_parts)
            for j in range(NJ):
                for h in range(2):
                    b = next_bank()
                    for k in range(8):
                        mm(banks[b][:], mT[:, k, j * 128:(j + 1) * 128], slots[s][:, k * 1024 + h * 512: k * 1024 + (h + 1) * 512],
                           k == 0, k == 7, [R_slot[s], R_m[k][j // 4]], [R_bank[b]])
                    tt("dve", x_tok[:, j, h * 512:(h + 1) * 512], x_tok[:, j, h * 512:(h + 1) * 512], banks[b][:], ALU.add,
                       [R_x[j][h], R_bank[b]], [R_x[j][h]])

            bL = next_bank()
            resv.add(bL)
            Lps = banks[bL][:].rearrange("p (j c) -> p j c", j=NJ)
            for j in range(NJ):
                xs_toks.append(dma("sp", xs_d[tok0 + j * 128: tok0 + (j + 1) * 128, :], x_tok[:, j, :], [R_x[j][0], R_x[j][1]], [Region()]))
            norm_batch([(x_tok[:, j, :], [R_x[j][0], R_x[j][1]], hT[:, :, j * 128:(j + 1) * 128], [R_hT[j]],
                         uA[:, j, :], [R_uA[j][0], R_uA[j][1]]) for j in range(NJ)], gain_bc=True)
            for j in range(NJ):
                for k in range(8):
                    mm(Lps[:, j, 0:36], hT[:, k, j * 128:(j + 1) * 128], wr[:, k, :], k == 0, False,
                       [R_hT[j], R_const, R_rw], [R_bank[bL]])
                mm(Lps[:, j, 0:36], ones_row[0:1, :], rb[0:1, :], False, True, [R_const, R_rw], [R_bank[bL]])
            A0, A1 = accs[0], accs[1]
            rr = [R_acc[0], R_acc[1]]
            rw = rr
            v3 = lambda ap, n: ap.rearrange("p (j c) -> p j c", j=NJ)
            L = v3(A0[:, 0:288], 36)
            sel = v3(A0[:, 288:544], 32)
            mask1 = v3(A0[:, 544:800], 32)
            sm = lambda i: A0[:, 800 + 8 * i: 808 + 8 * i]
            gmax, gsum, pgrp, m1, m2, dd, e2, den, w1, w2, s1f, s2f, ov1, ov2 = (sm(i) for i in range(14))
            gmask = v3(A0[:, 912:944], 4)
            pen = v3(A0[:, 944:976], 4)
            gex = v3(A0[:, 976:1008], 4)
            mask2 = v3(A1[:, 0:256], 32)
            rank = v3(A1[:, 256:512], 32)
            over = v3(A1[:, 512:768], 32)
            sel2 = v3(A1[:, 768:1024], 32)
            tmp = sel2
            bc = lambda ap, n: ap.unsqueeze(2).to_broadcast([128, NJ, n])
            red = lambda out, in_, op: S.op("dve", lambda e: e.tensor_reduce(out=out, in_=in_, axis=AX.X, op=op), rr, rw)
            act(L, Lps[:, :, 0:36], AF.Copy, [R_bank[bL]], rw)
            resv.discard(bL)
            red(gmax, L[:, :, 0:4], ALU.max)
            tt("dve", gmask, L[:, :, 0:4], bc(gmax, 4), ALU.is_ge, rr, rw)
            tt("dve", gex, L[:, :, 0:4], bc(gmax, 4), ALU.subtract, rr, rw)
            act(gex, gex, AF.Exp, rr, rw)
            red(gsum, gex, ALU.add)
            S.op("dve", lambda e: e.reciprocal(out=pgrp, in_=gsum), rr, rw)
            ts("dve", pen, gmask, 1.0, 1e30, ALU.subtract, ALU.mult, rr, rw)
            tt("dve", sel.rearrange("p j (g e) -> p j g e", g=4), L[:, :, 4:36].rearrange("p j (g e) -> p j g e", g=4),
               pen.unsqueeze(3).to_broadcast([128, NJ, 4, 8]), ALU.add, rr, rw)
            red(m1, sel, ALU.max)
            tt("dve", mask1, sel, bc(m1, 32), ALU.is_ge, rr, rw)
            stt(sel2, mask1, -1e30, sel, ALU.mult, ALU.add, rr, rw)
            red(m2, sel2, ALU.max)
            tt("dve", mask2, sel2, bc(m2, 32), ALU.is_ge, rr, rw)
            tt("dve", dd, m2, m1, ALU.subtract, rr, rw)
            act(e2, dd, AF.Exp, rr, rw)
            ts("dve", den, e2, 1.0, None, ALU.add, None, rr, rw)
            S.op("dve", lambda e: e.reciprocal(out=den, in_=den), rr, rw)
            tt("dve", w1, den, pgrp, ALU.mult, rr, rw)
            tt("dve", w2, w1, e2, ALU.mult, rr, rw)
            R_mb = Region()
            tt("dve", maskb[:], mask1, mask2, ALU.add, rr, [R_mb])
            b2 = next_bank()
            Rps = banks[b2][:].rearrange("p (j c) -> p j c", j=NJ)
            for j in range(NJ):
                mm(Rps[:, j, 0:NE], ustr_b[:], maskb[:, j, :], True, j == 0, [R_const, R_mb], [R_bank[b2]])
                for j2 in range(j):
                    mm(Rps[:, j, 0:NE], ones128[:], maskb[:, j2, :], False, j2 == j - 1, [R_const, R_mb], [R_bank[b2]])
            for j2 in range(NJ):
                mm(Rps[:, NJ - 1, NE:2 * NE], ones128[:], maskb[:, j2, :], j2 == 0, j2 == NJ - 1, [R_const, R_mb], [R_bank[b2]])
            tt("dve", rank, Rps[:, :, 0:NE], cnt_bc[:].unsqueeze(1).to_broadcast([128, NJ, NE]), ALU.add, rr + [R_bank[b2], R_cnt], rw)
            tt("dve", cnt_bc[:], cnt_bc[:], Rps[:, NJ - 1, NE:2 * NE], ALU.add, [R_cnt, R_bank[b2]], [R_cnt])
            ts("dve", over, rank, float(CAP), None, ALU.is_ge, None, rr, rw)
            tt("dve", rank, rank, ebase_t[:].unsqueeze(1).to_broadcast([128, NJ, NE]), ALU.add, rr + [R_const], rw)
            stt(rank, over, 1.0e6, rank, ALU.mult, ALU.add, rr, rw)
            for (mk, src_, dst_) in ((mask1, rank, s1f), (mask2, rank, s2f), (mask1, over, ov1), (mask2, over, ov2)):
                tt("dve", tmp, mk, src_, ALU.mult, rr, rw)
                red(dst_, tmp, ALU.add)
            ts("dve", ov1, ov1, -1.0, 1.0, ALU.mult, ALU.add, rr, rw)
            ts("dve", ov2, ov2, -1.0, 1.0, ALU.mult, ALU.add, rr, rw)
            g0 = ps * NJ
            R_sw = [R_slotw[g0 + j] for j in range(NJ)]
            w12v = w12[:, 2 * g0:2 * g0 + 2 * NJ].rearrange("p (j k) -> p j k", k=2)
            siv = slot_i[:, 2 * g0:2 * g0 + 2 * NJ].rearrange("p (j k) -> p j k", k=2)
            tt("dve", w12v[:, :, 0], w1, ov1, ALU.mult, rr, R_sw)
            tt("dve", w12v[:, :, 1], w2, ov2, ALU.mult, rr, R_sw)
            copy("dve", siv[:, :, 0], s1f, rr, R_sw)
            copy("dve", siv[:, :, 1], s2f, rr, R_sw)
            sgv = slot_g[:, 2 * g0:2 * g0 + 2 * NJ].rearrange("p (j k) -> p j k", k=2)
            ts("dve", s1f, s1f, float(NE * CAP - 1), None, ALU.min, None, rr, rw)
            ts("dve", s2f, s2f, float(NE * CAP - 1), None, ALU.min, None, rr, rw)
            copy("dve", sgv[:, :, 0], s1f, rr, R_sw)
            copy("dve", sgv[:, :, 1], s2f, rr, R_sw)
            if ps == 0:
                S.final_wait("pool", zero_toks)
            for j in range(NJ):
                g = g0 + j
                for kk in range(2):
                    idx = slot_i[:, 2 * g + kk:2 * g + kk + 1]
                    scat_toks.append(S.dma("pool", lambda e, idx=idx, j=j: e.indirect_dma_start(
                        out=buf_d, out_offset=bass.IndirectOffsetOnAxis(ap=idx, axis=0), in_=uA[:, j, :], in_offset=None,
                        bounds_check=bcheck(e), oob_is_err=False), [R_uA[j][0], R_uA[j][1], R_slotw[g]], [Region()]))

        barrier()
        hTe = [uA[:, :, 0:CAP], uB[:, :, 0:CAP]]
        actE = [mT[:, 0:4, 0:CAP], mT[:, 4:8, 0:CAP]]
        hbt = [hT[:, i, :] for i in (0, 1, 2, 5, 6, 7)]
        yts = [hT[:, 3 + i, :] for i in range(2)]
        R_hTe = [[Region() for _ in range(NBLK)] for _ in range(2)]
        R_actE = [[Region() for _ in range(4)] for _ in range(2)]
        R_hbt = [Region() for _ in range(6)]
        R_yts = [Region() for _ in range(2)]
        ystore_toks = []
        cnt = {"hbt": 0, "yt": 0}
        def p2_T(ex):
            eb = ex % 2
            for blk in range(NBLK):
                hi = cnt["hbt"] % 6
                cnt["hbt"] += 1
                r0 = ex * CAP + blk * 128
                dma("sp", hbt[hi], buf_d[r0:r0 + 128, :], (), [R_hbt[hi]])
                b = next_bank()
                pT = banks[b][:].bitcast(BF16).rearrange("p (c t) -> p c t", c=8)
                for c in range(8):
                    S.op("pe", lambda e, c=c, pT=pT, hi=hi: e.transpose(out=pT[:, c, :], in_=hbt[hi][:, c * 128:(c + 1) * 128], identity=identb[:]),
                         [R_hbt[hi], R_const], [R_bank[b]])
                if blk % 2 == 0:
                    act(hTe[eb][:, :, blk * 128:(blk + 1) * 128], pT, AF.Copy, [R_bank[b]], [R_hTe[eb][blk]])
                else:
                    copy("dve", hTe[eb][:, :, blk * 128:(blk + 1) * 128], pT, [R_bank[b]], [R_hTe[eb][blk]])

        def p2_GU(ex, s):
            eb = ex % 2
            for q in range(4):
                for cg in range(CAP // CG):
                    cs = slice(cg * CG, (cg + 1) * CG)
                    bg = next_bank()
                    bu = next_bank()
                    for k in range(8):
                        mm(banks[bg][:, 0:CG], slots[s][:, k * 512 + q * 128: k * 512 + (q + 1) * 128], hTe[eb][:, k, cs],
                           k == 0, k == 7, [R_slot[s]] + R_hTe[eb], [R_bank[bg]])
                    for k in range(8):
                        mm(banks[bu][:, 0:CG], slots[s][:, 4096 + k * 512 + q * 128: 4096 + k * 512 + (q + 1) * 128], hTe[eb][:, k, cs],
                           k == 0, k == 7, [R_slot[s]] + R_hTe[eb], [R_bank[bu]])
                    tg = next_t()
                    act(t512[tg][:, 0:CG], banks[bg][:, 0:CG], AF.Silu, [R_bank[bg]], [R_t[tg]])
                    tt("dve", actE[eb][:, q, cs], banks[bu][:, 0:CG], t512[tg][:, 0:CG], ALU.mult, [R_bank[bu], R_t[tg]], [R_actE[eb][q]])

        def p2_DN(ex, s):
            eb = ex % 2
            for blk in range(NBLK):
                yi = cnt["yt"] % 2
                cnt["yt"] += 1
                for h in range(2):
                    b = next_bank()
                    for q in range(4):
                        mm(banks[b][:], actE[eb][:, q, blk * 128:(blk + 1) * 128],
                           slots[s][:, 8192 + q * 1024 + h * 512: 8192 + q * 1024 + (h + 1) * 512],
                           q == 0, q == 3, [R_slot[s], R_actE[eb][q]], [R_bank[b]])
                    if h == 0:
                        act(yts[yi][:, 0:512], banks[b][:], AF.Copy, [R_bank[b]], [R_yts[yi]])
                    else:
                        copy("dve", yts[yi][:, 512:1024], banks[b][:], [R_bank[b]], [R_yts[yi]])
                r0 = ex * CAP + blk * 128
                ystore_toks.append(dma("sp", ybuf_d[r0:r0 + 128, :], yts[yi], [R_yts[yi]], [Region()]))

        slots.append(x_tok[:, 2:8, :].rearrange("p a b -> p (a b)").bitcast(BF16))
        s0 = nxt
        seq = [s0, 2, 1 - s0]
        if n_exp > 0:
            p2_T(0)
        if n_exp > 1:
            load_unit(exp_parts(1), seq[1])
        for ex in range(n_exp):
            s = seq[ex % 3]
            if ex + 2 < n_exp:
                load_unit(exp_parts(ex + 2), seq[(ex + 2) % 3])
            elif ex + 2 == n_exp:
                free01 = [q for q in (0, 1) if q not in (seq[ex % 3], seq[(ex + 1) % 3])]
                nxt = load_unit(ple_parts, free01[0])
            p2_GU(ex, s)
            if ex + 1 < n_exp:
                p2_T(ex + 1)
            p2_DN(ex, s)
        if n_exp < 2:
            nxt = load_unit(ple_parts, 1 - s0)

        barrier()
        s = nxt
        dma("sp", gbc[:], fin_g.partition_broadcast(128), (), [R_gbc])
        xt3 = [x_tok[:, i, :] for i in range(7)]
        y3 = [[uB[:, 2 * i, :], uB[:, 2 * i + 1, :]] for i in range(3)]
        ot3 = [accs[0][:], accs[1][:]]
        hT3 = [hT[:, :, i * 128:(i + 1) * 128] for i in range(3)]
        pT3 = [uA[:, 0:2, i * 128:(i + 1) * 128] for i in range(3)]
        R_xt3 = [[Region(), Region()] for _ in range(7)]
        R_y3 = [[Region(), Region()] for _ in range(3)]
        R_ot3 = [R_acc[0], R_acc[1]]
        R_hT3 = [Region(), Region(), Region()]
        R_pT3 = [Region(), Region(), Region()]
        n_tiles = n_pass * NJ

        def p3_s0(g):
            i, i3, iy = g % 2, g % 7, g % 3
            r0 = g * 128
            dma("sp", xt3[i3], xs_d[r0:r0 + 128, :], (), R_xt3[i3])
            dma("sp", ptile[iy][:], p_d[r0:r0 + 128, :], (), [R_pt[iy]])
            for kk in range(2):
                idx = slot_g[:, 2 * g + kk:2 * g + kk + 1]
                S.dma("pool", lambda e, idx=idx, dst=y3[iy][kk]: e.indirect_dma_start(
                    out=dst, out_offset=None, in_=ybuf_d, in_offset=bass.IndirectOffsetOnAxis(ap=idx, axis=0),
                    bounds_check=bcheck(e), oob_is_err=False), [R_slotw[g]], [R_y3[iy][kk]])

        junk3 = mT[:, 7, :]
        R_junk3 = [Region()]

        def p3_s1a(g):
            i, i3, iy = g % 2, g % 7, g % 3
            for kk in range(2):
                for h in range(2):
                    hs = slice(h * 512, (h + 1) * 512)
                    stt(xt3[i3][:, hs], y3[iy][kk][:, hs], w12[:, 2 * g + kk:2 * g + kk + 1], xt3[i3][:, hs], ALU.mult, ALU.add,
                        [R_y3[iy][kk], R_slotw[g], R_xt3[i3][h]], [R_xt3[i3][h]])
            si = rstd_of(xt3[i3], R_xt3[i3], junk3, R_junk3)
            ts("dve", hn[i][:], xt3[i3], stat[:, si:si + 1], None, ALU.mult, None, R_xt3[i3] + [R_stat[si]], [R_hn[i]])
            copy("pool", ptb[i][:], ptile[iy][:], [R_pt[iy]], [R_ptb[i]])

        def p3_s1b(g):
            i, i3, iy = g % 2, g % 7, g % 3
            b = next_bank()
            pT = banks[b][:].bitcast(BF16).rearrange("p (c t) -> p c t", c=8)
            for c in range(8):
                S.op("pe", lambda e, c=c: e.transpose(out=pT[:, c, :], in_=hn[i][:, c * 128:(c + 1) * 128], identity=identb[:]),
                     [R_hn[i], R_const], [R_bank[b]])
            b2 = next_bank()
            pT2 = banks[b2][:].bitcast(BF16).rearrange("p (c t) -> p c t", c=8)
            for c in range(2):
                S.op("pe", lambda e, c=c: e.transpose(out=pT2[:, c, :], in_=ptb[i][:, c * 128:(c + 1) * 128], identity=identb[:]),
                     [R_ptb[i], R_const], [R_bank[b2]])
            tt("dve", hT3[iy], pT, cv[:, :, GPLE:GPLE + 1].to_broadcast([128, 8, 128]), ALU.mult, [R_bank[b], R_cv], [R_hT3[iy]])
            act(pT3[iy], pT2[:, 0:2, :], AF.Copy, [R_bank[b2]], [R_pT3[iy]])

        def p3_s2(g):
            i, i3, iy = g % 2, g % 7, g % 3
            for h in range(2):
                hs = slice(h * 512, (h + 1) * 512)
                bg = next_bank()
                bp = next_bank()
                for k in range(8):
                    mm(banks[bg][:], hT3[iy][:, k, :], slots[s][:, k * 1024 + h * 512: k * 1024 + (h + 1) * 512],
                       k == 0, k == 7, [R_slot[s], R_hT3[iy]], [R_bank[bg]])
                for k in range(2):
                    mm(banks[bp][:], pT3[iy][:, k, :], slots[s][:, 8192 + k * 1024 + h * 512: 8192 + k * 1024 + (h + 1) * 512],
                       k == 0, k == 1, [R_slot[s], R_pT3[iy]], [R_bank[bp]])
                tg, tp = next_t(), next_t()
                act(t512[tg][:], banks[bg][:], AF.Sigmoid, [R_bank[bg]], [R_t[tg]])
                tt("dve", t512[tp][:], banks[bp][:], t512[tg][:], ALU.mult, [R_bank[bp], R_t[tg]], [R_t[tp]])
                tt("pool", xt3[i3][:, hs], xt3[i3][:, hs], t512[tp][:], ALU.add, [R_xt3[i3][h], R_t[tp]], [R_xt3[i3][h]])

        def p3_s3(g):
            i, i3, iy = g % 2, g % 7, g % 3
            r0 = g * 128
            si = rstd_of(xt3[i3], R_xt3[i3], junk3, R_junk3)
            stt(ot3[i], xt3[i3], stat[:, si:si + 1], gbc[:], ALU.mult, ALU.mult, R_xt3[i3] + [R_stat[si], R_gbc], [R_ot3[i]])
            out_toks.append(dma("sp", out_d[r0:r0 + 128, :], ot3[i], [R_ot3[i]], [Region()]))

        p3_s0(0)
        if n_tiles > 1:
            p3_s0(1)
        for step in range(n_tiles + 4):
            if 0 <= step - 4 < n_tiles:
                p3_s3(step - 4)
            if 0 <= step - 2 < n_tiles:
                p3_s2(step - 2)
            if 0 <= step - 1 < n_tiles:
                p3_s1b(step - 1)
            if step + 2 < n_tiles:
                p3_s0(step + 2)
            if step < n_tiles:
                p3_s1a(step)

        S.final_wait("sp", out_toks)
        S.emit()
    return nc


_NC_CACHE = {}


def make_in_maps(inputs, ncores=NCORES):
    f = lambda a: np.ascontiguousarray(np.asarray(a, dtype=np.float32))
    x = f(inputs["x"]).reshape(NCORES, TOK_CORE, D)
    p = f(inputs["p"]).reshape(NCORES, TOK_CORE, PLE)
    shared = {"ident": np.eye(128, dtype=np.float32),
              "ustrict": np.triu(np.ones((128, 128), dtype=np.float32), k=1),
              "ebase": np.ascontiguousarray(np.broadcast_to((np.arange(NE, dtype=np.float32) * CAP)[None, :], (128, NE)))}
    for name in ("mix_norm_g", "w_in", "conv_a_w", "w_out_a", "conv_b_w", "conv_b_b", "ln_b_g", "ln_b_b", "w_out_b",
                 "b_gate", "w_o", "ffn_norm_g", "w_router_group", "b_router_group", "w_router_expert", "b_router_expert",
                 "w_exp_gate", "w_exp_up", "w_exp_down", "ple_norm_g", "w_ple_gate", "w_ple_proj"):
        a = f(inputs[name])
        shared[name] = np.ascontiguousarray(a.reshape(a.shape[1:]))
    shared["final_norm_g"] = f(inputs["final_norm_g"])
    in_maps = []
    for c in range(ncores):
        m = dict(shared)
        m["x"] = x[c]
        m["p"] = p[c]
        in_maps.append(m)
    return in_maps


def kernel(**inputs):
    in_maps = make_in_maps(inputs)
    if "nc" not in _NC_CACHE:
        _NC_CACHE["nc"] = build_nc()
    nc = _NC_CACHE["nc"]
    res = run_bass_kernel_spmd(nc, in_maps, core_ids=list(range(NCORES)))
    out = np.stack([np.asarray(r["out"]) for r in res.results], axis=0)
    return out.reshape(16, SEQ, D).astype(np.float32)
```
